# Optimizing a Trainium2 kernel written in Bass

```python
import math
import jax, jax.numpy as jnp
from jax import lax
import numpy as np

D_MODEL = 2048
BATCH = 4
SEQ = 2048
DEPTH = 4

GRID_W = 64
CTX_LEN = 256

RET_HEADS = 4
RET_DIM = 128
RET_CHUNK = 128
RET_EPS = 1e-5
GQA_Q_HEADS = 8
GQA_KV_HEADS = 2
GQA_DIM = 64
WINDOW = 128
BAND_BLOCK = 128
RWKV_HEADS = 8
RWKV_DIM = 64
DECAY_LORA = 96
ICLR_LORA = 96
GATE_LORA = 256
RWKV_EPS = 64e-5
DECAY_SCALE = math.exp(-0.5)
LRU_WIDTH = 512
LRU_BLOCKS = 8
CONV_W = 4
CONV_PAD = ((CONV_W // 2, CONV_W - 1 - CONV_W // 2),)
LRU_C = 8.0
BRANCH_W = 512
N_BRANCH = 4
RET_WIDTH = RET_HEADS * RET_DIM
GQA_Q_WIDTH = GQA_Q_HEADS * GQA_DIM
GQA_KV_WIDTH = GQA_KV_HEADS * GQA_DIM
RWKV_WIDTH = RWKV_HEADS * RWKV_DIM
IN_RET = 4 * RET_WIDTH
IN_GQA = GQA_Q_WIDTH + 2 * GQA_KV_WIDTH
IN_RWKV = 3 * RWKV_WIDTH + 2 * DECAY_LORA + ICLR_LORA + GATE_LORA
IN_LRU = 2 * LRU_WIDTH
IN_WIDTH = IN_RET + IN_GQA + IN_RWKV + IN_LRU
IN_SPLITS = (IN_RET, IN_RET + IN_GQA, IN_RET + IN_GQA + IN_RWKV)
RWKV_SPLITS = (RWKV_WIDTH, 2 * RWKV_WIDTH, 3 * RWKV_WIDTH, 3 * RWKV_WIDTH + DECAY_LORA,
               3 * RWKV_WIDTH + 2 * DECAY_LORA, 3 * RWKV_WIDTH + 2 * DECAY_LORA + ICLR_LORA)
N_GROUPS = 4
EXPERTS_PER_GROUP = 8
N_EXPERTS = N_GROUPS * EXPERTS_PER_GROUP
TOP_K = 2
EXPERT_FF = 512
ROPE_BASE = 10000.0
LN_EPS = 1e-5
DN_ALPHA = (2.0 * DEPTH) ** 0.25
DN_BETA = (8.0 * DEPTH) ** -0.25

kernel_name = "hybrid_diffusion_parallel_mixers_hmoe"


def layer_norm(x, w, b):
    xf = x.astype(jnp.float32)
    mu = jnp.mean(xf, -1, keepdims=True)
    var = jnp.mean(jnp.square(xf - mu), -1, keepdims=True)
    return ((xf - mu) * lax.rsqrt(var + LN_EPS)).astype(x.dtype) * w + b


def head_norm(y, w, b, eps):
    yf = y.astype(jnp.float32)
    mu = jnp.mean(yf, -1, keepdims=True)
    var = jnp.mean(jnp.square(yf - mu), -1, keepdims=True)
    yn = ((yf - mu) * lax.rsqrt(var + eps)).reshape(y.shape[0], y.shape[1], -1)
    return yn.astype(w.dtype) * w + b


def to_heads(t, h):
    b_, l_, _ = t.shape
    return t.reshape(b_, l_, h, -1).transpose(0, 2, 1, 3)


def from_heads(t):
    b_, h, l_, d = t.shape
    return t.transpose(0, 2, 1, 3).reshape(b_, l_, h * d)


def axial_rope(x, row, col):
    da = x.shape[-1] // 2
    inv = ROPE_BASE ** (-jnp.arange(0, da, 2, dtype=jnp.float32) / da)

    def rot(xa, pos):
        ang = pos.astype(jnp.float32)[:, None] * inv[None, :]
        cos, sin = jnp.cos(ang), jnp.sin(ang)
        x1, x2 = xa[..., : da // 2], xa[..., da // 2:]
        return jnp.concatenate([x1 * cos - x2 * sin, x2 * cos + x1 * sin], -1)

    return jnp.concatenate([rot(x[..., :da], row), rot(x[..., da:], col)], -1).astype(x.dtype)


def adaln(cvec, w, b):
    m = jax.nn.silu(cvec) @ w + b
    return jnp.split(m[:, None, :], 6, axis=-1)


def modulate(x, shift, scale):
    return x * (1.0 + scale) + shift


def retention_dir(q, k, v, log_g, s0):
    b_, h, l_, dk = q.shape
    dv = v.shape[-1]
    n = l_ // RET_CHUNK
    qc = q.reshape(b_, h, n, RET_CHUNK, dk)
    kc = k.reshape(b_, h, n, RET_CHUNK, dk)
    vc = v.reshape(b_, h, n, RET_CHUNK, dv)
    idx = jnp.arange(RET_CHUNK, dtype=jnp.float32)
    diff = idx[:, None] - idx[None, :]
    decay = jnp.where(diff >= 0, jnp.exp(log_g[:, None, None] * jnp.maximum(diff, 0.0)), 0.0)
    scores = jnp.einsum('bhnid,bhnjd->bhnij', qc, kc) * decay[None, :, None]
    y_inner = jnp.einsum('bhnij,bhnje->bhnie', scores, vc)
    k_w = kc * jnp.exp(log_g[:, None] * (RET_CHUNK - 1 - idx))[None, :, None, :, None]
    contrib = jnp.einsum('bhnjd,bhnje->nbhde', k_w, vc).astype(jnp.float32)
    chunk_decay = jnp.exp(log_g * RET_CHUNK)[None, :, None, None]

    def step(s, u):
        return chunk_decay * s + u, s

    s_final, s_prev = lax.scan(step, s0, contrib)
    q_w = qc * jnp.exp(log_g[:, None] * (idx + 1.0))[None, :, None, :, None]
    y_cross = jnp.einsum('bhnid,nbhde->bhnie', q_w, s_prev)
    y = (y_inner + y_cross).reshape(b_, h, l_, dv).astype(v.dtype)
    return y, s_final


def retention_branch(pc, pl, row, col, decay_logit, gn_w, gn_b):
    def prep(p, rope):
        q, k, v, g = jnp.split(p, 4, axis=-1)
        q, k, v = to_heads(q, RET_HEADS), to_heads(k, RET_HEADS), to_heads(v, RET_HEADS)
        if rope:
            q, k = axial_rope(q, row, col), axial_rope(k, row, col)
        return q * (RET_DIM ** -0.5), k, v, g

    log_g = jax.nn.log_sigmoid(decay_logit.astype(jnp.float32))
    qc, kc, vc, gc = prep(pc, False)
    ql, kl, vl, gl = prep(pl, True)
    s0 = jnp.zeros((pc.shape[0], RET_HEADS, RET_DIM, RET_DIM), jnp.float32)
    f = lambda t: jnp.flip(t, 2)
    yc_f, sc_f = retention_dir(qc, kc, vc, log_g[0], s0)
    yc_b, sc_b = retention_dir(f(qc), f(kc), f(vc), log_g[1], s0)
    yl_f, _ = retention_dir(ql, kl, vl, log_g[0], sc_f)
    yl_b, _ = retention_dir(f(ql), f(kl), f(vl), log_g[1], sc_b)

    def out(yf, yb, g):
        y = (yf + f(yb)).transpose(0, 2, 1, 3)
        return head_norm(y, gn_w, gn_b, RET_EPS) * jax.nn.silu(g)

    return out(yc_f, yc_b, gc), out(yl_f, yl_b, gl)


def gqa_branch(pc, pl, row, col, sink):
    grp = GQA_Q_HEADS // GQA_KV_HEADS
    scale = GQA_DIM ** -0.5

    def prep(p, rope):
        q, k, v = jnp.split(p, (GQA_Q_WIDTH, GQA_Q_WIDTH + GQA_KV_WIDTH), axis=-1)
        q, k, v = to_heads(q, GQA_Q_HEADS), to_heads(k, GQA_KV_HEADS), to_heads(v, GQA_KV_HEADS)
        if rope:
            q, k = axial_rope(q, row, col), axial_rope(k, row, col)
        b_, _, l_, d = q.shape
        return q.reshape(b_, GQA_KV_HEADS, grp, l_, d) * scale, k, v

    qc, kc, vc = prep(pc, False)
    ql, kl, vl = prep(pl, True)
    sink_l = sink.astype(jnp.float32).reshape(GQA_KV_HEADS, grp)
    lc = kc.shape[2]

    s_cc = jnp.einsum('bkgqd,bksd->bkgqs', qc, kc).astype(jnp.float32)
    sink_c = jnp.broadcast_to(sink_l[None, :, :, None, None], s_cc.shape[:-1] + (1,))
    p_c = jax.nn.softmax(jnp.concatenate([s_cc, sink_c], -1), axis=-1)
    yc = jnp.einsum('bkgqs,bksd->bkgqd', p_c[..., :lc].astype(vc.dtype), vc)
    b_, _, _, lq, d = qc.shape
    yc = from_heads(yc.reshape(b_, GQA_Q_HEADS, lq, d))

    l_ = ql.shape[3]
    t = BAND_BLOCK
    nb = l_ // t
    qb = ql.reshape(b_, GQA_KV_HEADS, grp, nb, t, d)
    pad = lambda z: jnp.pad(z.reshape(b_, GQA_KV_HEADS, nb, t, d), ((0, 0), (0, 0), (1, 1), (0, 0), (0, 0)))
    band = lambda z: jnp.concatenate([z[:, :, :-2], z[:, :, 1:-1], z[:, :, 2:]], axis=3)
    kb, vb = band(pad(kl)), band(pad(vl))
    s_band = jnp.einsum('bkgntd,bknsd->bkgnts', qb, kb).astype(jnp.float32)
    i_q = jnp.arange(t)
    j_k = jnp.arange(3 * t)
    rel = j_k[None, :] - t - i_q[:, None]
    kpos = (jnp.arange(nb)[:, None] - 1) * t + j_k[None, :]
    valid = (jnp.abs(rel) <= WINDOW)[None] & ((kpos >= 0) & (kpos < l_))[:, None, :]
    s_band = jnp.where(valid, s_band, -jnp.inf)
    s_ctx = jnp.einsum('bkgntd,bksd->bkgnts', qb, kc).astype(jnp.float32)
    sink_b = jnp.broadcast_to(sink_l[None, :, :, None, None, None], s_band.shape[:-1] + (1,))
    p_l = jax.nn.softmax(jnp.concatenate([s_ctx, s_band, sink_b], -1), axis=-1)
    yl = (jnp.einsum('bkgnts,bksd->bkgntd', p_l[..., :lc].astype(vc.dtype), vc)
          + jnp.einsum('bkgnts,bknsd->bkgntd', p_l[..., lc:lc + 3 * t].astype(vb.dtype), vb))
    yl = from_heads(yl.reshape(b_, GQA_Q_HEADS, l_, d))
    return yc, yl


def centred_shift(p):
    prev = jnp.pad(p[:, :-1], ((0, 0), (1, 0), (0, 0)))
    nxt = jnp.pad(p[:, 1:], ((0, 0), (0, 1), (0, 0)))
    return 0.5 * (prev + nxt)


def rwkv_prep(p, mu, w0, w_up, a0, a_up, g_up, k_k, k_a):
    p = p + mu * (centred_shift(p) - p)
    r, k, v, wd_f, wd_b, ad, gd = jnp.split(p, RWKV_SPLITS, axis=-1)

    def decay(wd, dr):
        z = (w0[dr] + jnp.tanh(wd) @ w_up[dr]).astype(jnp.float32)
        return jnp.exp(-DECAY_SCALE * jax.nn.sigmoid(z))

    hd = lambda z: z.reshape(z.shape[0], z.shape[1], RWKV_HEADS, RWKV_DIM)
    a = jax.nn.sigmoid(a0 + ad @ a_up)
    g = jax.nn.sigmoid(gd) @ g_up
    kk = hd(k * k_k).astype(jnp.float32)
    kk = kk / jnp.maximum(jnp.sqrt(jnp.sum(jnp.square(kk), -1, keepdims=True)), 1e-12)
    k = k * (1.0 + (a - 1.0) * k_a)
    return hd(r), hd(k), hd(v), kk, hd(a), g, hd(decay(wd_f, 0)), hd(decay(wd_b, 1))


def rwkv_scan(r, w, k, v, kk, a, s0):
    def step(s, inp):
        r_t, w_t, k_t, v_t, kk_t, a_t = inp
        sa = jnp.einsum('bhvk,bhk->bhv', s, -kk_t)
        s = s * w_t[:, :, None, :] + sa[..., None] * (kk_t * a_t)[:, :, None, :] + v_t[..., None] * k_t[:, :, None, :]
        return s, jnp.einsum('bhvk,bhk->bhv', s, r_t)

    xs = tuple(jnp.moveaxis(z, 1, 0).astype(jnp.float32) for z in (r, w, k, v, kk, a))
    s_final, ys = lax.scan(step, s0, xs)
    return jnp.moveaxis(ys, 0, 1), s_final


def rwkv_branch(pc, pl, mu, w0, w_up, a0, a_up, g_up, k_k, k_a, r_k, ln_w, ln_b):
    rc, kc, vc, kkc, ac, gc, wfc, wbc = rwkv_prep(pc, mu, w0, w_up, a0, a_up, g_up, k_k, k_a)
    rl, kl, vl, kkl, al, gl, wfl, wbl = rwkv_prep(pl, mu, w0, w_up, a0, a_up, g_up, k_k, k_a)
    s0 = jnp.zeros((pc.shape[0], RWKV_HEADS, RWKV_DIM, RWKV_DIM), jnp.float32)
    f = lambda z: jnp.flip(z, 1)
    yc_f, sc_f = rwkv_scan(rc, wfc, kc, vc, kkc, ac, s0)
    yc_b, sc_b = rwkv_scan(f(rc), f(wbc), f(kc), f(vc), f(kkc), f(ac), s0)
    yl_f, _ = rwkv_scan(rl, wfl, kl, vl, kkl, al, sc_f)
    yl_b, _ = rwkv_scan(f(rl), f(wbl), f(kl), f(vl), f(kkl), f(al), sc_b)

    def out(yf, yb, r, k, v, g):
        y = head_norm(yf + f(yb), ln_w, ln_b, RWKV_EPS)
        bonus = (jnp.sum(r * k * r_k, -1, keepdims=True) * v).reshape(y.shape)
        return (y + bonus) * g

    return out(yc_f, yc_b, rc, kc, vc, gc), out(yl_f, yl_b, rl, kl, vl, gl)


def depthwise_conv(x, w, b):
    y = lax.conv_general_dilated(x, w[:, None, :].astype(x.dtype), window_strides=(1,), padding=CONV_PAD,
                                 dimension_numbers=('NWC', 'WIO', 'NWC'), feature_group_count=x.shape[-1])
    return y + b


def _lin_combine(e1, e2):
    a1, b1 = e1
    a2, b2 = e2
    return a1 * a2, a2 * b1 + b2


def rglru_dir(x, gate_w, gate_b, lam, h0):
    b_, l_, ch = x.shape
    xb = x.reshape(b_, l_, LRU_BLOCKS, -1)
    r = jax.nn.sigmoid(jnp.einsum('blni,nij->blnj', xb, gate_w[0]).reshape(b_, l_, ch) + gate_b[0])
    i = jax.nn.sigmoid(jnp.einsum('blni,nij->blnj', xb, gate_w[1]).reshape(b_, l_, ch) + gate_b[1])
    log_a = -LRU_C * r.astype(jnp.float32) * jax.nn.softplus(-lam.astype(jnp.float32))
    a = jnp.exp(log_a)
    bt = jnp.sqrt(-jnp.expm1(2.0 * log_a)) * (i * x).astype(jnp.float32)
    a_cum, b_cum = lax.associative_scan(_lin_combine, (a, bt), axis=1)
    h = a_cum * h0[:, None, :] + b_cum
    return h, h[:, -1]


def lru_branch(pc, pl, conv_w, conv_b, gate_w, gate_b, lam):
    def prep(p):
        xb, gb = jnp.split(p, 2, axis=-1)
        return depthwise_conv(xb, conv_w, conv_b), jax.nn.gelu(gb)

    xc, gc = prep(pc)
    xl, gl = prep(pl)
    h0 = jnp.zeros((pc.shape[0], LRU_WIDTH), jnp.float32)
    f = lambda z: jnp.flip(z, 1)
    hc_f, sc_f = rglru_dir(xc, gate_w[0], gate_b[0], lam[0], h0)
    hc_b, sc_b = rglru_dir(f(xc), gate_w[1], gate_b[1], lam[1], h0)
    hl_f, _ = rglru_dir(xl, gate_w[0], gate_b[0], lam[0], sc_f)
    hl_b, _ = rglru_dir(f(xl), gate_w[1], gate_b[1], lam[1], sc_b)
    out = lambda hf, hb, g: ((hf + f(hb)) * g).astype(pc.dtype)
    return out(hc_f, hc_b, gc), out(hl_f, hl_b, gl)


def merge_branches(u, ys, w_branch, w_bgate, b_bgate, w_out):
    yb = jnp.stack(ys, axis=2)
    proj = jnp.einsum('blkc,kcd->blkd', yb, w_branch)
    gates = jax.nn.sigmoid(u @ w_bgate + b_bgate).reshape(u.shape[0], u.shape[1], N_BRANCH, -1)
    return jnp.sum(gates * proj, axis=2) @ w_out


def hier_moe(u, w_grp, b_grp, w_exp, b_exp, w1, w3, w2):
    shape = u.shape
    t = u.reshape(-1, shape[-1])
    g_prob = jax.nn.softmax((t @ w_grp + b_grp).astype(jnp.float32), axis=-1)
    g_p, g_idx = lax.top_k(g_prob, 1)
    e_logits = (t @ w_exp + b_exp).astype(jnp.float32).reshape(-1, N_GROUPS, EXPERTS_PER_GROUP)
    e_sel = jnp.take_along_axis(e_logits, g_idx[:, :, None], axis=1)[:, 0]
    e_p, e_idx = lax.top_k(jax.nn.softmax(e_sel, axis=-1), TOP_K)
    wts = g_p * e_p / jnp.sum(e_p, -1, keepdims=True)
    ids = g_idx * EXPERTS_PER_GROUP + e_idx
    combine = jnp.sum(jax.nn.one_hot(ids, N_EXPERTS, dtype=jnp.float32) * wts[..., None], axis=1)

    def expert(acc, prm):
        w1e, w3e, w2e, ce = prm
        h = jax.nn.silu(t @ w1e) * (t @ w3e)
        return acc + ce[:, None] * (h @ w2e).astype(jnp.float32), None

    acc, _ = lax.scan(expert, jnp.zeros(t.shape, jnp.float32), (w1, w3, w2, combine.T))
    return acc.astype(u.dtype).reshape(shape)


def setup_inputs(seed: int = 0) -> dict:
    key = jax.random.key(seed)
    ks = iter(jax.random.split(key, 64))
    f32 = jnp.float32
    D = D_MODEL

    def nrm(shape, scale=1.0):
        return scale * jax.random.normal(next(ks), shape, f32)

    def near(shape, center, spread):
        return center + spread * jax.random.normal(next(ks), shape, f32)

    ret_base = jnp.log(2.0 ** (5.0 + jnp.arange(RET_HEADS, dtype=f32)) - 1.0)
    a_init = jax.random.uniform(next(ks), (DEPTH, 2, LRU_WIDTH), f32, 0.9 ** (1.0 / LRU_C), 0.999 ** (1.0 / LRU_C))
    return {
        "x": nrm((BATCH, SEQ, D)),
        "c": nrm((BATCH, D)),
        "ctx": nrm((BATCH, CTX_LEN, D)),
        "c_ctx": nrm((D,)),
        "w_ada": nrm((DEPTH, D, 6 * D), 0.5 * D ** -0.5),
        "b_ada": nrm((DEPTH, 6 * D), 0.02),
        "w_in": nrm((DEPTH, D, IN_WIDTH), D ** -0.5),
        "ret_decay_logit": ret_base + nrm((DEPTH, 2, RET_HEADS), 0.1),
        "ret_gn_w": near((DEPTH, RET_WIDTH), 1.0, 0.02),
        "ret_gn_b": nrm((DEPTH, RET_WIDTH), 0.02),
        "gqa_sink": nrm((DEPTH, GQA_Q_HEADS)),
        "rwkv_mu": jax.random.uniform(next(ks), (DEPTH, IN_RWKV), f32),
        "rwkv_w0": jax.random.uniform(next(ks), (DEPTH, 2, RWKV_WIDTH), f32, -6.0, 1.0),
        "rwkv_w_up": nrm((DEPTH, 2, DECAY_LORA, RWKV_WIDTH), 0.1 * DECAY_LORA ** -0.5),
        "rwkv_a0": nrm((DEPTH, RWKV_WIDTH), 0.5),
        "rwkv_a_up": nrm((DEPTH, ICLR_LORA, RWKV_WIDTH), 0.5 * ICLR_LORA ** -0.5),
        "rwkv_g_up": nrm((DEPTH, GATE_LORA, RWKV_WIDTH), GATE_LORA ** -0.5),
        "rwkv_k_k": near((DEPTH, RWKV_WIDTH), 0.85, 0.05),
        "rwkv_k_a": near((DEPTH, RWKV_WIDTH), 1.0, 0.05),
        "rwkv_r_k": nrm((DEPTH, RWKV_HEADS, RWKV_DIM), 0.1),
        "rwkv_ln_w": near((DEPTH, RWKV_WIDTH), 1.0, 0.02),
        "rwkv_ln_b": nrm((DEPTH, RWKV_WIDTH), 0.02),
        "lru_conv_w": nrm((DEPTH, CONV_W, LRU_WIDTH), CONV_W ** -0.5),
        "lru_conv_b": nrm((DEPTH, LRU_WIDTH), 0.02),
        "lru_gate_w": nrm((DEPTH, 2, 2, LRU_BLOCKS, LRU_WIDTH // LRU_BLOCKS, LRU_WIDTH // LRU_BLOCKS), (LRU_WIDTH // LRU_BLOCKS) ** -0.5),
        "lru_gate_b": nrm((DEPTH, 2, 2, LRU_WIDTH), 0.02),
        "lru_lambda": jnp.log(a_init) - jnp.log1p(-a_init),
        "w_branch": nrm((DEPTH, N_BRANCH, BRANCH_W, D), BRANCH_W ** -0.5),
        "w_bgate": nrm((DEPTH, D, N_BRANCH * D), D ** -0.5),
        "b_bgate": nrm((DEPTH, N_BRANCH * D), 0.02),
        "w_out": nrm((DEPTH, D, D), DN_BETA * D ** -0.5),
        "ln1_w": near((DEPTH, D), 1.0, 0.02),
        "ln1_b": nrm((DEPTH, D), 0.02),
        "ln2_w": near((DEPTH, D), 1.0, 0.02),
        "ln2_b": nrm((DEPTH, D), 0.02),
        "moe_w_grp": nrm((DEPTH, D, N_GROUPS), D ** -0.5),
        "moe_b_grp": nrm((DEPTH, N_GROUPS), 0.01),
        "moe_w_exp": nrm((DEPTH, D, N_EXPERTS), D ** -0.5),
        "moe_b_exp": nrm((DEPTH, N_EXPERTS), 0.01),
        "moe_w1": nrm((DEPTH, N_EXPERTS, D, EXPERT_FF), D ** -0.5),
        "moe_w3": nrm((DEPTH, N_EXPERTS, D, EXPERT_FF), D ** -0.5),
        "moe_w2": nrm((DEPTH, N_EXPERTS, EXPERT_FF, D), DN_BETA * EXPERT_FF ** -0.5),
    }


def reference(x, c, ctx, c_ctx, w_ada, b_ada, w_in, ret_decay_logit, ret_gn_w, ret_gn_b, gqa_sink,
              rwkv_mu, rwkv_w0, rwkv_w_up, rwkv_a0, rwkv_a_up, rwkv_g_up, rwkv_k_k, rwkv_k_a, rwkv_r_k,
              rwkv_ln_w, rwkv_ln_b, lru_conv_w, lru_conv_b, lru_gate_w, lru_gate_b, lru_lambda,
              w_branch, w_bgate, b_bgate, w_out, ln1_w, ln1_b, ln2_w, ln2_b,
              moe_w_grp, moe_b_grp, moe_w_exp, moe_b_exp, moe_w1, moe_w3, moe_w2):
    n_tok = x.shape[1]
    n_rows = n_tok // GRID_W
    row = jnp.repeat(jnp.arange(n_rows), GRID_W)
    col = jnp.tile(jnp.arange(GRID_W), n_rows)
    xl, xc = x, ctx
    for i in range(DEPTH):
        update_ctx = i < DEPTH - 1
        sh1_l, sc1_l, g1_l, sh2_l, sc2_l, g2_l = adaln(c, w_ada[i], b_ada[i])
        sh1_c, sc1_c, g1_c, sh2_c, sc2_c, g2_c = adaln(c_ctx[None, :], w_ada[i], b_ada[i])

        ul = modulate(xl, sh1_l, sc1_l)
        uc = modulate(xc, sh1_c, sc1_c)
        ret_l, gqa_l, rwkv_l, lru_l = jnp.split(ul @ w_in[i], IN_SPLITS, axis=-1)
        ret_c, gqa_c, rwkv_c, lru_c = jnp.split(uc @ w_in[i], IN_SPLITS, axis=-1)
        yr_c, yr_l = retention_branch(ret_c, ret_l, row, col, ret_decay_logit[i], ret_gn_w[i], ret_gn_b[i])
        yg_c, yg_l = gqa_branch(gqa_c, gqa_l, row, col, gqa_sink[i])
        yw_c, yw_l = rwkv_branch(rwkv_c, rwkv_l, rwkv_mu[i], rwkv_w0[i], rwkv_w_up[i], rwkv_a0[i], rwkv_a_up[i],
                                 rwkv_g_up[i], rwkv_k_k[i], rwkv_k_a[i], rwkv_r_k[i], rwkv_ln_w[i], rwkv_ln_b[i])
        yd_c, yd_l = lru_branch(lru_c, lru_l, lru_conv_w[i], lru_conv_b[i], lru_gate_w[i], lru_gate_b[i], lru_lambda[i])

        yl = merge_branches(ul, (yr_l, yg_l, yw_l, yd_l), w_branch[i], w_bgate[i], b_bgate[i], w_out[i])
        xl = layer_norm(DN_ALPHA * xl + g1_l * yl, ln1_w[i], ln1_b[i])
        ml = hier_moe(modulate(xl, sh2_l, sc2_l), moe_w_grp[i], moe_b_grp[i], moe_w_exp[i], moe_b_exp[i],
                      moe_w1[i], moe_w3[i], moe_w2[i])
        xl = layer_norm(DN_ALPHA * xl + g2_l * ml, ln2_w[i], ln2_b[i])

        if update_ctx:
            yc = merge_branches(uc, (yr_c, yg_c, yw_c, yd_c), w_branch[i], w_bgate[i], b_bgate[i], w_out[i])
            xc = layer_norm(DN_ALPHA * xc + g1_c * yc, ln1_w[i], ln1_b[i])
            mc = hier_moe(modulate(xc, sh2_c, sc2_c), moe_w_grp[i], moe_b_grp[i], moe_w_exp[i], moe_b_exp[i],
                          moe_w1[i], moe_w3[i], moe_w2[i])
            xc = layer_norm(DN_ALPHA * xc + g2_c * mc, ln2_w[i], ln2_b[i])
    return xl
```

```python
import numpy as np
from contextlib import ExitStack
import concourse.bass as bass
import concourse.mybir as mybir

F32 = mybir.dt.float32
BF16 = mybir.dt.bfloat16
AF = mybir.ActivationFunctionType
ALU = mybir.AluOpType
AX = mybir.AxisListType

NDQ = 8


class R:
    __slots__ = ("w", "rd", "name")

    def __init__(self, name=""):
        self.w = None
        self.rd = {}
        self.name = name


class KB:
    def __init__(self, nc):
        self.nc = nc
        self.es = ExitStack()
        self.eng = {"pe": nc.tensor, "act": nc.scalar, "dve": nc.vector, "pool": nc.gpsimd, "sp": nc.sync}
        self.real = list(self.eng.keys())
        self.virt = ["dq%d" % i for i in range(NDQ)]
        self.all = self.real + self.virt
        self.sem = {}
        for e in self.all:
            self.sem[e] = self.es.enter_context(nc.semaphore("s_" + e))
        self.inc = {e: (1 if e in self.real else 16) for e in self.all}
        self.cnt = {e: 0 for e in self.all}
        self.seen = {e: {f: 0 for f in self.all} for e in self.real}
        self.q = {e: [] for e in self.real}
        self.dq_next = 0
        self.n_ops = 0

    def sb(self, name, shape, dtype, stack=None):
        self.uid = getattr(self, "uid", 0) + 1
        t = (stack or self.es).enter_context(self.nc.sbuf_tensor("%s_u%d" % (name, self.uid), list(shape), dtype))
        return t

    def ps(self, name, shape, dtype, stack=None):
        t = (stack or self.es).enter_context(self.nc.psum_tensor(name, list(shape), dtype))
        return t

    def _waits(self, eng, reads, writes):
        needs = {}
        for r in reads:
            if r.w is not None:
                f, n = r.w
                if n > needs.get(f, 0):
                    needs[f] = n
        for w in writes:
            if w.w is not None:
                f, n = w.w
                if f != eng and n > needs.get(f, 0):
                    needs[f] = n
            for f, n in w.rd.items():
                if f != eng and n > needs.get(f, 0):
                    needs[f] = n
        seen = self.seen[eng]
        for f, n in needs.items():
            if seen[f] >= n:
                continue
            seen[f] = n
            self.q[eng].append(("w", self.sem[f], n * self.inc[f]))

    def _commit(self, tag, reads, writes):
        self.cnt[tag] += 1
        n = self.cnt[tag]
        for r in reads:
            if n > r.rd.get(tag, 0):
                r.rd[tag] = n
        for w in writes:
            w.w = (tag, n)
            w.rd = {}
        return n

    def op(self, eng, fn, reads=(), writes=()):
        self._waits(eng, reads, writes)
        self._commit(eng, reads, writes)
        self.q[eng].append(("o", fn, self.sem[eng], 1))
        self.n_ops += 1

    def dma(self, out, in_, reads=(), writes=(), issuer="sp", **kw):
        slot = self.virt[self.dq_next]
        self.dq_next = (self.dq_next + 1) % NDQ
        seen = self.seen[issuer]
        if seen[slot] < self.cnt[slot]:
            seen[slot] = self.cnt[slot]
            self.q[issuer].append(("w", self.sem[slot], self.cnt[slot] * 16))
        self._waits(issuer, reads, writes)
        self._commit(slot, reads, writes)
        self.q[issuer].append(("o", (lambda e, o=out, i=in_, k=kw: e.dma_start(out=o, in_=i, **k)), self.sem[slot], 16))
        self.n_ops += 1

    def barrier(self):
        for e in self.real:
            seen = self.seen[e]
            for f in self.all:
                if f == e:
                    continue
                if seen[f] < self.cnt[f]:
                    seen[f] = self.cnt[f]
                    self.q[e].append(("w", self.sem[f], self.cnt[f] * self.inc[f]))

    def finish(self):
        self.barrier()
        nc = self.nc
        q = self.q

        def replay(name):
            def f(e):
                for it in q[name]:
                    if it[0] == "w":
                        e.wait_ge(it[1], it[2])
                    else:
                        ins = it[1](e)
                        ins.then_inc(it[2], it[3])
            return f

        with nc.Block() as block:
            block.tensor(replay("pe"))
            block.scalar(replay("act"))
            block.vector(replay("dve"))
            block.gpsimd(replay("pool"))
            block.sync(replay("sp"))
        self.es.close()

    def mm(self, out, lhsT, rhs, start=True, stop=True, reads=(), writes=(), **kw):
        self.op("pe", lambda e: e.matmul(out, lhsT, rhs, start=start, stop=stop, **kw), reads, writes)

    def tr(self, out, in_, ident, reads=(), writes=()):
        self.op("pe", lambda e: e.transpose(out, in_, ident), reads, writes)

    def act(self, out, in_, func, bias=None, scale=None, reads=(), writes=(), eng="act", **kw):
        k = dict(kw)
        if bias is not None:
            k["bias"] = bias
        if scale is not None:
            k["scale"] = scale
        self.op("act", lambda e: e.activation(out, in_, func, **k), reads, writes)

    def tt(self, out, a, b, op, reads=(), writes=(), eng="dve"):
        self.op(eng, lambda e: e.tensor_tensor(out, a, b, op), reads, writes)

    def ts(self, out, a, s1, s2, op0, op1=None, reads=(), writes=(), eng="dve", **kw):
        if op1 is None:
            self.op(eng, lambda e: e.tensor_scalar(out, a, s1, None, op0, **kw), reads, writes)
        else:
            self.op(eng, lambda e: e.tensor_scalar(out, a, s1, s2, op0, op1, **kw), reads, writes)

    def stt(self, out, a, s, b, op0, op1, reads=(), writes=(), eng="dve"):
        self.op(eng, lambda e: e.scalar_tensor_tensor(out, a, s, b, op0, op1), reads, writes)

    def cp(self, out, in_, reads=(), writes=(), eng="dve"):
        if eng == "act":
            self.op("act", lambda e: e.copy(out, in_), reads, writes)
        else:
            self.op(eng, lambda e: e.tensor_copy(out, in_), reads, writes)

    def memset(self, ap, val, writes=(), eng="pool"):
        self.op(eng, lambda e: e.memset(ap, val), (), writes)

import math
import numpy as np
from contextlib import ExitStack
import concourse.bass as bass
import concourse.mybir as mybir

T = 2304
LC = 256
D = 2048
KC = 16
BLKS = [(0, 256), (256, 512), (768, 512), (1280, 512), (1792, 512)]
NT = 18
DEPTH = 4
ALPHA = (2.0 * DEPTH) ** 0.25
C_RW = 64
NCH = T // C_RW

CHUNKS = {}
_o = 0
for h in range(4):
    CHUNKS["ret_q%d" % h] = [(0 + h * 128, 128)]
    CHUNKS["ret_k%d" % h] = [(512 + h * 128, 128)]
    CHUNKS["ret_g%d" % h] = [(1536 + h * 128, 128)]
for c in range(4):
    CHUNKS["gqa_q%d" % c] = [(2048 + c * 128, 128)]
for g in range(2):
    CHUNKS["gqa_k%d" % g] = [(2560 + g * 64, 64), (2560 + g * 64, 64)]
RW0 = 2816
for c in range(4):
    CHUNKS["rw_r%d" % c] = [(RW0 + c * 128, 128)]
    CHUNKS["rw_k%d" % c] = [(RW0 + 512 + c * 128, 128)]
    CHUNKS["rw_v%d" % c] = [(RW0 + 1024 + c * 128, 128)]
CHUNKS["rw_wdf"] = [(RW0 + 1536, 96)]
CHUNKS["rw_wdb"] = [(RW0 + 1632, 96)]
CHUNKS["rw_ad"] = [(RW0 + 1728, 96)]
CHUNKS["rw_gd0"] = [(RW0 + 1824, 128)]
CHUNKS["rw_gd1"] = [(RW0 + 1952, 128)]
LR0 = 4896
for c in range(4):
    CHUNKS["lru_x%d" % c] = [(LR0 + c * 128, 128)]
    CHUNKS["lru_g%d" % c] = [(LR0 + 512 + c * 128, 128)]
CH_NAMES = list(CHUNKS.keys())
CH_ID = {n: i for i, n in enumerate(CH_NAMES)}
NCHUNK = len(CH_NAMES)


def ch_width(name):
    return sum(w for _, w in CHUNKS[name])


def host_consts():
    cs = {}
    cs["ident"] = np.eye(128, dtype=np.float32)
    tok = np.arange(2048)
    row = (tok // 64).astype(np.float32)
    col = (tok % 64).astype(np.float32)

    def tables(dh):
        da = dh // 2
        inv = (10000.0 ** (-np.arange(0, da, 2, dtype=np.float32) / da)).astype(np.float32)
        nf = da // 2
        cos = np.ones((dh, T), np.float32)
        sin = np.zeros((dh, T), np.float32)
        for d in range(dh):
            first = d < da
            dd = d if first else d - da
            fi = dd % nf
            pos = row if first else col
            ang = (pos * inv[fi]).astype(np.float32)
            cos[d, LC:] = np.cos(ang)
            sin[d, LC:] = np.sin(ang)
        P = np.zeros((dh, dh), np.float32)
        for m in range(dh):
            dd = m % da
            if dd < nf:
                P[m + nf, m] = -1.0
            else:
                P[m - nf, m] = 1.0
        return cos, sin, P

    c, s, P = tables(128)
    cs["ret_cos"], cs["ret_sin"], cs["ret_P"] = c, s, P
    c, s, P = tables(64)
    cs["gqa_cos"] = np.concatenate([c, c], 0)
    cs["gqa_sin"] = np.concatenate([s, s], 0)
    P2 = np.zeros((128, 128), np.float32)
    P2[:64, :64] = P
    P2[64:, 64:] = P
    cs["gqa_P"] = P2
    j = np.arange(128)[:, None].astype(np.float32)
    i = np.arange(128)[None, :].astype(np.float32)
    sc = 128.0 ** -0.5
    ret = np.zeros((128, 6, 128), np.float32)
    ret[:, 0] = np.maximum(i - j, 0)
    ret[:, 1] = np.maximum(j - i, 0)
    ret[:, 2] = (i >= j) * sc
    ret[:, 3] = (j >= i) * sc
    ret[:, 4] = np.broadcast_to(i + 1.0, (128, 128))
    ret[:, 5] = np.broadcast_to(128.0 - i, (128, 128))
    cs["ret_tab"] = ret
    cj = np.zeros((128, 2), np.float32)
    cj[:, 0] = 127.0 - np.arange(128)
    cj[:, 1] = np.arange(128)
    cs["ret_cj"] = cj
    gm = np.zeros((128, 2, 128), np.float32)
    gm[:, 0] = (j >= i)
    gm[:, 1] = (j <= i)
    cs["gqa_mask"] = gm
    blk = (np.arange(128)[:, None] // 64) == (np.arange(128)[None, :] // 64)
    a = np.arange(128)[:, None] % 64
    b = np.arange(128)[None, :] % 64
    rm = np.zeros((128, 5, 128), np.float32)
    rm[:, 0] = blk & (a > b)
    rm[:, 1] = blk & (b > a)
    rm[:, 2] = blk & (b > a)
    rm[:, 3] = blk & (b >= a)
    rm[:, 4] = blk & (b >= a)
    cs["rw_mask"] = rm
    cs["blk_ones"] = blk.astype(np.float32)
    ist = np.zeros((128, 64), np.float32)
    ist[np.arange(128), np.arange(128) % 64] = 1.0
    cs["ist"] = ist
    return cs


CONST_SHAPES = None


W_SHAPES = {
    "w_ada": [4, 2048, 12288], "b_ada": [4, 12288], "w_in": [4, 2048, 5920],
    "ret_decay_logit": [4, 2, 4], "ret_gn_w": [4, 512], "ret_gn_b": [4, 512], "gqa_sink": [4, 8],
    "rwkv_mu": [4, 2080], "rwkv_w0": [4, 2, 512], "rwkv_w_up": [4, 2, 96, 512], "rwkv_a0": [4, 512],
    "rwkv_a_up": [4, 96, 512], "rwkv_g_up": [4, 256, 512], "rwkv_k_k": [4, 512], "rwkv_k_a": [4, 512],
    "rwkv_r_k": [4, 8, 64], "rwkv_ln_w": [4, 512], "rwkv_ln_b": [4, 512],
    "lru_conv_w": [4, 4, 512], "lru_conv_b": [4, 512], "lru_gate_w": [4, 2, 2, 8, 64, 64],
    "lru_gate_b": [4, 2, 2, 512], "lru_lambda": [4, 2, 512],
    "w_branch": [4, 4, 512, 2048], "w_bgate": [4, 2048, 8192], "b_bgate": [4, 8192], "w_out": [4, 2048, 2048],
    "ln1_w": [4, 2048], "ln1_b": [4, 2048], "ln2_w": [4, 2048], "ln2_b": [4, 2048],
    "moe_w_grp": [4, 2048, 4], "moe_b_grp": [4, 4], "moe_w_exp": [4, 2048, 32], "moe_b_exp": [4, 32],
    "moe_w1": [4, 32, 2048, 512], "moe_w3": [4, 32, 2048, 512], "moe_w2": [4, 32, 512, 2048],
}


class Ctx:
    pass


def build(nl=4, dump=(), stop=None, n_exp=32):
    nc = bass.Bass("TRN2", target_bir_lowering=False)
    g = Ctx()
    g.nc = nc
    g.nl = nl
    W = {}
    for n, s in W_SHAPES.items():
        W[n] = nc.dram_tensor(n, [nl] + list(s[1:]), F32, kind="ExternalInput").ap()
    g.W = W
    g.xin = nc.dram_tensor("xin", [T, D], F32, kind="ExternalInput").ap()
    g.c2 = nc.dram_tensor("c2", [2, D], F32, kind="ExternalInput").ap()
    cs = host_consts()
    g.C = {n: nc.dram_tensor("k_" + n, list(v.shape), F32, kind="ExternalInput").ap() for n, v in cs.items()}
    g.out = nc.dram_tensor("out", [2048, D], F32, kind="ExternalOutput").ap()

    def scratch(name, shape, dt):
        kind = "ExternalOutput" if name in dump else "Internal"
        return nc.dram_tensor(name, shape, dt, kind=kind).ap()

    g.xs_d = scratch("xs_d", [KC, 128, T], F32)
    g.p_d = scratch("p_d", [NCHUNK, 128, T], F32)
    g.vtm_d = scratch("vtm_d", [T, 640], BF16)
    g.ybr_d = scratch("ybr_d", [16, 128, T], BF16)
    g.rw_d = scratch("rw_d", [8, 4, 128, T], F32)
    g.u2_d = scratch("u2_d", [KC, 128, T], BF16)
    g.modT_d = scratch("modT_d", [128, 4 * 96 * 2], F32)
    g.cmb_d = scratch("cmb_d", [32, T], F32)

    k = KB(nc)
    g.k = k
    g.ident = k.sb("ident", [128, 128], F32)
    g.identb = k.sb("identb", [128, 128], BF16)
    g.ones = k.sb("ones", [128, 512], F32)
    g.onesb = k.sb("onesb", [128, 128], BF16)
    g.modT = k.sb("modT", [128, 4, 96, 2], F32)
    g.rconst = R("const")
    g.rmod = R("mod")
    k.dma(g.ident[:], g.C["ident"], writes=[g.rconst])
    k.cp(g.identb[:], g.ident[:], [g.rconst], [g.rconst])
    k.memset(g.ones[:], 1.0, writes=[g.rconst], eng="dve")
    k.memset(g.onesb[:], 1.0, writes=[g.rconst], eng="dve")
    g.pd = [k.ps("pd%d" % i, [128, 1024], F32) for i in range(4)]
    g.pr = [[R("ps%d_%d" % (i, h)) for h in range(2)] for i in range(4)]
    g.pi = 0

    def psb():
        i = g.pi
        g.pi = (g.pi + 1) % 8
        t = g.pd[i // 2]
        h = i % 2
        return t, h * 512, g.pr[i // 2][h]

    def psd():
        if g.pi % 2:
            g.pi = (g.pi + 1) % 8
        i = g.pi // 2
        g.pi = (g.pi + 2) % 8
        return g.pd[i], g.pr[i]

    g.psb = psb
    g.psd = psd

    prologue(g)
    if stop == "prologue":
        return finish(g)
    for l in range(nl):
        last = (l == DEPTH - 1)
        phase_inproj(g, l)
        if stop == "inproj":
            return finish(g)
        mix_ret(g, l)
        if stop == "ret":
            return finish(g)
        mix_gqa(g, l)
        if stop == "gqa":
            return finish(g)
        mix_lru(g, l)
        if stop == "lru":
            return finish(g)
        mix_rwkv(g, l)
        if stop == "rwkv":
            return finish(g)
        phase_merge(g, l, last)
        if stop == "merge":
            return finish(g)
        phase_moe(g, l, last, n_exp)
    epilogue(g)
    return finish(g)


def finish(g):
    g.k.finish()
    return g.nc


def mod(g, l, which, c, j):
    return g.modT[:, l, which * 16 + c, j:j + 1]


def prologue(g):
    k, nc, W = g.k, g.nc, g.W
    with ExitStack() as st:
        c2s = k.sb("c2s", [2, D], F32, st)
        scT = k.sb("scT", [128, 16, 2], F32, st)
        modtm = k.sb("modtm", [2, 12288], F32, st)
        bada = k.sb("bada", [2, 12288], F32, st)
        wst = [k.sb("wst%d" % i, [128, 16, 512], F32, st) for i in range(2)]
        rw = [R() for _ in range(2)]
        rc2, rscT, rmt, rba = R(), R(), R(), R()
        k.dma(c2s[:], g.c2, writes=[rc2])
        k.act(c2s[:], c2s[:], AF.Silu, reads=[rc2], writes=[rc2])
        pt, c0, rp = g.psb()
        for c in range(16):
            k.tr(pt[:, c0 + c * 2:c0 + c * 2 + 2], c2s[0:2, c * 128:(c + 1) * 128], g.ident[0:2, 0:2],
                 reads=[rc2, g.rconst], writes=[rp])
        k.cp(scT[:].rearrange("p c j -> p (c j)"), pt[:, c0:c0 + 32], [rp], [rscT])
        gi = 0
        for l in range(g.nl):
            k.dma(bada[:], W["b_ada"][l, :].partition_broadcast(2), writes=[rba])
            for grp in range(24):
                b = gi % 2
                gi += 1
                k.dma(wst[b][:], W["w_ada"][l, :, grp * 512:(grp + 1) * 512].rearrange("(c p) n -> p c n", p=128),
                      writes=[rw[b]])
                pt, c0, rp = g.psb()
                for c in range(16):
                    k.mm(pt[0:2, c0:c0 + 512], scT[:, c, :], wst[b][:, c, :], start=(c == 0), stop=(c == 15),
                         reads=[rscT, rw[b]], writes=[rp])
                k.tt(modtm[:, grp * 512:(grp + 1) * 512], pt[0:2, c0:c0 + 512], bada[:, grp * 512:(grp + 1) * 512],
                     ALU.add, reads=[rp, rba], writes=[rmt])
            pt, c0, rp = g.psb()
            for j in range(96):
                k.tr(pt[:, c0 + j * 2:c0 + j * 2 + 2], modtm[0:2, j * 128:(j + 1) * 128], g.ident[0:2, 0:2],
                     reads=[rmt, g.rconst], writes=[rp])
            k.cp(g.modT[:, l, :, :].rearrange("p c j -> p (c j)"), pt[:, c0:c0 + 192], [rp], [g.rmod])
            for which in (1, 4):
                sl = g.modT[:, l, which * 16:(which + 1) * 16, :]
                k.ts(sl, sl, 1.0, None, ALU.add, reads=[g.rmod], writes=[g.rmod])
        k.dma(g.modT_d, g.modT[:].rearrange("p l c j -> p (l c j)"), reads=[g.rmod])
        k.barrier()
    with ExitStack() as st:
        xt = [k.sb("xt%d" % i, [128, D], F32, st) for i in range(2)]
        xT = [k.sb("xT%d" % i, [128, 16, 128], F32, st) for i in range(2)]
        rxt = [R(), R()]
        rxT = [R(), R()]
        g.rxs = R("xs_d")
        for i in range(NT):
            b = i % 2
            k.dma(xt[b][:], g.xin[i * 128:(i + 1) * 128, :], writes=[rxt[b]])
            for q in range(4):
                pt, c0, rp = g.psb()
                for cc in range(4):
                    c = q * 4 + cc
                    k.tr(pt[:, c0 + cc * 128:c0 + (cc + 1) * 128], xt[b][:, c * 128:(c + 1) * 128], g.ident[:],
                         reads=[rxt[b], g.rconst], writes=[rp])
                dst = xT[b][:, q * 4:(q + 1) * 4, :].rearrange("p c t -> p (c t)")
                if q % 2 == 0:
                    k.cp(dst, pt[:, c0:c0 + 512], [rp], [rxT[b]])
                else:
                    k.cp(dst, pt[:, c0:c0 + 512], [rp], [rxT[b]], eng="act")
            k.dma(g.xs_d[:, :, i * 128:(i + 1) * 128].rearrange("c p t -> p c t"), xT[b][:], reads=[rxT[b]],
                  writes=[g.rxs])
        k.barrier()


def phase_inproj(g, l):
    k, nc, W = g.k, g.nc, g.W
    with ExitStack() as st:
        u1T = k.sb("u1T", [128, 16, T], BF16, st)
        ru1 = R("u1T")
        xb = [k.sb("xb%d" % i, [128, 16, 512], F32, st) for i in range(2)]
        rxb = [R(), R()]
        for bi, (t0, n) in enumerate(BLKS):
            j = 1 if t0 == 0 else 0
            b = bi % 2
            k.dma(xb[b][:, :, 0:n], g.xs_d[:, :, t0:t0 + n].rearrange("c p t -> p c t"), reads=[g.rxs],
                  writes=[rxb[b]])
            for c in range(16):
                k.act(u1T[:, c, t0:t0 + n], xb[b][:, c, 0:n], AF.Identity, bias=mod(g, l, 0, c, j),
                      scale=mod(g, l, 1, c, j), reads=[rxb[b], g.rmod], writes=[ru1])
        wbf = [k.sb("wbf%d" % i, [128, 16, 512], BF16, st) for i in range(2)]
        rwb = [R(), R()]
        stg = [k.sb("stg%d" % i, [128, 512], F32, st) for i in range(4)]
        rstg = [R() for _ in range(4)]
        g.rp_d = R("p_d")
        g.rvtm = R("vtm_d")
        win = W["w_in"]
        si = 0
        groups = [CH_NAMES[i:i + 4] for i in range(0, NCHUNK, 4)]
        for gi, grp in enumerate(groups):
            b = gi % 2
            for ci, name in enumerate(grp):
                off = 0
                for (c0_, w_) in CHUNKS[name]:
                    k.dma(wbf[b][:, :, ci * 128 + off:ci * 128 + off + w_],
                          win[l, :, c0_:c0_ + w_].rearrange("(c p) n -> p c n", p=128), writes=[rwb[b]], issuer="pool")
                    off += w_
            for (t0, n) in BLKS:
                for ci, name in enumerate(grp):
                    M = ch_width(name)
                    pt, c0, rp = g.psb()
                    for c in range(16):
                        k.mm(pt[0:M, c0:c0 + n], wbf[b][:, c, ci * 128:ci * 128 + M], u1T[:, c, t0:t0 + n],
                             start=(c == 0), stop=(c == 15), reads=[rwb[b], ru1], writes=[rp])
                    s = si % 4
                    si += 1
                    if si % 2:
                        k.cp(stg[s][0:M, 0:n], pt[0:M, c0:c0 + n], [rp], [rstg[s]])
                    else:
                        k.cp(stg[s][0:M, 0:n], pt[0:M, c0:c0 + n], [rp], [rstg[s]], eng="act")
                    k.dma(g.p_d[CH_ID[name], 0:M, t0:t0 + n], stg[s][0:M, 0:n], reads=[rstg[s]], writes=[g.rp_d])
        vst = [k.sb("vst%d" % i, [128, 640], BF16, st) for i in range(2)]
        rvst = [R(), R()]
        b = len(groups) % 2
        k.dma(wbf[b][:, :, 0:512], win[l, :, 1024:1536].rearrange("(c p) n -> p c n", p=128), writes=[rwb[b]],
              issuer="pool")
        b2 = 1 - b
        k.dma(wbf[b2][:, :, 0:128], win[l, :, 2688:2816].rearrange("(c p) n -> p c n", p=128), writes=[rwb[b2]],
              issuer="pool")
        for i in range(NT):
            vb = i % 2
            pt, c0, rp = g.psb()
            for c in range(16):
                k.mm(pt[:, c0:c0 + 512], u1T[:, c, i * 128:(i + 1) * 128], wbf[b][:, c, 0:512], start=(c == 0),
                     stop=(c == 15), reads=[rwb[b], ru1], writes=[rp])
            k.cp(vst[vb][:, 0:512], pt[:, c0:c0 + 512], [rp], [rvst[vb]])
            pt, c0, rp = g.psb()
            for c in range(16):
                k.mm(pt[:, c0:c0 + 128], u1T[:, c, i * 128:(i + 1) * 128], wbf[b2][:, c, 0:128], start=(c == 0),
                     stop=(c == 15), reads=[rwb[b2], ru1], writes=[rp])
            k.cp(vst[vb][:, 512:640], pt[:, c0:c0 + 128], [rp], [rvst[vb]], eng="act")
            k.dma(g.vtm_d[i * 128:(i + 1) * 128, :], vst[vb][:], reads=[rvst[vb]], writes=[g.rvtm])
        k.barrier()


def epilogue(g):
    k = g.k
    with ExitStack() as st:
        xb = [k.sb("exb%d" % i, [128, 16, 128], F32, st) for i in range(2)]
        ot = [k.sb("eot%d" % i, [128, D], F32, st) for i in range(2)]
        rxb = [R(), R()]
        rot = [R(), R()]
        for i in range(16):
            b = i % 2
            t0 = LC + i * 128
            k.dma(xb[b][:], g.xs_d[:, :, t0:t0 + 128].rearrange("c p t -> p c t"), reads=[g.rxs], writes=[rxb[b]])
            for q in range(4):
                pt, c0, rp = g.psb()
                for cc in range(4):
                    c = q * 4 + cc
                    k.tr(pt[:, c0 + cc * 128:c0 + (cc + 1) * 128], xb[b][:, c, :], g.ident[:],
                         reads=[rxb[b], g.rconst], writes=[rp])
                if q % 2 == 0:
                    k.cp(ot[b][:, q * 512:(q + 1) * 512], pt[:, c0:c0 + 512], [rp], [rot[b]])
                else:
                    k.cp(ot[b][:, q * 512:(q + 1) * 512], pt[:, c0:c0 + 512], [rp], [rot[b]], eng="act")
            k.dma(g.out[i * 128:(i + 1) * 128, :], ot[b][:], reads=[rot[b]])
        k.barrier()


SEGS = [(0, LC), (LC, T)]


def rev_ap(t, a, b, p0=0, p1=128, width=T):
    return bass.AP(t, p0 * width + (b - 1), [[width, p1 - p0], [-1, b - a]])


def small_consts(g, st):
    k = g.k
    if not hasattr(g, "_od128"):
        pass
    od = k.sb("od128", [128, 128], F32, st)
    r = R()
    k.memset(od[:], 1.0 / 128.0, writes=[r], eng="dve")
    return od, r


def rope(g, st_tiles, xT, rx, cosT, sinT, Pb, rtab, outb, rout):
    k = g.k
    xb16, rxb16, t1, rt1, t2, rt2 = st_tiles
    for bi, (t0, n) in enumerate(BLKS):
        b = bi % 2
        k.act(xb16[b][:, 0:n], xT[:, t0:t0 + n], AF.Copy, reads=[rx], writes=[rxb16[b]])
        pt, c0, rp = g.psb()
        k.mm(pt[:, c0:c0 + n], Pb[:], xb16[b][:, 0:n], reads=[rtab, rxb16[b]], writes=[rp])
        k.tt(t1[b][:, 0:n], xT[:, t0:t0 + n], cosT[:, t0:t0 + n], ALU.mult, reads=[rx, rtab], writes=[rt1[b]],
             eng="pool")
        k.tt(t2[b][:, 0:n], pt[:, c0:c0 + n], sinT[:, t0:t0 + n], ALU.mult, reads=[rp, rtab], writes=[rt2[b]])
        k.tt(outb[:, t0:t0 + n], t1[b][:, 0:n], t2[b][:, 0:n], ALU.add, reads=[rt1[b], rt2[b]], writes=[rout])


def rope_tiles(g, st, pfx):
    k = g.k
    xb16 = [k.sb(pfx + "xb16_%d" % i, [128, 512], BF16, st) for i in range(2)]
    t1 = [k.sb(pfx + "t1_%d" % i, [128, 512], F32, st) for i in range(2)]
    t2 = [k.sb(pfx + "t2_%d" % i, [128, 512], F32, st) for i in range(2)]
    return (xb16, [R(), R()], t1, [R(), R()], t2, [R(), R()])


def mix_ret(g, l):
    k, W, C = g.k, g.W, g.C
    with ExitStack() as st:
        cosT = k.sb("rcos", [128, T], F32, st)
        sinT = k.sb("rsin", [128, T], F32, st)
        Pf = k.sb("rPf", [128, 128], F32, st)
        Pb = k.sb("rPb", [128, 128], BF16, st)
        tab = k.sb("rtab", [128, 6, 128], F32, st)
        cj = k.sb("rcj", [128, 2], F32, st)
        lg = k.sb("rlg", [128, 8], F32, st)
        gnw = k.sb("rgnw", [128, 4], F32, st)
        gnb = k.sb("rgnb", [128, 4], F32, st)
        epsc = k.sb("repsc", [128, 1], F32, st)
        rtab = R()
        k.dma(cosT[:], C["ret_cos"], writes=[rtab])
        k.dma(sinT[:], C["ret_sin"], writes=[rtab])
        k.dma(Pf[:], C["ret_P"], writes=[rtab])
        k.dma(tab[:], C["ret_tab"], writes=[rtab])
        k.dma(cj[:], C["ret_cj"], writes=[rtab])
        k.dma(lg[:], W["ret_decay_logit"][l].rearrange("a b -> (a b)").partition_broadcast(128), writes=[rtab])
        k.dma(gnw[:], W["ret_gn_w"][l].rearrange("(h p) -> p h", p=128), writes=[rtab], allow_slow_non_contiguous=True)
        k.dma(gnb[:], W["ret_gn_b"][l].rearrange("(h p) -> p h", p=128), writes=[rtab], allow_slow_non_contiguous=True)
        k.cp(Pb[:], Pf[:], [rtab], [rtab])
        k.memset(epsc[:], 1e-5, writes=[rtab], eng="dve")
        k.act(lg[:], lg[:], AF.Exp, scale=-1.0, reads=[rtab], writes=[rtab])
        k.ts(lg[:], lg[:], 1.0, None, ALU.add, reads=[rtab], writes=[rtab])
        k.act(lg[:], lg[:], AF.Ln, reads=[rtab], writes=[rtab])
        k.ts(lg[:], lg[:], -1.0, None, ALU.mult, reads=[rtab], writes=[rtab])
        od, rod = small_consts(g, st)
        rt = rope_tiles(g, st, "r")
        qT = k.sb("rqT", [128, T], F32, st)
        kT = k.sb("rkT", [128, T], F32, st)
        gT = k.sb("rgT", [128, T], F32, st)
        qTr = k.sb("rqTr", [128, T], BF16, st)
        kTr = k.sb("rkTr", [128, T], BF16, st)
        sg = k.sb("rsg", [128, T], BF16, st)
        ktm = k.sb("rktm", [128, NT, 128], BF16, st)
        vtm = k.sb("rvtm", [128, NT, 128], BF16, st)
        Sall = [k.sb("rSall%d" % d, [128, NT, 128], BF16, st) for d in range(2)]
        S = k.sb("rS", [128, 128], F32, st)
        DT = k.sb("rDT", [128, 128], F32, st)
        tmpa = k.sb("rtmpa", [128, 128], F32, st)
        Gd = [k.sb("rG%d" % d, [128, 128], BF16, st) for d in range(2)]
        Gt = k.sb("rGt", [128, 128], F32, st)
        cdir = k.sb("rcdir", [128, 4], F32, st)
        kw = [k.sb("rkw%d" % i, [128, 128], BF16, st) for i in range(2)]
        Pm = [k.sb("rPm%d" % i, [128, 128], BF16, st) for i in range(2)]
        qw = [[k.sb("rqw%d_%d" % (d, i), [128, 128], BF16, st) for i in range(2)] for d in range(2)]
        y = k.sb("ry", [128, T], F32, st)
        hn = [k.sb("rhn%d" % i, [128, 512], F32, st) for i in range(4)]
        yo = [k.sb("ryo%d" % i, [128, 512], BF16, st) for i in range(2)]
        rq, rk, rg_, rqr, rkr, rsg, rktm, rvtm = [R() for _ in range(8)]
        rSall = [R(), R()]
        rS, rDT, rtmpa, rGt, rcd, ry = [R() for _ in range(6)]
        rG = [R(), R()]
        rkw = [R(), R()]
        rPm = [R(), R()]
        rqw = [[R(), R()], [R(), R()]]
        rhn = [R() for _ in range(4)]
        ryo = [R(), R()]
        g.rybr = getattr(g, "rybr", None) or R("ybr")
        for h in range(4):
            k.dma(qT[:], g.p_d[CH_ID["ret_q%d" % h]], reads=[g.rp_d], writes=[rq])
            k.dma(kT[:], g.p_d[CH_ID["ret_k%d" % h]], reads=[g.rp_d], writes=[rk])
            k.dma(gT[:], g.p_d[CH_ID["ret_g%d" % h]], reads=[g.rp_d], writes=[rg_])
            k.dma(vtm[:], g.vtm_d[:, h * 128:(h + 1) * 128].rearrange("(i p) e -> p i e", p=128), reads=[g.rvtm],
                  writes=[rvtm])
            rope(g, rt, qT, rq, cosT, sinT, Pb, rtab, qTr, rqr)
            rope(g, rt, kT, rk, cosT, sinT, Pb, rtab, kTr, rkr)
            k.act(sg[:], gT[:], AF.Silu, reads=[rg_], writes=[rsg])
            for i in range(NT):
                pt, c0, rp = g.psb()
                ptb = pt.bitcast(BF16)
                k.tr(ptb[:, 2 * c0:2 * c0 + 128], kTr[:, i * 128:(i + 1) * 128], g.identb[:], reads=[rkr, g.rconst],
                     writes=[rp])
                if i % 2:
                    k.cp(ktm[:, i, :], ptb[:, 2 * c0:2 * c0 + 128], [rp], [rktm])
                else:
                    k.cp(ktm[:, i, :], ptb[:, 2 * c0:2 * c0 + 128], [rp], [rktm], eng="act")
            lgf = lg[:, h:h + 1]
            lgb = lg[:, 4 + h:5 + h]
            k.act(DT[:], tab[:, 0, :], AF.Exp, scale=lgf, reads=[rtab], writes=[rDT])
            k.tt(DT[:], DT[:], tab[:, 2, :], ALU.mult, reads=[rDT, rtab], writes=[rDT])
            k.act(tmpa[:], tab[:, 1, :], AF.Exp, scale=lgb, reads=[rtab], writes=[rtmpa])
            k.tt(tmpa[:], tmpa[:], tab[:, 3, :], ALU.mult, reads=[rtmpa, rtab], writes=[rtmpa])
            k.tt(DT[:], DT[:], tmpa[:], ALU.add, reads=[rDT, rtmpa], writes=[rDT])
            for d in range(2):
                k.act(Gt[:], tab[:, 4 + d, :], AF.Exp, scale=(lgf if d == 0 else lgb), reads=[rtab], writes=[rGt])
                k.ts(Gd[d][:], Gt[:], 128.0 ** -0.5, None, ALU.mult, reads=[rGt], writes=[rG[d]])
                k.act(cdir[:, d:d + 1], cj[:, d:d + 1], AF.Exp, scale=(lgf if d == 0 else lgb), reads=[rtab],
                      writes=[rcd])
                k.act(cdir[:, 2 + d:3 + d], (lgf if d == 0 else lgb), AF.Exp, scale=128.0, reads=[rtab],
                      writes=[rcd])
            orders = [list(range(NT)), [1, 0] + list(range(NT - 1, 1, -1))]
            ci = 0
            for d in range(2):
                k.memset(S[:], 0.0, writes=[rS], eng="dve")
                order = orders[d]
                for oi, n in enumerate(order):
                    k.cp(Sall[d][:, n, :], S[:], [rS], [rSall[d]], eng="act")
                    if oi == len(order) - 1:
                        break
                    b = ci % 2
                    ci += 1
                    k.ts(kw[b][:], ktm[:, n, :], cdir[:, d:d + 1], None, ALU.mult, reads=[rktm, rcd],
                         writes=[rkw[b]], eng="pool")
                    pt, c0, rp = g.psb()
                    k.mm(pt[:, c0:c0 + 128], kw[b][:], vtm[:, n, :], reads=[rkw[b], rvtm], writes=[rp])
                    k.stt(S[:], S[:], cdir[:, 2 + d:3 + d], pt[:, c0:c0 + 128], ALU.mult, ALU.add,
                          reads=[rS, rcd, rp], writes=[rS])
            pt = None
            for n in range(NT):
                b = n % 2
                ts_ = slice(n * 128, (n + 1) * 128)
                ps_, cs_, rps = g.psb()
                k.mm(ps_[:, cs_:cs_ + 128], kTr[:, ts_], qTr[:, ts_], reads=[rkr, rqr], writes=[rps])
                k.tt(Pm[b][:], ps_[:, cs_:cs_ + 128], DT[:], ALU.mult, reads=[rps, rDT], writes=[rPm[b]])
                for d in range(2):
                    k.tt(qw[d][b][:], qTr[:, ts_], Gd[d][:], ALU.mult, reads=[rqr, rG[d]], writes=[rqw[d][b]],
                         eng="pool")
                if n % 4 == 0:
                    pt, c0, rp = g.psb()
                o = c0 + (n % 4) * 128
                k.mm(pt[:, o:o + 128], vtm[:, n, :], Pm[b][:], start=True, stop=False, reads=[rvtm, rPm[b]],
                     writes=[rp])
                k.mm(pt[:, o:o + 128], Sall[0][:, n, :], qw[0][b][:], start=False, stop=False,
                     reads=[rSall[0], rqw[0][b]], writes=[rp])
                k.mm(pt[:, o:o + 128], Sall[1][:, n, :], qw[1][b][:], start=False, stop=True,
                     reads=[rSall[1], rqw[1][b]], writes=[rp])
                if n % 4 == 3 or n == NT - 1:
                    n0 = (n // 4) * 4
                    w_ = (n - n0 + 1) * 128
                    k.cp(y[:, n0 * 128:n0 * 128 + w_], pt[:, c0:c0 + w_], [rp], [ry], eng="act")
            for bi, (t0, n) in enumerate(BLKS):
                b = bi % 2
                sl = slice(t0, t0 + n)
                p1, c1, rp1 = g.psb()
                k.mm(p1[:, c1:c1 + n], od[:], y[:, sl], reads=[rod, ry], writes=[rp1])
                k.act(hn[0][:, 0:n], y[:, sl], AF.Square, reads=[ry], writes=[rhn[0]])
                p2, c2, rp2 = g.psb()
                k.mm(p2[:, c2:c2 + n], od[:], hn[0][:, 0:n], reads=[rod, rhn[0]], writes=[rp2])
                k.cp(hn[1][:, 0:n], p1[:, c1:c1 + n], [rp1], [rhn[1]], eng="act")
                k.tt(hn[2][:, 0:n], hn[1][:, 0:n], hn[1][:, 0:n], ALU.mult, reads=[rhn[1]], writes=[rhn[2]],
                     eng="pool")
                k.tt(hn[2][:, 0:n], p2[:, c2:c2 + n], hn[2][:, 0:n], ALU.subtract, reads=[rp2, rhn[2]],
                     writes=[rhn[2]])
                k.act(hn[2][:, 0:n], hn[2][:, 0:n], AF.Sqrt, bias=epsc[:, 0:1], scale=1.0, reads=[rhn[2], rtab],
                      writes=[rhn[2]])
                k.op("dve", lambda e, o_=hn[2][:, 0:n]: e.reciprocal(o_, o_), [rhn[2]], [rhn[2]])
                k.tt(hn[3][:, 0:n], y[:, sl], hn[1][:, 0:n], ALU.subtract, reads=[ry, rhn[1]], writes=[rhn[3]])
                k.tt(hn[3][:, 0:n], hn[3][:, 0:n], hn[2][:, 0:n], ALU.mult, reads=[rhn[3], rhn[2]],
                     writes=[rhn[3]])
                k.act(hn[3][:, 0:n], hn[3][:, 0:n], AF.Identity, bias=gnb[:, h:h + 1], scale=gnw[:, h:h + 1],
                      reads=[rhn[3], rtab], writes=[rhn[3]])
                k.tt(yo[b][:, 0:n], hn[3][:, 0:n], sg[:, sl], ALU.mult, reads=[rhn[3], rsg], writes=[ryo[b]])
                k.dma(g.ybr_d[0 + h, :, sl], yo[b][:, 0:n], reads=[ryo[b]], writes=[g.rybr])
        k.barrier()


def mix_gqa(g, l):
    k, W, C = g.k, g.W, g.C
    with ExitStack() as st:
        cosT = k.sb("gcos", [128, T], F32, st)
        sinT = k.sb("gsin", [128, T], F32, st)
        Pf = k.sb("gPf", [128, 128], F32, st)
        Pb = k.sb("gPb", [128, 128], BF16, st)
        mkf = k.sb("gmkf", [128, 2, 128], F32, st)
        mk = k.sb("gmk", [128, 2, 128], BF16, st)
        esk = k.sb("gesk", [128, 8], F32, st)
        rtab = R()
        k.dma(cosT[:], C["gqa_cos"], writes=[rtab])
        k.dma(sinT[:], C["gqa_sin"], writes=[rtab])
        k.dma(Pf[:], C["gqa_P"], writes=[rtab])
        k.dma(mkf[:], C["gqa_mask"], writes=[rtab])
        k.dma(esk[:], W["gqa_sink"][l].partition_broadcast(128), writes=[rtab])
        k.cp(Pb[:], Pf[:], [rtab], [rtab])
        k.cp(mk[:], mkf[:], [rtab], [rtab])
        k.act(esk[:], esk[:], AF.Exp, reads=[rtab], writes=[rtab])
        rt = rope_tiles(g, st, "g")
        xT = [k.sb("gxT%d" % i, [128, T], F32, st) for i in range(2)]
        rx = [R(), R()]
        qTr = [k.sb("gqTr%d" % c, [128, T], BF16, st) for c in range(4)]
        rqr = [R() for _ in range(4)]
        K2T = [k.sb("gK2T%d" % c, [128, T], BF16, st) for c in range(2)]
        rk2 = [R(), R()]
        V2 = [k.sb("gV2%d" % c, [128, NT, 128], BF16, st) for c in range(2)]
        rv2 = [R(), R()]
        yg = [k.sb("gyg%d" % c, [128, T], BF16, st) for c in range(4)]
        ryg = [R() for _ in range(4)]
        E = [k.sb("gE%d" % i, [128, 640], BF16, st) for i in range(3)]
        rE = [R() for _ in range(3)]
        rd = [k.sb("grd%d" % i, [128, 128], F32, st) for i in range(2)]
        rrd = [R(), R()]
        xi = 0
        for c in range(4):
            b = xi % 2
            xi += 1
            k.dma(xT[b][:], g.p_d[CH_ID["gqa_q%d" % c]], reads=[g.rp_d], writes=[rx[b]])
            rope(g, rt, xT[b], rx[b], cosT, sinT, Pb, rtab, qTr[c], rqr[c])
        for c in range(2):
            b = xi % 2
            xi += 1
            k.dma(xT[b][:], g.p_d[CH_ID["gqa_k%d" % c]], reads=[g.rp_d], writes=[rx[b]])
            rope(g, rt, xT[b], rx[b], cosT, sinT, Pb, rtab, K2T[c], rk2[c])
            for hh in range(2):
                k.dma(V2[c][:, :, hh * 64:(hh + 1) * 64],
                      g.vtm_d[:, 512 + c * 64:512 + (c + 1) * 64].rearrange("(i p) e -> p i e", p=128),
                      reads=[g.rvtm], writes=[rv2[c]])
        it = 0
        for h in range(8):
            gk = h // 4
            c = h // 2
            hp = h % 2
            prt = slice(hp * 64, hp * 64 + 64)
            for qt in range(NT):
                if qt < 2:
                    keys = [(0, None), (1, None)]
                else:
                    keys = [(0, None), (1, None)]
                    for s in (qt - 1, qt, qt + 1):
                        if 2 <= s <= NT - 1:
                            keys.append((s, s - qt))
                nk = len(keys)
                qs = slice(qt * 128, (qt + 1) * 128)
                pd2, rpd = g.psd()
                for idx, (s, rel_) in enumerate(keys):
                    k.mm(pd2[:, idx * 128:(idx + 1) * 128], K2T[gk][prt, s * 128:(s + 1) * 128], qTr[c][prt, qs],
                         reads=[rk2[gk], rqr[c]], writes=[rpd[idx // 4]])
                e = it % 3
                it += 1
                k.act(E[e][:, 0:nk * 128], pd2[:, 0:nk * 128], AF.Exp, scale=0.125, reads=rpd, writes=[rE[e]])
                for idx, (s, rel_) in enumerate(keys):
                    if rel_ == -1 or rel_ == 1:
                        mi = 0 if rel_ == -1 else 1
                        k.tt(E[e][:, idx * 128:(idx + 1) * 128], E[e][:, idx * 128:(idx + 1) * 128], mk[:, mi, :],
                             ALU.mult, reads=[rE[e], rtab], writes=[rE[e]], eng="pool")
                pt, c0, rp = g.psb()
                for idx, (s, rel_) in enumerate(keys):
                    k.mm(pt[:, c0:c0 + 128], V2[gk][:, s, :], E[e][:, idx * 128:(idx + 1) * 128], start=(idx == 0),
                         stop=(idx == nk - 1), reads=[rv2[gk], rE[e]], writes=[rp])
                for idx, (s, rel_) in enumerate(keys):
                    k.mm(pt[:, c0 + 128:c0 + 256], g.onesb[:], E[e][:, idx * 128:(idx + 1) * 128],
                         start=(idx == 0), stop=(idx == nk - 1), reads=[g.rconst, rE[e]], writes=[rp])
                b = it % 2
                k.ts(rd[b][:], pt[:, c0 + 128:c0 + 256], esk[:, h:h + 1], None, ALU.add, reads=[rp, rtab],
                     writes=[rrd[b]])
                k.op("dve", lambda e_, o_=rd[b][:]: e_.reciprocal(o_, o_), [rrd[b]], [rrd[b]])
                k.tt(yg[c][prt, qs], pt[prt, c0:c0 + 128], rd[b][prt, :], ALU.mult, reads=[rp, rrd[b]],
                     writes=[ryg[c]])
        g.rybr = getattr(g, "rybr", None) or R("ybr")
        for c in range(4):
            k.dma(g.ybr_d[4 + c], yg[c][:], reads=[ryg[c]], writes=[g.rybr])
        k.barrier()


def mix_lru(g, l):
    k, W, C = g.k, g.W, g.C
    with ExitStack() as st:
        cw = k.sb("lcw", [128, 4, 4], F32, st)
        cb = k.sb("lcb", [128, 4], F32, st)
        gb = k.sb("lgb", [128, 2, 2, 4], F32, st)
        lm = k.sb("llm", [128, 2, 4], F32, st)
        sp16 = k.sb("lsp16", [128, 2, 4], F32, st)
        onec = k.sb("lonec", [128, 1], F32, st)
        bd = k.sb("lbd", [128, 16, 128], F32, st)
        bdb = k.sb("lbdb", [128, 16, 128], BF16, st)
        rc = R()
        rbd = R()
        for j_ in range(4):
            k.dma(cw[:, :, j_], W["lru_conv_w"][l, j_].rearrange("(c p) -> p c", p=128), writes=[rc],
                  allow_slow_non_contiguous=True)
        k.dma(cb[:], W["lru_conv_b"][l].rearrange("(c p) -> p c", p=128), writes=[rc],
              allow_slow_non_contiguous=True)
        for a_ in range(2):
            for b_ in range(2):
                k.dma(gb[:, a_, b_, :], W["lru_gate_b"][l, a_, b_].rearrange("(c p) -> p c", p=128), writes=[rc],
                      allow_slow_non_contiguous=True)
            k.dma(lm[:, a_, :], W["lru_lambda"][l, a_].rearrange("(c p) -> p c", p=128), writes=[rc],
                  allow_slow_non_contiguous=True)
        k.memset(onec[:], 1.0, writes=[rc], eng="dve")
        k.act(lm[:], lm[:], AF.Exp, scale=-1.0, reads=[rc], writes=[rc])
        k.ts(lm[:], lm[:], 1.0, None, ALU.add, reads=[rc], writes=[rc])
        k.act(lm[:], lm[:], AF.Ln, reads=[rc], writes=[rc])
        k.ts(sp16[:], lm[:], -16.0, None, ALU.mult, reads=[rc], writes=[rc])
        k.ts(lm[:], lm[:], -8.0, None, ALU.mult, reads=[rc], writes=[rc])
        k.memset(bd[:], 0.0, writes=[rbd], eng="dve")
        for d in range(2):
            for gt_ in range(2):
                for c in range(4):
                    idx = (d * 2 + gt_) * 4 + c
                    for hh in range(2):
                        k.dma(bd[hh * 64:(hh + 1) * 64, idx, hh * 64:(hh + 1) * 64],
                              W["lru_gate_w"][l, d, gt_, 2 * c + hh], writes=[rbd])
        k.cp(bdb[:], bd[:], [rbd], [rbd])
        x = k.sb("lx", [128, T], F32, st)
        gt = k.sb("lgt", [128, T], F32, st)
        xc = k.sb("lxc", [128, T], F32, st)
        xcb = k.sb("lxcb", [128, T], BF16, st)
        rg = k.sb("lrg", [128, T], F32, st)
        ig = k.sb("lig", [128, T], F32, st)
        aa = k.sb("laa", [128, T], F32, st)
        bt = k.sb("lbt", [128, T], F32, st)
        hh_ = [k.sb("lh%d" % d, [128, T], F32, st) for d in range(2)]
        yo = k.sb("lyo", [128, T], BF16, st)
        rx, rgt, rxc, rxcb, rrg, rig, raa, rbt, ryo = [R() for _ in range(9)]
        rh = [R(), R()]
        g.rybr = getattr(g, "rybr", None) or R("ybr")
        for c in range(4):
            k.dma(x[:], g.p_d[CH_ID["lru_x%d" % c]], reads=[g.rp_d], writes=[rx])
            k.dma(gt[:], g.p_d[CH_ID["lru_g%d" % c]], reads=[g.rp_d], writes=[rgt])
            for (a, b) in SEGS:
                k.ts(xc[:, a:b], x[:, a:b], cw[:, c, 2:3], cb[:, c:c + 1], ALU.mult, ALU.add, reads=[rx, rc],
                     writes=[rxc])
                k.stt(xc[:, a + 2:b], x[:, a:b - 2], cw[:, c, 0:1], xc[:, a + 2:b], ALU.mult, ALU.add,
                      reads=[rx, rc, rxc], writes=[rxc])
                k.stt(xc[:, a + 1:b], x[:, a:b - 1], cw[:, c, 1:2], xc[:, a + 1:b], ALU.mult, ALU.add,
                      reads=[rx, rc, rxc], writes=[rxc])
                k.stt(xc[:, a:b - 1], x[:, a + 1:b], cw[:, c, 3:4], xc[:, a:b - 1], ALU.mult, ALU.add,
                      reads=[rx, rc, rxc], writes=[rxc])
            k.act(xcb[:], xc[:], AF.Copy, reads=[rxc], writes=[rxcb])
            for d in range(2):
                for (t0, n) in BLKS:
                    sl = slice(t0, t0 + n)
                    p1, c1, rp1 = g.psb()
                    k.mm(p1[:, c1:c1 + n], bdb[:, (d * 2 + 0) * 4 + c, :], xcb[:, sl], reads=[rbd, rxcb],
                         writes=[rp1])
                    k.act(rg[:, sl], p1[:, c1:c1 + n], AF.Sigmoid, bias=gb[:, d, 0, c:c + 1], scale=1.0,
                          reads=[rp1, rc], writes=[rrg])
                    p2, c2, rp2 = g.psb()
                    k.mm(p2[:, c2:c2 + n], bdb[:, (d * 2 + 1) * 4 + c, :], xcb[:, sl], reads=[rbd, rxcb],
                         writes=[rp2])
                    k.act(ig[:, sl], p2[:, c2:c2 + n], AF.Sigmoid, bias=gb[:, d, 1, c:c + 1], scale=1.0,
                          reads=[rp2, rc], writes=[rig])
                k.act(aa[:], rg[:], AF.Exp, scale=lm[:, d, c:c + 1], reads=[rrg, rc], writes=[raa])
                k.act(bt[:], rg[:], AF.Exp, scale=sp16[:, d, c:c + 1], reads=[rrg, rc], writes=[rbt])
                k.act(bt[:], bt[:], AF.Sqrt, bias=onec[:, 0:1], scale=-1.0, reads=[rbt, rc], writes=[rbt])
                k.tt(ig[:], ig[:], xc[:], ALU.mult, reads=[rig, rxc], writes=[rig], eng="pool")
                k.tt(bt[:], bt[:], ig[:], ALU.mult, reads=[rbt, rig], writes=[rbt])
                h_ = hh_[d]
                if d == 0:
                    k.op("dve", lambda e, o_=h_[:], a_=aa[:], b_=bt[:]: e.tensor_tensor_scan(o_, a_, b_, 0.0, ALU.mult, ALU.add),
                         [raa, rbt], [rh[d]])
                else:
                    k.op("dve", lambda e, o_=rev_ap(h_, 0, LC), a_=rev_ap(aa, 0, LC), b_=rev_ap(bt, 0, LC):
                         e.tensor_tensor_scan(o_, a_, b_, 0.0, ALU.mult, ALU.add), [raa, rbt], [rh[d]])
                    k.op("dve", lambda e, o_=rev_ap(h_, LC, T), a_=rev_ap(aa, LC, T), b_=rev_ap(bt, LC, T), i_=h_[:, 0:1]:
                         e.tensor_tensor_scan(o_, a_, b_, i_, ALU.mult, ALU.add), [raa, rbt, rh[d]], [rh[d]])
            k.act(gt[:], gt[:], AF.Gelu, reads=[rgt], writes=[rgt])
            k.tt(hh_[0][:], hh_[0][:], hh_[1][:], ALU.add, reads=[rh[0], rh[1]], writes=[rh[0]], eng="pool")
            k.tt(yo[:], hh_[0][:], gt[:], ALU.mult, reads=[rh[0], rgt], writes=[ryo])
            k.dma(g.ybr_d[12 + c], yo[:], reads=[ryo], writes=[g.rybr])
        k.barrier()


DECAY_SCALE = math.exp(-0.5)
RW_Q = ["r", "k", "kk", "b", "v", "lwf", "lwb", "g"]
MU_COLS = [(c * 128, 128) for c in range(12)] + [(1536, 96), (1632, 96), (1728, 96), (1824, 128), (1952, 128)]


def shift_mix(g, x, rx, tmp, rtmp, out, rout, M, om, hm, rmu):
    k = g.k
    for (a, b) in SEGS:
        k.cp(tmp[0:M, a:b - 1], x[0:M, a + 1:b], [rx], [rtmp], eng="pool")
        k.memset(tmp[0:M, b - 1:b], 0.0, writes=[rtmp], eng="pool")
        k.tt(tmp[0:M, a + 1:b], tmp[0:M, a + 1:b], x[0:M, a:b - 1], ALU.add, reads=[rtmp, rx], writes=[rtmp])
    k.ts(out[0:M, :], x[0:M, :], om, None, ALU.mult, reads=[rx, rmu], writes=[rout])
    k.stt(out[0:M, :], tmp[0:M, :], hm, out[0:M, :], ALU.mult, ALU.add, reads=[rtmp, rmu, rout], writes=[rout])


def dv(arr, d, seg, p0=0, p1=128):
    a, b = SEGS[seg]
    nch = (b - a) // C_RW
    if d == 0:
        return arr[p0:p1, a:b].rearrange("p (n c) -> p n c", c=C_RW)
    return bass.AP(arr, p0 * T + (b - 1), [[T, p1 - p0], [-C_RW, nch], [-1, C_RW]])


def chs(seg):
    a, b = SEGS[seg]
    return slice(a // C_RW, b // C_RW)


def mix_rwkv(g, l):
    rwkv_prep(g, l)
    rwkv_scan(g, l)


def rwkv_prep(g, l):
    k, W, C = g.k, g.W, g.C
    with ExitStack() as st:
        mu = k.sb("wmu", [128, 17], F32, st)
        om = k.sb("wom", [128, 17], F32, st)
        hm = k.sb("whm", [128, 17], F32, st)
        rmu = R()
        k.memset(mu[:], 0.0, writes=[rmu], eng="dve")
        for i, (c0, w) in enumerate(MU_COLS):
            k.dma(mu[0:w, i:i + 1], W["rwkv_mu"][l, c0:c0 + w].rearrange("(p o) -> p o", o=1), writes=[rmu])
        k.ts(om[:], mu[:], -1.0, 1.0, ALU.mult, ALU.add, reads=[rmu], writes=[rmu])
        k.ts(hm[:], mu[:], 0.5, None, ALU.mult, reads=[rmu], writes=[rmu])
        par = k.sb("wpar", [128, 8, 4], F32, st)
        rpar = R()
        srcs = [W["rwkv_w0"][l, 0], W["rwkv_w0"][l, 1], W["rwkv_a0"][l], W["rwkv_k_k"][l], W["rwkv_k_a"][l]]
        for i, s in enumerate(srcs):
            k.dma(par[:, i, :], s.rearrange("(c p) -> p c", p=128), writes=[rpar], allow_slow_non_contiguous=True)
        k.ts(par[:, 5, :], par[:, 4, :], -1.0, 1.0, ALU.mult, ALU.add, reads=[rpar], writes=[rpar])
        wup = k.sb("wwup", [96, 2, 512], BF16, st)
        aup = k.sb("waup", [96, 512], BF16, st)
        gup = k.sb("wgup", [128, 2, 512], BF16, st)
        bo = k.sb("wbo", [128, 128], F32, st)
        rw = R()
        for d in range(2):
            k.dma(wup[:, d, :], W["rwkv_w_up"][l, d], writes=[rw], issuer="pool")
        k.dma(aup[:], W["rwkv_a_up"][l], writes=[rw], issuer="pool")
        k.dma(gup[:], W["rwkv_g_up"][l].rearrange("(c p) n -> p c n", p=128), writes=[rw], issuer="pool")
        k.dma(bo[:], C["blk_ones"], writes=[rw])
        x = k.sb("wx", [128, T], F32, st)
        tmp = k.sb("wtmp", [128, T], F32, st)
        xs_ = k.sb("wxs", [128, T], F32, st)
        rx, rtmp, rxs = R(), R(), R()
        twd = [k.sb("wtwd%d" % d, [96, T], BF16, st) for d in range(2)]
        adb = k.sb("wadb", [96, T], BF16, st)
        sgd = k.sb("wsgd", [128, 2, T], BF16, st)
        rlo = R()
        for i, (name, M) in enumerate([("rw_wdf", 96), ("rw_wdb", 96), ("rw_ad", 96), ("rw_gd0", 128), ("rw_gd1", 128)]):
            mi = 12 + i
            k.dma(x[0:M, :], g.p_d[CH_ID[name], 0:M, :], reads=[g.rp_d], writes=[rx])
            shift_mix(g, x, rx, tmp, rtmp, xs_, rxs, M, om[0:M, mi:mi + 1], hm[0:M, mi:mi + 1], rmu)
            if i < 2:
                k.act(twd[i][:], xs_[0:96, :], AF.Tanh, reads=[rxs], writes=[rlo])
            elif i == 2:
                k.act(adb[:], xs_[0:96, :], AF.Copy, reads=[rxs], writes=[rlo])
            else:
                k.act(sgd[:, i - 3, :], xs_[:], AF.Sigmoid, reads=[rxs], writes=[rlo])
        names = ["r", "k", "v", "lwf", "lwb", "a", "gq", "kk", "t"]
        A = {n: k.sb("wA_" + n, [128, T], F32, st) for n in names}
        RA = {n: R() for n in names}
        g.rrw = getattr(g, "rrw", None) or R("rw_d")
        for c in range(4):
            for qi, (nm, pfx) in enumerate([("r", "rw_r"), ("k", "rw_k"), ("v", "rw_v")]):
                mi = qi * 4 + c
                k.dma(x[:], g.p_d[CH_ID["%s%d" % (pfx, c)]], reads=[g.rp_d], writes=[rx])
                shift_mix(g, x, rx, tmp, rtmp, A[nm], RA[nm], 128, om[:, mi:mi + 1], hm[:, mi:mi + 1], rmu)
            cs_ = slice(c * 128, (c + 1) * 128)
            for (t0, n) in BLKS:
                sl = slice(t0, t0 + n)
                for d in range(2):
                    pt, c0, rp = g.psb()
                    k.mm(pt[:, c0:c0 + n], wup[:, d, cs_], twd[d][:, sl], reads=[rw, rlo], writes=[rp])
                    nm = "lwf" if d == 0 else "lwb"
                    k.act(A[nm][:, sl], pt[:, c0:c0 + n], AF.Sigmoid, bias=par[:, d, c:c + 1], scale=1.0,
                          reads=[rp, rpar], writes=[RA[nm]])
                pt, c0, rp = g.psb()
                k.mm(pt[:, c0:c0 + n], aup[:, cs_], adb[:, sl], reads=[rw, rlo], writes=[rp])
                k.act(A["a"][:, sl], pt[:, c0:c0 + n], AF.Sigmoid, bias=par[:, 2, c:c + 1], scale=1.0,
                      reads=[rp, rpar], writes=[RA["a"]])
                pt, c0, rp = g.psb()
                k.mm(pt[:, c0:c0 + n], gup[:, 0, cs_], sgd[:, 0, sl], start=True, stop=False, reads=[rw, rlo],
                     writes=[rp])
                k.mm(pt[:, c0:c0 + n], gup[:, 1, cs_], sgd[:, 1, sl], start=False, stop=True, reads=[rw, rlo],
                     writes=[rp])
                k.cp(A["gq"][:, sl], pt[:, c0:c0 + n], [rp], [RA["gq"]])
            for nm in ("lwf", "lwb"):
                k.ts(A[nm][:], A[nm][:], -DECAY_SCALE, None, ALU.mult, reads=[RA[nm]], writes=[RA[nm]], eng="pool")
            k.ts(A["kk"][:], A["k"][:], par[:, 3, c:c + 1], None, ALU.mult, reads=[RA["k"], rpar], writes=[RA["kk"]])
            k.act(A["t"][:], A["kk"][:], AF.Square, reads=[RA["kk"]], writes=[RA["t"]])
            for (t0, n) in BLKS:
                sl = slice(t0, t0 + n)
                pt, c0, rp = g.psb()
                k.mm(pt[:, c0:c0 + n], bo[:], A["t"][:, sl], reads=[rw, RA["t"]], writes=[rp])
                k.act(tmp[:, sl], pt[:, c0:c0 + n], AF.Sqrt, reads=[rp], writes=[rtmp])
            k.ts(tmp[:], tmp[:], 1e-12, None, ALU.max, reads=[rtmp], writes=[rtmp])
            k.op("dve", lambda e, o_=tmp[:]: e.reciprocal(o_, o_), [rtmp], [rtmp])
            k.tt(A["kk"][:], A["kk"][:], tmp[:], ALU.mult, reads=[RA["kk"], rtmp], writes=[RA["kk"]])
            k.ts(A["t"][:], A["a"][:], par[:, 4, c:c + 1], par[:, 5, c:c + 1], ALU.mult, ALU.add,
                 reads=[RA["a"], rpar, RA["t"]], writes=[RA["t"]])
            k.tt(A["k"][:], A["k"][:], A["t"][:], ALU.mult, reads=[RA["k"], RA["t"]], writes=[RA["k"]], eng="pool")
            k.tt(A["a"][:], A["a"][:], A["kk"][:], ALU.mult, reads=[RA["a"], RA["kk"]], writes=[RA["a"]])
            for qi, nm in enumerate(["r", "k", "kk", "a", "v", "lwf", "lwb", "gq"]):
                k.dma(g.rw_d[qi, c], A[nm][:], reads=[RA[nm]], writes=[g.rrw])
        k.barrier()


def rwkv_scan(g, l):
    k, W, C = g.k, g.W, g.C
    with ExitStack() as st:
        mkf = k.sb("smkf", [128, 5, 128], F32, st)
        mk = k.sb("smk", [128, 5, 128], BF16, st)
        ist = k.sb("sist", [128, 64], F32, st)
        istb = k.sb("sistb", [128, 64], BF16, st)
        bo64 = k.sb("sbo64", [128, 128], F32, st)
        bo = k.sb("sbo", [128, 128], F32, st)
        par = k.sb("spar", [128, 3, 4], F32, st)
        epsc = k.sb("sepsc", [128, 1], F32, st)
        rcs = R()
        k.dma(mkf[:], C["rw_mask"], writes=[rcs])
        k.dma(ist[:], C["ist"], writes=[rcs])
        k.dma(bo[:], C["blk_ones"], writes=[rcs])
        k.cp(mk[:], mkf[:], [rcs], [rcs])
        k.cp(istb[:], ist[:], [rcs], [rcs])
        k.ts(bo64[:], bo[:], 1.0 / 64.0, None, ALU.mult, reads=[rcs], writes=[rcs])
        k.memset(epsc[:], 64e-5, writes=[rcs], eng="dve")
        for i, s in enumerate([W["rwkv_ln_w"][l], W["rwkv_ln_b"][l], W["rwkv_r_k"][l].rearrange("h d -> (h d)")]):
            k.dma(par[:, i, :], s.rearrange("(c p) -> p c", p=128), writes=[rcs], allow_slow_non_contiguous=True)
        nat = {n: k.sb("sN_" + n, [128, T], F32, st) for n in ["r", "k", "kk", "b", "v", "lw"]}
        rnat = {n: R() for n in nat}
        yd = [k.sb("syd%d" % d, [128, T], F32, st) for d in range(2)]
        ryd = [R(), R()]
        cum = k.sb("scum", [128, NCH, C_RW], F32, st)
        lwd = k.sb("slwd", [128, NCH, C_RW], F32, st)
        c0t = k.sb("sc0", [128, NCH], F32, st)
        eL = k.sb("seL", [128, NCH, C_RW], F32, st)
        eLx = k.sb("seLx", [128, NCH, C_RW], F32, st)
        rcum, rlwd, rc0, reL, reLx = [R() for _ in range(5)]
        enL, renL = cum, rcum
        tq, rtq = lwd, rlwd
        BD = {n: k.sb("sBD_" + n, [128, NCH, 128], BF16, st) for n in ["R", "A", "B", "K", "V", "BH", "KH"]}
        rBD = {n: R() for n in BD}
        for n in BD:
            k.memset(BD[n][:], 0.0, writes=[rBD[n]], eng="pool")
        Ybd = [k.sb("sYbd%d" % i, [128, 128], F32, st) for i in range(2)]
        rYbd = [R(), R()]
        for i in range(2):
            k.memset(Ybd[i][:], 0.0, writes=[rYbd[i]], eng="pool")
        H = k.sb("sH", [128, 64], F32, st)
        Hb = k.sb("sHb", [128, 64], BF16, st)
        rH, rHb = R(), R()
        NB = 3
        M2 = [k.sb("sM2_%d" % i, [128, 256], BF16, st) for i in range(NB * 2)]
        rM2 = [R() for _ in range(NB * 2)]
        XT = [k.sb("sXT_%d" % i, [128, 128], BF16, st) for i in range(NB * 2)]
        rXT = [R() for _ in range(NB * 2)]
        A3 = [k.sb("sA3_%d" % i, [128, 384], BF16, st) for i in range(NB)]
        rA3 = [R() for _ in range(NB)]
        Vst = [k.sb("sVst_%d" % i, [128, 64], BF16, st) for i in range(NB)]
        rVst = [R() for _ in range(NB)]
        BK = [k.sb("sBK_%d" % i, [128, 256], BF16, st) for i in range(NB)]
        rBK = [R() for _ in range(NB)]
        Bm = [k.sb("sBm_%d" % i, [128, 64], BF16, st) for i in range(2)]
        rBm = [R(), R()]
        Ub = [k.sb("sUb_%d" % i, [128, 64], BF16, st) for i in range(2)]
        rUb = [R(), R()]
        hn = [k.sb("shn%d" % i, [128, 512], F32, st) for i in range(4)]
        rhn = [R() for _ in range(4)]
        gq = eLx[:].rearrange("p n c -> p (n c)")
        rgq = reLx
        yo = [k.sb("syo%d" % i, [128, 512], BF16, st) for i in range(2)]
        ryo = [R(), R()]
        g.rybr = getattr(g, "rybr", None) or R("ybr")
        ev = 0
        for c in range(4):
            for qi, nm in enumerate(["r", "k", "kk", "b", "v"]):
                k.dma(nat[nm][:], g.rw_d[qi, c], reads=[g.rrw], writes=[rnat[nm]])
            for d in range(2):
                lw = nat["lw"]
                rlw = rnat["lw"]
                k.dma(lw[:], g.rw_d[5 + d, c], reads=[g.rrw], writes=[rlw])
                cumf = cum[:].rearrange("p n c -> p (n c)")
                for seg in range(2):
                    a, b = SEGS[seg]
                    k.cp(lwd[:, chs(seg), :], dv(lw, d, seg), [rlw], [rlwd], eng="pool")
                lwdf = lwd[:].rearrange("p n c -> p (n c)")
                k.op("dve", lambda e, o_=cumf, a_=g.ones[:, 0:1].to_broadcast([128, T]), b_=lwdf:
                     e.tensor_tensor_scan(o_, a_, b_, 0.0, ALU.mult, ALU.add), [rlwd, g.rconst], [rcum])
                k.tt(c0t[:], cum[:, :, 0], lwd[:, :, 0], ALU.subtract, reads=[rcum, rlwd], writes=[rc0])
                c0b = c0t[:].rearrange("p (n o) -> p n o", o=1).to_broadcast([128, NCH, C_RW])
                k.tt(cum[:], cum[:], c0b, ALU.subtract, reads=[rcum, rc0], writes=[rcum])
                k.tt(lwd[:], cum[:], lwd[:], ALU.subtract, reads=[rcum, rlwd], writes=[rlwd], eng="pool")
                k.act(eL[:], cum[:], AF.Exp, reads=[rcum], writes=[reL])
                k.act(eLx[:], lwd[:], AF.Exp, reads=[rlwd], writes=[reLx])
                k.act(cum[:], cum[:], AF.Exp, scale=-1.0, reads=[rcum], writes=[rcum])
                WCb = eL[:, :, C_RW - 1:C_RW].to_broadcast([128, NCH, C_RW])
                for seg in range(2):
                    cs_ = chs(seg)
                    for hh in range(2):
                        p0, p1 = hh * 64, hh * 64 + 64
                        ps_ = slice(p0, p1)
                        k.tt(BD["R"][ps_, cs_, ps_], dv(nat["r"], d, seg, p0, p1), eL[ps_, cs_, :], ALU.mult,
                             reads=[rnat["r"], reL], writes=[rBD["R"]])
                        k.stt(BD["A"][ps_, cs_, ps_], dv(nat["kk"], d, seg, p0, p1), -1.0, eLx[ps_, cs_, :],
                              ALU.mult, ALU.mult, reads=[rnat["kk"], reLx], writes=[rBD["A"]])
                        k.cp(BD["V"][ps_, cs_, ps_], dv(nat["v"], d, seg, p0, p1), [rnat["v"]], [rBD["V"]],
                             eng="act")
                    for (src, nb, nh) in (("b", "B", "BH"), ("k", "K", "KH")):
                        k.tt(tq[:, cs_, :], dv(nat[src], d, seg), enL[:, cs_, :], ALU.mult, reads=[rnat[src], renL],
                             writes=[rtq])
                        for hh in range(2):
                            ps_ = slice(hh * 64, hh * 64 + 64)
                            k.cp(BD[nb][ps_, cs_, ps_], tq[ps_, cs_, :], [rtq], [rBD[nb]], eng="act")
                            k.tt(BD[nh][ps_, cs_, ps_], tq[ps_, cs_, :], WCb[ps_, cs_, :], ALU.mult,
                                 reads=[rtq, reL], writes=[rBD[nh]], eng="pool")
                k.memset(H[:], 0.0, writes=[rH], eng="dve")
                k.memset(Hb[:], 0.0, writes=[rHb], eng="dve")
                for n in range(NCH):
                    i3 = n % NB
                    Ab, Bb, Kb, Rb = BD["A"][:, n, :], BD["B"][:, n, :], BD["K"][:, n, :], BD["R"][:, n, :]
                    Vb, BHb, KHb = BD["V"][:, n, :], BD["BH"][:, n, :], BD["KH"][:, n, :]
                    pa, ca, rpa = g.psb()
                    k.mm(pa[:, ca:ca + 128], Ab, Bb, reads=[rBD["A"], rBD["B"]], writes=[rpa])
                    k.mm(pa[:, ca + 128:ca + 256], Bb, Ab, reads=[rBD["A"], rBD["B"]], writes=[rpa])
                    mi = (n % NB) * 2
                    k.tt(M2[mi][:], pa[:, ca:ca + 256], mk[:, 0:2, :].rearrange("p a b -> p (a b)"), ALU.mult,
                         reads=[rpa, rcs], writes=[rM2[mi]])
                    xi = (n % NB) * 2
                    k.tt(XT[xi][:], M2[mi][:, 128:256], g.identb[:], ALU.add, reads=[rM2[mi], g.rconst],
                         writes=[rXT[xi]], eng="pool")
                    curM, rcurM, curX, rcurX = M2[mi], rM2[mi], XT[xi], rXT[xi]
                    for s in range(5):
                        nm_, rnm_ = (M2[mi + 1], rM2[mi + 1]) if curM is M2[mi] else (M2[mi], rM2[mi])
                        nx_, rnx_ = (XT[xi + 1], rXT[xi + 1]) if curX is XT[xi] else (XT[xi], rXT[xi])
                        pm, cm, rpm = g.psb()
                        k.mm(pm[:, cm:cm + 128], curM[:, 128:256], curM[:, 0:128], reads=[rcurM], writes=[rpm])
                        wcols = 128
                        if s < 4:
                            k.mm(pm[:, cm + 128:cm + 256], curM[:, 0:128], curM[:, 128:256], reads=[rcurM],
                                 writes=[rpm])
                            wcols = 256
                        k.act(nm_[:, 0:wcols], pm[:, cm:cm + wcols], AF.Copy, reads=[rpm], writes=[rnm_])
                        px, cx, rpx = g.psb()
                        k.mm(px[:, cx:cx + 128], nm_[:, 0:128], curX[:], reads=[rnm_, rcurX], writes=[rpx])
                        k.tt(nx_[:], px[:, cx:cx + 128], curX[:], ALU.add, reads=[rpx, rcurX], writes=[rnx_])
                        curM, rcurM, curX, rcurX = nm_, rnm_, nx_, rnx_
                    pb, cb_, rpb = g.psb()
                    k.mm(pb[:, cb_:cb_ + 128], Kb, Ab, reads=[rBD["K"], rBD["A"]], writes=[rpb])
                    k.mm(pb[:, cb_ + 128:cb_ + 256], Bb, Rb, reads=[rBD["B"], rBD["R"]], writes=[rpb])
                    k.mm(pb[:, cb_ + 256:cb_ + 384], Kb, Rb, reads=[rBD["K"], rBD["R"]], writes=[rpb])
                    k.tt(A3[i3][:], pb[:, cb_:cb_ + 384], mk[:, 2:5, :].rearrange("p a b -> p (a b)"), ALU.mult,
                         reads=[rpb, rcs], writes=[rA3[i3]])
                    pv, cv, rpv = g.psb()
                    k.mm(pv[:, cv:cv + 64], Vb, istb[:], reads=[rBD["V"], rcs], writes=[rpv])
                    k.act(Vst[i3][:], pv[:, cv:cv + 64], AF.Copy, reads=[rpv], writes=[rVst[i3]])
                    ptt, ct, rpt = g.psb()
                    ptb = ptt.bitcast(BF16)
                    k.tr(ptb[:, 2 * ct:2 * ct + 128], BHb, g.identb[:], reads=[rBD["BH"], g.rconst], writes=[rpt])
                    k.tr(ptb[:, 2 * ct + 128:2 * ct + 256], KHb, g.identb[:], reads=[rBD["KH"], g.rconst],
                         writes=[rpt])
                    k.act(BK[i3][:], ptb[:, 2 * ct:2 * ct + 256], AF.Copy, reads=[rpt], writes=[rBK[i3]])
                    b2 = n % 2
                    p1_, c1, rp1 = g.psb()
                    k.mm(p1_[:, c1:c1 + 64], Ab, Hb[:], start=True, stop=False, reads=[rBD["A"], rHb], writes=[rp1])
                    k.mm(p1_[:, c1:c1 + 64], A3[i3][:, 0:128], Vst[i3][:], start=False, stop=True,
                         reads=[rA3[i3], rVst[i3]], writes=[rp1])
                    k.act(Bm[b2][:], p1_[:, c1:c1 + 64], AF.Copy, reads=[rp1], writes=[rBm[b2]])
                    p2_, c2, rp2 = g.psb()
                    k.mm(p2_[:, c2:c2 + 64], curX[:], Bm[b2][:], reads=[rcurX, rBm[b2]], writes=[rp2])
                    k.cp(Ub[b2][:], p2_[:, c2:c2 + 64], [rp2], [rUb[b2]])
                    py, cy, rpy = g.psb()
                    k.mm(py[:, cy:cy + 64], Rb, Hb[:], start=True, stop=False, reads=[rBD["R"], rHb], writes=[rpy])
                    k.mm(py[:, cy:cy + 64], A3[i3][:, 128:256], Ub[b2][:], start=False, stop=False,
                         reads=[rA3[i3], rUb[b2]], writes=[rpy])
                    k.mm(py[:, cy:cy + 64], A3[i3][:, 256:384], Vst[i3][:], start=False, stop=True,
                         reads=[rA3[i3], rVst[i3]], writes=[rpy])
                    k.cp(Ybd[b2][0:64, 0:64], py[0:64, cy:cy + 64], [rpy], [rYbd[b2]], eng="act")
                    k.cp(Ybd[b2][64:128, 64:128], py[64:128, cy:cy + 64], [rpy], [rYbd[b2]], eng="act")
                    ph, ch_, rph = g.psb()
                    k.mm(ph[:, ch_:ch_ + 64], BK[i3][:, 0:128], Ub[b2][:], start=True, stop=False,
                         reads=[rBK[i3], rUb[b2]], writes=[rph])
                    k.mm(ph[:, ch_:ch_ + 64], BK[i3][:, 128:256], Vst[i3][:], start=False, stop=True,
                         reads=[rBK[i3], rVst[i3]], writes=[rph])
                    k.stt(H[:], H[:], eL[:, n, C_RW - 1:C_RW], ph[:, ch_:ch_ + 64], ALU.mult, ALU.add,
                          reads=[rH, reL, rph], writes=[rH])
                    k.cp(Hb[:], H[:], [rH], [rHb])
                    po, co, rpo = g.psb()
                    k.mm(po[:, co:co + 64], Ybd[b2][:], ist[:], reads=[rYbd[b2], rcs], writes=[rpo])
                    if d == 0:
                        dst = yd[d][:, n * 64:(n + 1) * 64]
                    elif n < 4:
                        dst = rev_ap(yd[d], LC - (n + 1) * 64, LC - n * 64)
                    else:
                        dst = rev_ap(yd[d], T - (n - 3) * 64, T - (n - 4) * 64)
                    k.cp(dst, po[:, co:co + 64], [rpo], [ryd[d]], eng="pool" if False else "dve")
            k.dma(gq, g.rw_d[7, c], reads=[g.rrw], writes=[rgq])
            k.tt(yd[0][:], yd[0][:], yd[1][:], ALU.add, reads=[ryd[0], ryd[1]], writes=[ryd[0]], eng="pool")
            k.stt(yd[1][:], nat["r"][:], par[:, 2, c:c + 1], nat["k"][:], ALU.mult, ALU.mult,
                  reads=[rnat["r"], rnat["k"], rcs, ryd[1]], writes=[ryd[1]])
            y = yd[0]
            ry = ryd[0]
            for bi, (t0, n) in enumerate(BLKS):
                b = bi % 2
                sl = slice(t0, t0 + n)
                p1, c1, rp1 = g.psb()
                k.mm(p1[:, c1:c1 + n], bo64[:], y[:, sl], reads=[rcs, ry], writes=[rp1])
                k.act(hn[0][:, 0:n], y[:, sl], AF.Square, reads=[ry], writes=[rhn[0]])
                p2, c2, rp2 = g.psb()
                k.mm(p2[:, c2:c2 + n], bo64[:], hn[0][:, 0:n], reads=[rcs, rhn[0]], writes=[rp2])
                k.cp(hn[1][:, 0:n], p1[:, c1:c1 + n], [rp1], [rhn[1]], eng="act")
                k.tt(hn[2][:, 0:n], hn[1][:, 0:n], hn[1][:, 0:n], ALU.mult, reads=[rhn[1]], writes=[rhn[2]],
                     eng="pool")
                k.tt(hn[2][:, 0:n], p2[:, c2:c2 + n], hn[2][:, 0:n], ALU.subtract, reads=[rp2, rhn[2]],
                     writes=[rhn[2]])
                k.act(hn[2][:, 0:n], hn[2][:, 0:n], AF.Sqrt, bias=epsc[:, 0:1], scale=1.0, reads=[rhn[2], rcs],
                      writes=[rhn[2]])
                k.op("dve", lambda e, o_=hn[2][:, 0:n]: e.reciprocal(o_, o_), [rhn[2]], [rhn[2]])
                k.tt(hn[3][:, 0:n], y[:, sl], hn[1][:, 0:n], ALU.subtract, reads=[ry, rhn[1]], writes=[rhn[3]])
                k.tt(hn[3][:, 0:n], hn[3][:, 0:n], hn[2][:, 0:n], ALU.mult, reads=[rhn[3], rhn[2]],
                     writes=[rhn[3]])
                k.act(hn[3][:, 0:n], hn[3][:, 0:n], AF.Identity, bias=par[:, 1, c:c + 1], scale=par[:, 0, c:c + 1],
                      reads=[rhn[3], rcs], writes=[rhn[3]])
                p3, c3, rp3 = g.psb()
                k.mm(p3[:, c3:c3 + n], bo[:], yd[1][:, sl], reads=[rcs, ryd[1]], writes=[rp3])
                k.tt(hn[0][:, 0:n], p3[:, c3:c3 + n], nat["v"][:, sl], ALU.mult, reads=[rp3, rnat["v"], rhn[0]],
                     writes=[rhn[0]])
                k.tt(hn[3][:, 0:n], hn[3][:, 0:n], hn[0][:, 0:n], ALU.add, reads=[rhn[3], rhn[0]],
                     writes=[rhn[3]], eng="pool")
                k.tt(yo[b][:, 0:n], hn[3][:, 0:n], gq[:, sl], ALU.mult, reads=[rhn[3], rgq], writes=[ryo[b]])
                k.dma(g.ybr_d[8 + c, :, sl], yo[b][:, 0:n], reads=[ryo[b]], writes=[g.rybr])
        k.barrier()


LN_EPS = 1e-5


def ln_block(g, z, rz, n, scr, rscr, lnw, lnb, rpar, od, rod, epsc, st_tiles):
    k = g.k
    mean, rmean, rstd, rrstd = st_tiles
    pm, cm, rpm = g.psb()
    for c in range(16):
        k.mm(pm[:, cm:cm + n], od[:], z[:, c, 0:n], start=(c == 0), stop=(c == 15), reads=[rod, rz], writes=[rpm])
    for c in range(16):
        k.act(scr[:, c, 0:n], z[:, c, 0:n], AF.Square, reads=[rz], writes=[rscr])
    pv, cv, rpv = g.psb()
    for c in range(16):
        k.mm(pv[:, cv:cv + n], od[:], scr[:, c, 0:n], start=(c == 0), stop=(c == 15), reads=[rod, rscr],
             writes=[rpv])
    k.cp(mean[:, 0:n], pm[:, cm:cm + n], [rpm], [rmean], eng="act")
    k.tt(rstd[:, 0:n], mean[:, 0:n], mean[:, 0:n], ALU.mult, reads=[rmean], writes=[rrstd], eng="pool")
    k.tt(rstd[:, 0:n], pv[:, cv:cv + n], rstd[:, 0:n], ALU.subtract, reads=[rpv, rrstd], writes=[rrstd])
    k.act(rstd[:, 0:n], rstd[:, 0:n], AF.Sqrt, bias=epsc[:, 0:1], scale=1.0, reads=[rrstd, rpar], writes=[rrstd])
    k.op("dve", lambda e, o_=rstd[:, 0:n]: e.reciprocal(o_, o_), [rrstd], [rrstd])
    for c in range(16):
        k.tt(scr[:, c, 0:n], z[:, c, 0:n], mean[:, 0:n], ALU.subtract, reads=[rz, rmean], writes=[rscr])
        k.tt(scr[:, c, 0:n], scr[:, c, 0:n], rstd[:, 0:n], ALU.mult, reads=[rscr, rrstd], writes=[rscr], eng="pool")
        k.act(z[:, c, 0:n], scr[:, c, 0:n], AF.Identity, bias=lnb[:, c:c + 1], scale=lnw[:, c:c + 1],
              reads=[rscr, rpar], writes=[rz])


def phase_merge(g, l, last):
    k, W, C = g.k, g.W, g.C
    with ExitStack() as st:
        xb = k.sb("mxb", [128, 16, 512], F32, st)
        z = k.sb("mz", [128, 16, 512], F32, st)
        u1 = k.sb("mu1", [128, 16, 512], BF16, st)
        yb = k.sb("myb", [128, 16, 512], BF16, st)
        mg = k.sb("mmg", [128, 16, 512], BF16, st)
        rxb, rz, ru1, ryb, rmg = [R() for _ in range(5)]
        wbg = [k.sb("mwbg%d" % i, [128, 16, 512], BF16, st) for i in range(2)]
        rwbg = [R(), R()]
        wbr = [k.sb("mwbr%d" % i, [128, 4, 4, 128], BF16, st) for i in range(2)]
        rwbr = [R(), R()]
        gtt = [k.sb("mgt%d" % i, [128, 512], F32, st) for i in range(2)]
        rgt = [R(), R()]
        tt_ = [k.sb("mtt%d" % i, [128, 512], F32, st) for i in range(2)]
        rtt = [R(), R()]
        macc = k.sb("mmacc", [128, 512], F32, st)
        rmacc = R()
        bbg = k.sb("mbbg", [128, 64], F32, st)
        lnw = k.sb("mlnw", [128, 16], F32, st)
        lnb = k.sb("mlnb", [128, 16], F32, st)
        wr = k.sb("mwr", [128, 16, 36], F32, st)
        br = k.sb("mbr", [36, 1], F32, st)
        od = k.sb("mod2048", [128, 128], F32, st)
        epsc = k.sb("mepsc", [128, 1], F32, st)
        mean = k.sb("mmean", [128, 512], F32, st)
        rstd = k.sb("mrstd", [128, 512], F32, st)
        lgt = k.sb("mlgt", [36, 512], F32, st)
        rlgt = R()
        rpar = R()
        rod = R()
        k.dma(bbg[:], W["b_bgate"][l].rearrange("(j p) -> p j", p=128), writes=[rpar], allow_slow_non_contiguous=True)
        k.dma(lnw[:], W["ln1_w"][l].rearrange("(c p) -> p c", p=128), writes=[rpar], allow_slow_non_contiguous=True)
        k.dma(lnb[:], W["ln1_b"][l].rearrange("(c p) -> p c", p=128), writes=[rpar], allow_slow_non_contiguous=True)
        k.dma(wr[:, :, 0:4], W["moe_w_grp"][l].rearrange("(c p) n -> p c n", p=128), writes=[rpar])
        k.dma(wr[:, :, 4:36], W["moe_w_exp"][l].rearrange("(c p) n -> p c n", p=128), writes=[rpar])
        k.dma(br[0:4, :], W["moe_b_grp"][l].rearrange("(p o) -> p o", o=1), writes=[rpar])
        k.dma(br[4:36, :], W["moe_b_exp"][l].rearrange("(p o) -> p o", o=1), writes=[rpar])
        k.memset(od[:], 1.0 / 2048.0, writes=[rod], eng="dve")
        k.memset(epsc[:], LN_EPS, writes=[rpar], eng="dve")
        lnt = (mean, R(), rstd, R())
        L = k.sb("mL", [128, 36], F32, st)
        sm = k.sb("msm", [128, 16], F32, st)
        gh = k.sb("mgh", [128, 4], F32, st)
        mk1 = k.sb("mmk1", [128, 32], F32, st)
        mk2 = k.sb("mmk2", [128, 32], F32, st)
        oh1 = k.sb("moh1", [128, 32], F32, st)
        oh2 = k.sb("moh2", [128, 32], F32, st)
        cmb = k.sb("mcmb", [128, 32], F32, st)
        cmbT = k.sb("mcmbT", [32, 512], F32, st)
        rL, rsm, rgh, rmk1, rmk2, roh1, roh2, rcmb, rcmbT = [R() for _ in range(9)]
        g.ru2 = getattr(g, "ru2", None) or R("u2_d")
        g.rcmbd = getattr(g, "rcmbd", None) or R("cmb_d")
        wi = 0
        for bi, (t0, n) in enumerate(BLKS):
            if last and t0 == 0:
                continue
            j = 1 if t0 == 0 else 0
            k.dma(xb[:, :, 0:n], g.xs_d[:, :, t0:t0 + n].rearrange("c p t -> p c t"), reads=[g.rxs], writes=[rxb])
            k.dma(yb[:, :, 0:n], g.ybr_d[:, :, t0:t0 + n].rearrange("i p t -> p i t"), reads=[g.rybr], writes=[ryb])
            for c in range(16):
                k.act(u1[:, c, 0:n], xb[:, c, 0:n], AF.Identity, bias=mod(g, l, 0, c, j), scale=mod(g, l, 1, c, j),
                      reads=[rxb, g.rmod], writes=[ru1])
            k.ts(xb[:, :, 0:n], xb[:, :, 0:n], ALPHA, None, ALU.mult, reads=[rxb], writes=[rxb], eng="pool")
            for c in range(16):
                b = wi % 2
                wi += 1
                for kbr in range(4):
                    k.dma(wbg[b][:, :, kbr * 128:(kbr + 1) * 128],
                          W["w_bgate"][l, :, kbr * 2048 + c * 128:kbr * 2048 + (c + 1) * 128].rearrange(
                              "(cc p) n -> p cc n", p=128), writes=[rwbg[b]], issuer="pool")
                    k.dma(wbr[b][:, kbr, :, :],
                          W["w_branch"][l, kbr, :, c * 128:(c + 1) * 128].rearrange("(cc p) n -> p cc n", p=128),
                          writes=[rwbr[b]], issuer="pool")
                for kbr in range(4):
                    pg, cg, rpg = g.psb()
                    for kc in range(16):
                        k.mm(pg[:, cg:cg + n], wbg[b][:, kc, kbr * 128:(kbr + 1) * 128], u1[:, kc, 0:n],
                             start=(kc == 0), stop=(kc == 15), reads=[rwbg[b], ru1], writes=[rpg])
                    pp, cp_, rpp = g.psb()
                    for cc in range(4):
                        k.mm(pp[:, cp_:cp_ + n], wbr[b][:, kbr, cc, :], yb[:, kbr * 4 + cc, 0:n], start=(cc == 0),
                             stop=(cc == 3), reads=[rwbr[b], ryb], writes=[rpp])
                    gb_ = kbr % 2
                    k.act(gtt[gb_][:, 0:n], pg[:, cg:cg + n], AF.Sigmoid, bias=bbg[:, kbr * 16 + c:kbr * 16 + c + 1],
                          scale=1.0, reads=[rpg, rpar], writes=[rgt[gb_]])
                    if kbr == 0:
                        k.tt(macc[:, 0:n], pp[:, cp_:cp_ + n], gtt[gb_][:, 0:n], ALU.mult, reads=[rpp, rgt[gb_]],
                             writes=[rmacc])
                    else:
                        k.tt(tt_[gb_][:, 0:n], pp[:, cp_:cp_ + n], gtt[gb_][:, 0:n], ALU.mult,
                             reads=[rpp, rgt[gb_]], writes=[rtt[gb_]])
                        if kbr < 3:
                            k.tt(macc[:, 0:n], macc[:, 0:n], tt_[gb_][:, 0:n], ALU.add, reads=[rmacc, rtt[gb_]],
                                 writes=[rmacc], eng="pool")
                        else:
                            k.tt(mg[:, c, 0:n], macc[:, 0:n], tt_[gb_][:, 0:n], ALU.add, reads=[rmacc, rtt[gb_]],
                                 writes=[rmg], eng="pool")
            for grp in range(4):
                b = wi % 2
                wi += 1
                k.dma(wbg[b][:], W["w_out"][l, :, grp * 512:(grp + 1) * 512].rearrange("(cc p) n -> p cc n", p=128),
                      writes=[rwbg[b]], issuer="pool")
                for nn in range(4):
                    ni = grp * 4 + nn
                    po, co, rpo = g.psb()
                    for c in range(16):
                        k.mm(po[:, co:co + n], wbg[b][:, c, nn * 128:(nn + 1) * 128], mg[:, c, 0:n], start=(c == 0),
                             stop=(c == 15), reads=[rwbg[b], rmg], writes=[rpo])
                    k.stt(z[:, ni, 0:n], po[:, co:co + n], mod(g, l, 2, ni, j), xb[:, ni, 0:n], ALU.mult, ALU.add,
                          reads=[rpo, g.rmod, rxb], writes=[rz])
            ln_block(g, z, rz, n, xb, rxb, lnw, lnb, rpar, od, rod, epsc, lnt)
            k.dma(g.xs_d[:, :, t0:t0 + n].rearrange("c p t -> p c t"), z[:, :, 0:n], reads=[rz], writes=[g.rxs])
            for c in range(16):
                k.act(xb[:, c, 0:n], z[:, c, 0:n], AF.Identity, bias=mod(g, l, 3, c, j), scale=mod(g, l, 4, c, j),
                      reads=[rz, g.rmod], writes=[rxb])
            k.cp(u1[:, :, 0:n], xb[:, :, 0:n], [rxb], [ru1], eng="pool")
            k.dma(g.u2_d[:, :, t0:t0 + n].rearrange("c p t -> p c t"), u1[:, :, 0:n], reads=[ru1], writes=[g.ru2])
            pl_, cl, rpl = g.psb()
            for c in range(16):
                k.mm(pl_[0:36, cl:cl + n], wr[:, c, :], xb[:, c, 0:n], start=(c == 0), stop=(c == 15),
                     reads=[rpar, rxb], writes=[rpl])
            k.act(lgt[:, 0:n], pl_[0:36, cl:cl + n], AF.Identity, bias=br[:, 0:1], scale=1.0, reads=[rpl, rpar],
                  writes=[rlgt])
            for ti in range(n // 128):
                tsl = slice(ti * 128, (ti + 1) * 128)
                pt, ct, rpt = g.psb()
                k.tr(pt[:, ct:ct + 36], lgt[:, tsl], g.ident[0:36, 0:36], reads=[rlgt, g.rconst], writes=[rpt])
                k.cp(L[:], pt[:, ct:ct + 36], [rpt], [rL])
                D_ = "dve"
                k.op(D_, lambda e: e.tensor_reduce(sm[:, 0:1], L[:, 0:4], AX.X, ALU.max), [rL], [rsm])
                k.ts(gh[:], L[:, 0:4], sm[:, 0:1], None, ALU.subtract, reads=[rL, rsm], writes=[rgh])
                k.act(gh[:], gh[:], AF.Exp, reads=[rgh], writes=[rgh])
                k.op(D_, lambda e: e.tensor_reduce(sm[:, 1:2], gh[:], AX.X, ALU.add), [rgh], [rsm])
                k.op(D_, lambda e: e.reciprocal(sm[:, 2:3], sm[:, 1:2]), [rsm], [rsm])
                k.ts(gh[:], L[:, 0:4], sm[:, 0:1], None, ALU.is_equal, reads=[rL, rsm, rgh], writes=[rgh])
                k.ts(gh[:], gh[:], -1.0, 1e30, ALU.add, ALU.mult, reads=[rgh], writes=[rgh])
                k.tt(mk1[:].rearrange("p (a b) -> p a b", b=8), L[:, 4:36].rearrange("p (a b) -> p a b", b=8),
                     gh[:].rearrange("p (a o) -> p a o", o=1).to_broadcast([128, 4, 8]), ALU.add,
                     reads=[rL, rgh], writes=[rmk1])
                k.op(D_, lambda e: e.tensor_reduce(sm[:, 3:4], mk1[:], AX.X, ALU.max), [rmk1], [rsm])
                k.ts(oh1[:], mk1[:], sm[:, 3:4], None, ALU.is_equal, reads=[rmk1, rsm], writes=[roh1])
                k.stt(mk2[:], oh1[:], -1e30, mk1[:], ALU.mult, ALU.add, reads=[roh1, rmk1], writes=[rmk2])
                k.op(D_, lambda e: e.tensor_reduce(sm[:, 4:5], mk2[:], AX.X, ALU.max), [rmk2], [rsm])
                k.ts(oh2[:], mk2[:], sm[:, 4:5], None, ALU.is_equal, reads=[rmk2, rsm], writes=[roh2])
                k.tt(sm[:, 5:6], sm[:, 4:5], sm[:, 3:4], ALU.subtract, reads=[rsm], writes=[rsm])
                k.act(sm[:, 6:7], sm[:, 5:6], AF.Exp, reads=[rsm], writes=[rsm])
                k.ts(sm[:, 7:8], sm[:, 6:7], 1.0, None, ALU.add, reads=[rsm], writes=[rsm])
                k.op(D_, lambda e: e.reciprocal(sm[:, 8:9], sm[:, 7:8]), [rsm], [rsm])
                k.tt(sm[:, 9:10], sm[:, 8:9], sm[:, 2:3], ALU.mult, reads=[rsm], writes=[rsm])
                k.tt(sm[:, 10:11], sm[:, 9:10], sm[:, 6:7], ALU.mult, reads=[rsm], writes=[rsm])
                k.ts(cmb[:], oh1[:], sm[:, 9:10], None, ALU.mult, reads=[roh1, rsm], writes=[rcmb])
                k.stt(cmb[:], oh2[:], sm[:, 10:11], cmb[:], ALU.mult, ALU.add, reads=[roh2, rsm, rcmb], writes=[rcmb])
                pt2, ct2, rpt2 = g.psb()
                k.tr(pt2[0:32, ct2:ct2 + 128], cmb[:], g.ident[:], reads=[rcmb, g.rconst], writes=[rpt2])
                k.cp(cmbT[:, tsl], pt2[0:32, ct2:ct2 + 128], [rpt2], [rcmbT], eng="act")
            k.dma(g.cmb_d[:, t0:t0 + n], cmbT[:, 0:n], reads=[rcmbT], writes=[g.rcmbd])
        k.barrier()


def phase_moe(g, l, last, n_exp=32):
    k, W, C = g.k, g.W, g.C
    parts = [[0, 1, 2], [3, 4]]
    if last:
        parts = [[1, 2], [3, 4]]
    with ExitStack() as st:
        u2 = k.sb("eu2", [128, 16, 1280], BF16, st)
        acc = k.sb("eacc", [128, 16, 1280], F32, st)
        ru2s, racc = R(), R()
        lnw = k.sb("elnw", [128, 16], F32, st)
        lnb = k.sb("elnb", [128, 16], F32, st)
        od = k.sb("eod", [128, 128], F32, st)
        epsc = k.sb("eepsc", [128, 1], F32, st)
        rpar, rod = R(), R()
        k.dma(lnw[:], W["ln2_w"][l].rearrange("(c p) -> p c", p=128), writes=[rpar], allow_slow_non_contiguous=True)
        k.dma(lnb[:], W["ln2_b"][l].rearrange("(c p) -> p c", p=128), writes=[rpar], allow_slow_non_contiguous=True)
        k.memset(od[:], 1.0 / 2048.0, writes=[rod], eng="dve")
        k.memset(epsc[:], LN_EPS, writes=[rpar], eng="dve")
        for part in parts:
            blks = [BLKS[i] for i in part]
            p0 = blks[0][0]
            np_ = sum(n for _, n in blks)
            k.dma(u2[:, :, 0:np_], g.u2_d[:, :, p0:p0 + np_].rearrange("c p t -> p c t"), reads=[g.ru2],
                  writes=[ru2s])
            k.memset(acc[:, :, 0:np_], 0.0, writes=[racc], eng="pool")
            with ExitStack() as st2:
                w13 = [k.sb("ew13_%d" % i, [128, 16, 2, 128], BF16, st2) for i in range(2)]
                rw13 = [R(), R()]
                w2b = [k.sb("ew2_%d" % i, [128, 4, 2048], BF16, st2) for i in range(2)]
                rw2 = [R(), R()]
                hT = k.sb("ehT", [128, 4, 1280], BF16, st2)
                rhT = R()
                cb = [k.sb("ecb%d" % i, [128, 512], F32, st2) for i in range(3)]
                rcb = [R() for _ in range(3)]
                sg = [k.sb("esg%d" % i, [128, 512], F32, st2) for i in range(2)]
                rsg = [R(), R()]
                wi = 0
                ci = 0
                for e in range(n_exp):
                    eb = e % 2
                    k.dma(w2b[eb][:], W["moe_w2"][l, e].rearrange("(f p) n -> p f n", p=128), writes=[rw2[eb]],
                          issuer="pool")
                    cbs = []
                    for (t0, n) in blks:
                        cix = ci % 3
                        ci += 1
                        k.dma(cb[cix][:, 0:n], g.cmb_d[e, t0:t0 + n].partition_broadcast(128), reads=[g.rcmbd],
                              writes=[rcb[cix]])
                        cbs.append(cix)
                    for f in range(4):
                        b = wi % 2
                        wi += 1
                        k.dma(w13[b][:, :, 0, :],
                              W["moe_w1"][l, e, :, f * 128:(f + 1) * 128].rearrange("(c p) n -> p c n", p=128),
                              writes=[rw13[b]], issuer="pool")
                        k.dma(w13[b][:, :, 1, :],
                              W["moe_w3"][l, e, :, f * 128:(f + 1) * 128].rearrange("(c p) n -> p c n", p=128),
                              writes=[rw13[b]], issuer="pool")
                        for bi, (t0, n) in enumerate(blks):
                            o = t0 - p0
                            p1, c1, rp1 = g.psb()
                            for c in range(16):
                                k.mm(p1[:, c1:c1 + n], w13[b][:, c, 0, :], u2[:, c, o:o + n], start=(c == 0),
                                     stop=(c == 15), reads=[rw13[b], ru2s], writes=[rp1])
                            p3, c3, rp3 = g.psb()
                            for c in range(16):
                                k.mm(p3[:, c3:c3 + n], w13[b][:, c, 1, :], u2[:, c, o:o + n], start=(c == 0),
                                     stop=(c == 15), reads=[rw13[b], ru2s], writes=[rp3])
                            sb_ = (f * 8 + bi) % 2
                            k.act(sg[sb_][:, 0:n], p1[:, c1:c1 + n], AF.Silu, reads=[rp1], writes=[rsg[sb_]])
                            k.tt(sg[sb_][:, 0:n], p3[:, c3:c3 + n], sg[sb_][:, 0:n], ALU.mult, reads=[rp3, rsg[sb_]],
                                 writes=[rsg[sb_]])
                            k.tt(hT[:, f, o:o + n], sg[sb_][:, 0:n], cb[cbs[bi]][:, 0:n], ALU.mult,
                                 reads=[rsg[sb_], rcb[cbs[bi]]], writes=[rhT], eng="pool")
                    for bi, (t0, n) in enumerate(blks):
                        o = t0 - p0
                        for nn in range(16):
                            po, co, rpo = g.psb()
                            for f in range(4):
                                k.mm(po[:, co:co + n], w2b[eb][:, f, nn * 128:(nn + 1) * 128], hT[:, f, o:o + n],
                                     start=(f == 0), stop=(f == 3), reads=[rw2[eb], rhT], writes=[rpo])
                            k.tt(acc[:, nn, o:o + n], po[:, co:co + n], acc[:, nn, o:o + n], ALU.add,
                                 reads=[rpo, racc], writes=[racc])
                k.barrier()
            with ExitStack() as st3:
                xb = k.sb("exb", [128, 16, 512], F32, st3)
                zz = k.sb("ezz", [128, 16, 512], F32, st3)
                mean = k.sb("emean", [128, 512], F32, st3)
                rstd = k.sb("erstd", [128, 512], F32, st3)
                rxb, rzz = R(), R()
                lnt = (mean, R(), rstd, R())
                for (t0, n) in blks:
                    j = 1 if t0 == 0 else 0
                    o = t0 - p0
                    k.dma(xb[:, :, 0:n], g.xs_d[:, :, t0:t0 + n].rearrange("c p t -> p c t"), reads=[g.rxs],
                          writes=[rxb])
                    k.ts(xb[:, :, 0:n], xb[:, :, 0:n], ALPHA, None, ALU.mult, reads=[rxb], writes=[rxb], eng="pool")
                    for c in range(16):
                        k.stt(zz[:, c, 0:n], acc[:, c, o:o + n], mod(g, l, 5, c, j), xb[:, c, 0:n], ALU.mult,
                              ALU.add, reads=[racc, g.rmod, rxb], writes=[rzz])
                    ln_block(g, zz, rzz, n, xb, rxb, lnw, lnb, rpar, od, rod, epsc, lnt)
                    k.dma(g.xs_d[:, :, t0:t0 + n].rearrange("c p t -> p c t"), zz[:, :, 0:n], reads=[rzz],
                          writes=[g.rxs])
                k.barrier()
        k.barrier()

from concourse.bass_utils import run_bass_kernel_spmd

N_CORES = 4


def kernel(**inputs):
    nc = build(nl=DEPTH)
    cs = host_consts()
    in_maps = []
    for b in range(N_CORES):
        m = {}
        for n in W_SHAPES:
            m[n] = np.ascontiguousarray(np.asarray(inputs[n], dtype=np.float32))
        m["xin"] = np.ascontiguousarray(
            np.concatenate([np.asarray(inputs["ctx"][b]), np.asarray(inputs["x"][b])], 0).astype(np.float32))
        m["c2"] = np.ascontiguousarray(
            np.stack([np.asarray(inputs["c"][b]), np.asarray(inputs["c_ctx"])], 0).astype(np.float32))
        for n, v in cs.items():
            m["k_" + n] = v
        in_maps.append(m)
    res = run_bass_kernel_spmd(nc, in_maps, core_ids=list(range(N_CORES)))
    out = np.stack([np.asarray(r["out"], dtype=np.float32) for r in res.results], 0)
    return out
```

```python
import numpy as np
from contextlib import ExitStack
import concourse.bass as bass
import concourse.mybir as mybir

F32 = mybir.dt.float32
BF16 = mybir.dt.bfloat16
AF = mybir.ActivationFunctionType
ALU = mybir.AluOpType
AX = mybir.AxisListType

NDQ = 8


class R:
    __slots__ = ("w", "rd", "name")

    def __init__(self, name=""):
        self.w = None
        self.rd = {}
        self.name = name


class KB:
    def __init__(self, nc):
        self.nc = nc
        self.es = ExitStack()
        self.eng = {"pe": nc.tensor, "act": nc.scalar, "dve": nc.vector, "pool": nc.gpsimd, "sp": nc.sync}
        self.real = list(self.eng.keys())
        self.virt = ["dq%d" % i for i in range(NDQ)]
        self.all = self.real + self.virt
        self.sem = {}
        for e in self.all:
            self.sem[e] = self.es.enter_context(nc.semaphore("s_" + e))
        self.inc = {e: (1 if e in self.real else 16) for e in self.all}
        self.cnt = {e: 0 for e in self.all}
        self.seen = {e: {f: 0 for f in self.all} for e in self.real}
        self.q = {e: [] for e in self.real}
        self.dq_next = 0
        self.n_ops = 0

    def sb(self, name, shape, dtype, stack=None):
        self.uid = getattr(self, "uid", 0) + 1
        t = (stack or self.es).enter_context(self.nc.sbuf_tensor("%s_u%d" % (name, self.uid), list(shape), dtype))
        return t

    def ps(self, name, shape, dtype, stack=None):
        t = (stack or self.es).enter_context(self.nc.psum_tensor(name, list(shape), dtype))
        return t

    def _waits(self, eng, reads, writes):
        needs = {}
        for r in reads:
            if r.w is not None:
                f, n = r.w
                if n > needs.get(f, 0):
                    needs[f] = n
        for w in writes:
            if w.w is not None:
                f, n = w.w
                if f != eng and n > needs.get(f, 0):
                    needs[f] = n
            for f, n in w.rd.items():
                if f != eng and n > needs.get(f, 0):
                    needs[f] = n
        seen = self.seen[eng]
        for f, n in needs.items():
            if seen[f] >= n:
                continue
            seen[f] = n
            self.q[eng].append(("w", self.sem[f], n * self.inc[f]))

    def _commit(self, tag, reads, writes):
        self.cnt[tag] += 1
        n = self.cnt[tag]
        for r in reads:
            if n > r.rd.get(tag, 0):
                r.rd[tag] = n
        for w in writes:
            w.w = (tag, n)
            w.rd = {}
        return n

    def op(self, eng, fn, reads=(), writes=(), inc=True):
        self._waits(eng, reads, writes)
        if inc:
            self._commit(eng, reads, writes)
            self.q[eng].append(("o", fn, self.sem[eng], 1))
        else:
            n = self.cnt[eng] + 1
            for r in reads:
                if n > r.rd.get(eng, 0):
                    r.rd[eng] = n
            for w in writes:
                w.w = (eng, n)
                w.rd = {}
            self.q[eng].append(("n", fn))
        self.n_ops += 1

    def dma(self, out, in_, reads=(), writes=(), issuer="sp", **kw):
        slot = self.virt[self.dq_next]
        self.dq_next = (self.dq_next + 1) % NDQ
        seen = self.seen[issuer]
        if seen[slot] < self.cnt[slot]:
            seen[slot] = self.cnt[slot]
            self.q[issuer].append(("w", self.sem[slot], self.cnt[slot] * 16))
        self._waits(issuer, reads, writes)
        self._commit(slot, reads, writes)
        self.q[issuer].append(("o", (lambda e, o=out, i=in_, k=kw: e.dma_start(out=o, in_=i, **k)), self.sem[slot], 16))
        self.n_ops += 1

    def barrier(self):
        for e in self.real:
            seen = self.seen[e]
            for f in self.all:
                if f == e:
                    continue
                if seen[f] < self.cnt[f]:
                    seen[f] = self.cnt[f]
                    self.q[e].append(("w", self.sem[f], self.cnt[f] * self.inc[f]))

    def finish(self):
        self.barrier()
        nc = self.nc
        q = self.q

        def replay(name):
            def f(e):
                for it in q[name]:
                    if it[0] == "w":
                        e.wait_ge(it[1], it[2])
                    elif it[0] == "n":
                        it[1](e)
                    else:
                        ins = it[1](e)
                        ins.then_inc(it[2], it[3])
            return f

        with nc.Block() as block:
            block.tensor(replay("pe"))
            block.scalar(replay("act"))
            block.vector(replay("dve"))
            block.gpsimd(replay("pool"))
            block.sync(replay("sp"))
        self.es.close()

    def mm(self, out, lhsT, rhs, start=True, stop=True, reads=(), writes=(), **kw):
        self.op("pe", lambda e: e.matmul(out, lhsT, rhs, start=start, stop=stop, **kw), reads, writes, inc=bool(stop))

    def tr(self, out, in_, ident, reads=(), writes=()):
        self.op("pe", lambda e: e.transpose(out, in_, ident), reads, writes)

    def act(self, out, in_, func, bias=None, scale=None, reads=(), writes=(), eng="act", **kw):
        k = dict(kw)
        if bias is not None:
            k["bias"] = bias
        if scale is not None:
            k["scale"] = scale
        self.op("act", lambda e: e.activation(out, in_, func, **k), reads, writes)

    def tt(self, out, a, b, op, reads=(), writes=(), eng="dve"):
        self.op(eng, lambda e: e.tensor_tensor(out, a, b, op), reads, writes)

    def ts(self, out, a, s1, s2, op0, op1=None, reads=(), writes=(), eng="dve", **kw):
        if op1 is None:
            self.op(eng, lambda e: e.tensor_scalar(out, a, s1, None, op0, **kw), reads, writes)
        else:
            self.op(eng, lambda e: e.tensor_scalar(out, a, s1, s2, op0, op1, **kw), reads, writes)

    def stt(self, out, a, s, b, op0, op1, reads=(), writes=(), eng="dve"):
        self.op(eng, lambda e: e.scalar_tensor_tensor(out, a, s, b, op0, op1), reads, writes)

    def cp(self, out, in_, reads=(), writes=(), eng="dve"):
        if eng == "act":
            self.op("act", lambda e: e.copy(out, in_), reads, writes)
        else:
            self.op(eng, lambda e: e.tensor_copy(out, in_), reads, writes)

    def memset(self, ap, val, writes=(), eng="pool"):
        self.op(eng, lambda e: e.memset(ap, val), (), writes)

import math
import numpy as np
from contextlib import ExitStack
import concourse.bass as bass
import concourse.mybir as mybir

T = 2304
LC = 256
D = 2048
KC = 16
BLKS = [(0, 256), (256, 512), (768, 512), (1280, 512), (1792, 512)]
NT = 18
DEPTH = 4
ALPHA = (2.0 * DEPTH) ** 0.25
C_RW = 64
NCH = T // C_RW

CHUNKS = {}
_o = 0
for h in range(4):
    CHUNKS["ret_q%d" % h] = [(0 + h * 128, 128)]
    CHUNKS["ret_k%d" % h] = [(512 + h * 128, 128)]
    CHUNKS["ret_g%d" % h] = [(1536 + h * 128, 128)]
for c in range(4):
    CHUNKS["gqa_q%d" % c] = [(2048 + c * 128, 128)]
for g in range(2):
    CHUNKS["gqa_k%d" % g] = [(2560 + g * 64, 64), (2560 + g * 64, 64)]
RW0 = 2816
for c in range(4):
    CHUNKS["rw_r%d" % c] = [(RW0 + c * 128, 128)]
    CHUNKS["rw_k%d" % c] = [(RW0 + 512 + c * 128, 128)]
    CHUNKS["rw_v%d" % c] = [(RW0 + 1024 + c * 128, 128)]
CHUNKS["rw_wdf"] = [(RW0 + 1536, 96)]
CHUNKS["rw_wdb"] = [(RW0 + 1632, 96)]
CHUNKS["rw_ad"] = [(RW0 + 1728, 96)]
CHUNKS["rw_gd0"] = [(RW0 + 1824, 128)]
CHUNKS["rw_gd1"] = [(RW0 + 1952, 128)]
LR0 = 4896
for c in range(4):
    CHUNKS["lru_x%d" % c] = [(LR0 + c * 128, 128)]
    CHUNKS["lru_g%d" % c] = [(LR0 + 512 + c * 128, 128)]
CH_NAMES = list(CHUNKS.keys())
CH_ID = {n: i for i, n in enumerate(CH_NAMES)}
NCHUNK = len(CH_NAMES)


def ch_width(name):
    return sum(w for _, w in CHUNKS[name])


def host_consts():
    cs = {}
    cs["ident"] = np.eye(128, dtype=np.float32)
    tok = np.arange(2048)
    row = (tok // 64).astype(np.float32)
    col = (tok % 64).astype(np.float32)

    def tables(dh):
        da = dh // 2
        inv = (10000.0 ** (-np.arange(0, da, 2, dtype=np.float32) / da)).astype(np.float32)
        nf = da // 2
        cos = np.ones((dh, T), np.float32)
        sin = np.zeros((dh, T), np.float32)
        for d in range(dh):
            first = d < da
            dd = d if first else d - da
            fi = dd % nf
            pos = row if first else col
            ang = (pos * inv[fi]).astype(np.float32)
            cos[d, LC:] = np.cos(ang)
            sin[d, LC:] = np.sin(ang)
        P = np.zeros((dh, dh), np.float32)
        for m in range(dh):
            dd = m % da
            if dd < nf:
                P[m + nf, m] = -1.0
            else:
                P[m - nf, m] = 1.0
        return cos, sin, P

    c, s, P = tables(128)
    cs["ret_cos"], cs["ret_sin"], cs["ret_P"] = c, s, P
    c, s, P = tables(64)
    cs["gqa_cos"] = np.concatenate([c, c], 0)
    cs["gqa_sin"] = np.concatenate([s, s], 0)
    P2 = np.zeros((128, 128), np.float32)
    P2[:64, :64] = P
    P2[64:, 64:] = P
    cs["gqa_P"] = P2
    j = np.arange(128)[:, None].astype(np.float32)
    i = np.arange(128)[None, :].astype(np.float32)
    sc = 128.0 ** -0.5
    ret = np.zeros((128, 6, 128), np.float32)
    ret[:, 0] = np.maximum(i - j, 0)
    ret[:, 1] = np.maximum(j - i, 0)
    ret[:, 2] = (i >= j) * sc
    ret[:, 3] = (j >= i) * sc
    ret[:, 4] = np.broadcast_to(i + 1.0, (128, 128))
    ret[:, 5] = np.broadcast_to(128.0 - i, (128, 128))
    cs["ret_tab"] = ret
    cj = np.zeros((128, 2), np.float32)
    cj[:, 0] = 127.0 - np.arange(128)
    cj[:, 1] = np.arange(128)
    cs["ret_cj"] = cj
    gm = np.zeros((128, 2, 128), np.float32)
    gm[:, 0] = (j >= i)
    gm[:, 1] = (j <= i)
    cs["gqa_mask"] = gm
    blk = (np.arange(128)[:, None] // 64) == (np.arange(128)[None, :] // 64)
    a = np.arange(128)[:, None] % 64
    b = np.arange(128)[None, :] % 64
    rm = np.zeros((128, 5, 128), np.float32)
    rm[:, 0] = blk & (a > b)
    rm[:, 1] = blk & (b > a)
    rm[:, 2] = blk & (b > a)
    rm[:, 3] = blk & (b >= a)
    rm[:, 4] = blk & (b >= a)
    cs["rw_mask"] = rm
    cs["blk_ones"] = blk.astype(np.float32)
    ist = np.zeros((128, 64), np.float32)
    ist[np.arange(128), np.arange(128) % 64] = 1.0
    cs["ist"] = ist
    return cs


CONST_SHAPES = None


W_SHAPES = {
    "w_ada": [4, 2048, 12288], "b_ada": [4, 12288], "w_in": [4, 2048, 5920],
    "ret_decay_logit": [4, 2, 4], "ret_gn_w": [4, 512], "ret_gn_b": [4, 512], "gqa_sink": [4, 8],
    "rwkv_mu": [4, 2080], "rwkv_w0": [4, 2, 512], "rwkv_w_up": [4, 2, 96, 512], "rwkv_a0": [4, 512],
    "rwkv_a_up": [4, 96, 512], "rwkv_g_up": [4, 256, 512], "rwkv_k_k": [4, 512], "rwkv_k_a": [4, 512],
    "rwkv_r_k": [4, 8, 64], "rwkv_ln_w": [4, 512], "rwkv_ln_b": [4, 512],
    "lru_conv_w": [4, 4, 512], "lru_conv_b": [4, 512], "lru_gate_w": [4, 2, 2, 8, 64, 64],
    "lru_gate_b": [4, 2, 2, 512], "lru_lambda": [4, 2, 512],
    "w_branch": [4, 4, 512, 2048], "w_bgate": [4, 2048, 8192], "b_bgate": [4, 8192], "w_out": [4, 2048, 2048],
    "ln1_w": [4, 2048], "ln1_b": [4, 2048], "ln2_w": [4, 2048], "ln2_b": [4, 2048],
    "moe_w_grp": [4, 2048, 4], "moe_b_grp": [4, 4], "moe_w_exp": [4, 2048, 32], "moe_b_exp": [4, 32],
    "moe_w1": [4, 32, 2048, 512], "moe_w3": [4, 32, 2048, 512], "moe_w2": [4, 32, 512, 2048],
}


class Ctx:
    pass


def build(nl=4, dump=(), stop=None, n_exp=32):
    nc = bass.Bass("TRN2", target_bir_lowering=False)
    g = Ctx()
    g.nc = nc
    g.nl = nl
    W = {}
    for n, s in W_SHAPES.items():
        W[n] = nc.dram_tensor(n, [nl] + list(s[1:]), F32, kind="ExternalInput").ap()
    g.W = W
    g.xin = nc.dram_tensor("xin", [T, D], F32, kind="ExternalInput").ap()
    g.c2 = nc.dram_tensor("c2", [2, D], F32, kind="ExternalInput").ap()
    cs = host_consts()
    g.C = {n: nc.dram_tensor("k_" + n, list(v.shape), F32, kind="ExternalInput").ap() for n, v in cs.items()}
    g.out = nc.dram_tensor("out", [2048, D], F32, kind="ExternalOutput").ap()

    def scratch(name, shape, dt):
        kind = "ExternalOutput" if name in dump else "Internal"
        return nc.dram_tensor(name, shape, dt, kind=kind).ap()

    g.xs_d = scratch("xs_d", [KC, 128, T], F32)
    g.p_d = scratch("p_d", [NCHUNK, 128, T], F32)
    g.vtm_d = scratch("vtm_d", [T, 640], BF16)
    g.ybr_d = scratch("ybr_d", [16, 128, T], BF16)
    g.rw_d = scratch("rw_d", [8, 4, 128, T], F32)
    g.u2_d = scratch("u2_d", [KC, 128, T], BF16)
    g.modT_d = scratch("modT_d", [128, 4 * 96 * 2], F32)
    g.cmb_d = scratch("cmb_d", [32, T], F32)

    k = KB(nc)
    g.k = k
    g.ident = k.sb("ident", [128, 128], F32)
    g.identb = k.sb("identb", [128, 128], BF16)
    g.ones = k.sb("ones", [128, 512], F32)
    g.onesb = k.sb("onesb", [128, 128], BF16)
    g.modT = k.sb("modT", [128, 4, 96, 2], F32)
    g.rconst = R("const")
    g.rmod = R("mod")
    k.dma(g.ident[:], g.C["ident"], writes=[g.rconst])
    k.cp(g.identb[:], g.ident[:], [g.rconst], [g.rconst])
    k.memset(g.ones[:], 1.0, writes=[g.rconst], eng="dve")
    k.memset(g.onesb[:], 1.0, writes=[g.rconst], eng="dve")
    g.pd = [k.ps("pd%d" % i, [128, 1024], F32) for i in range(4)]
    g.pr = [[R("ps%d_%d" % (i, h)) for h in range(2)] for i in range(4)]
    g.pi = 0

    def psb():
        i = g.pi
        g.pi = (g.pi + 1) % 8
        t = g.pd[i // 2]
        h = i % 2
        return t, h * 512, g.pr[i // 2][h]

    def psd():
        if g.pi % 2:
            g.pi = (g.pi + 1) % 8
        i = g.pi // 2
        g.pi = (g.pi + 2) % 8
        return g.pd[i], g.pr[i]

    g.psb = psb
    g.psd = psd

    prologue(g)
    if stop == "prologue":
        return finish(g)
    for l in range(nl):
        last = (l == DEPTH - 1)
        phase_inproj(g, l)
        if stop == "inproj":
            return finish(g)
        mix_ret(g, l)
        if stop == "ret":
            return finish(g)
        mix_gqa(g, l)
        if stop == "gqa":
            return finish(g)
        mix_lru(g, l)
        if stop == "lru":
            return finish(g)
        mix_rwkv(g, l)
        if stop == "rwkv":
            return finish(g)
        phase_merge(g, l, last)
        if stop == "merge":
            return finish(g)
        phase_moe(g, l, last, n_exp)
    epilogue(g)
    return finish(g)


def finish(g):
    g.k.finish()
    return g.nc


def mod(g, l, which, c, j):
    return g.modT[:, l, which * 16 + c, j:j + 1]


def prologue(g):
    k, nc, W = g.k, g.nc, g.W
    with ExitStack() as st:
        c2s = k.sb("c2s", [2, D], F32, st)
        scT = k.sb("scT", [128, 16, 2], F32, st)
        modtm = k.sb("modtm", [2, 12288], F32, st)
        bada = k.sb("bada", [2, 12288], F32, st)
        wst = [k.sb("wst%d" % i, [128, 16, 512], F32, st) for i in range(2)]
        rw = [R() for _ in range(2)]
        rc2, rscT, rmt, rba = R(), R(), R(), R()
        k.dma(c2s[:], g.c2, writes=[rc2])
        k.act(c2s[:], c2s[:], AF.Silu, reads=[rc2], writes=[rc2])
        pt, c0, rp = g.psb()
        for c in range(16):
            k.tr(pt[:, c0 + c * 2:c0 + c * 2 + 2], c2s[0:2, c * 128:(c + 1) * 128], g.ident[0:2, 0:2],
                 reads=[rc2, g.rconst], writes=[rp])
        k.cp(scT[:].rearrange("p c j -> p (c j)"), pt[:, c0:c0 + 32], [rp], [rscT])
        gi = 0
        for l in range(g.nl):
            k.dma(bada[:], W["b_ada"][l, :].partition_broadcast(2), writes=[rba])
            for grp in range(24):
                b = gi % 2
                gi += 1
                k.dma(wst[b][:], W["w_ada"][l, :, grp * 512:(grp + 1) * 512].rearrange("(c p) n -> p c n", p=128),
                      writes=[rw[b]])
                pt, c0, rp = g.psb()
                for c in range(16):
                    k.mm(pt[0:2, c0:c0 + 512], scT[:, c, :], wst[b][:, c, :], start=(c == 0), stop=(c == 15),
                         reads=[rscT, rw[b]], writes=[rp])
                k.tt(modtm[:, grp * 512:(grp + 1) * 512], pt[0:2, c0:c0 + 512], bada[:, grp * 512:(grp + 1) * 512],
                     ALU.add, reads=[rp, rba], writes=[rmt])
            pt, c0, rp = g.psb()
            for j in range(96):
                k.tr(pt[:, c0 + j * 2:c0 + j * 2 + 2], modtm[0:2, j * 128:(j + 1) * 128], g.ident[0:2, 0:2],
                     reads=[rmt, g.rconst], writes=[rp])
            k.cp(g.modT[:, l, :, :].rearrange("p c j -> p (c j)"), pt[:, c0:c0 + 192], [rp], [g.rmod])
            for which in (1, 4):
                sl = g.modT[:, l, which * 16:(which + 1) * 16, :]
                k.ts(sl, sl, 1.0, None, ALU.add, reads=[g.rmod], writes=[g.rmod])
        k.dma(g.modT_d, g.modT[:].rearrange("p l c j -> p (l c j)"), reads=[g.rmod])
        k.barrier()
    with ExitStack() as st:
        xt = [k.sb("xt%d" % i, [128, D], F32, st) for i in range(2)]
        xT = [k.sb("xT%d" % i, [128, 16, 128], F32, st) for i in range(2)]
        rxt = [R(), R()]
        rxT = [R(), R()]
        g.rxs = R("xs_d")
        for i in range(NT):
            b = i % 2
            k.dma(xt[b][:], g.xin[i * 128:(i + 1) * 128, :], writes=[rxt[b]])
            for q in range(4):
                pt, c0, rp = g.psb()
                for cc in range(4):
                    c = q * 4 + cc
                    k.tr(pt[:, c0 + cc * 128:c0 + (cc + 1) * 128], xt[b][:, c * 128:(c + 1) * 128], g.ident[:],
                         reads=[rxt[b], g.rconst], writes=[rp])
                dst = xT[b][:, q * 4:(q + 1) * 4, :].rearrange("p c t -> p (c t)")
                if q % 2 == 0:
                    k.cp(dst, pt[:, c0:c0 + 512], [rp], [rxT[b]])
                else:
                    k.cp(dst, pt[:, c0:c0 + 512], [rp], [rxT[b]], eng="act")
            k.dma(g.xs_d[:, :, i * 128:(i + 1) * 128].rearrange("c p t -> p c t"), xT[b][:], reads=[rxT[b]],
                  writes=[g.rxs])
        k.barrier()


def phase_inproj(g, l):
    k, nc, W = g.k, g.nc, g.W
    with ExitStack() as st:
        u1T = k.sb("u1T", [128, 16, T], BF16, st)
        ru1 = R("u1T")
        xb = [k.sb("xb%d" % i, [128, 16, 512], F32, st) for i in range(2)]
        rxb = [R(), R()]
        for bi, (t0, n) in enumerate(BLKS):
            j = 1 if t0 == 0 else 0
            b = bi % 2
            k.dma(xb[b][:, :, 0:n], g.xs_d[:, :, t0:t0 + n].rearrange("c p t -> p c t"), reads=[g.rxs],
                  writes=[rxb[b]])
            for c in range(16):
                k.act(u1T[:, c, t0:t0 + n], xb[b][:, c, 0:n], AF.Identity, bias=mod(g, l, 0, c, j),
                      scale=mod(g, l, 1, c, j), reads=[rxb[b], g.rmod], writes=[ru1])
        wbf = [k.sb("wbf%d" % i, [128, 16, 512], BF16, st) for i in range(2)]
        rwb = [R(), R()]
        stg = [k.sb("stg%d" % i, [128, 512], F32, st) for i in range(4)]
        rstg = [R() for _ in range(4)]
        g.rp_d = R("p_d")
        g.rvtm = R("vtm_d")
        win = W["w_in"]
        si = 0
        groups = [CH_NAMES[i:i + 4] for i in range(0, NCHUNK, 4)]
        for gi, grp in enumerate(groups):
            b = gi % 2
            for ci, name in enumerate(grp):
                off = 0
                for (c0_, w_) in CHUNKS[name]:
                    k.dma(wbf[b][:, :, ci * 128 + off:ci * 128 + off + w_],
                          win[l, :, c0_:c0_ + w_].rearrange("(c p) n -> p c n", p=128), writes=[rwb[b]], issuer="pool")
                    off += w_
            for (t0, n) in BLKS:
                for ci, name in enumerate(grp):
                    M = ch_width(name)
                    pt, c0, rp = g.psb()
                    for c in range(16):
                        k.mm(pt[0:M, c0:c0 + n], wbf[b][:, c, ci * 128:ci * 128 + M], u1T[:, c, t0:t0 + n],
                             start=(c == 0), stop=(c == 15), reads=[rwb[b], ru1], writes=[rp])
                    s = si % 4
                    si += 1
                    if si % 2:
                        k.cp(stg[s][0:M, 0:n], pt[0:M, c0:c0 + n], [rp], [rstg[s]])
                    else:
                        k.cp(stg[s][0:M, 0:n], pt[0:M, c0:c0 + n], [rp], [rstg[s]], eng="act")
                    k.dma(g.p_d[CH_ID[name], 0:M, t0:t0 + n], stg[s][0:M, 0:n], reads=[rstg[s]], writes=[g.rp_d])
        vst = [k.sb("vst%d" % i, [128, 640], BF16, st) for i in range(2)]
        rvst = [R(), R()]
        b = len(groups) % 2
        k.dma(wbf[b][:, :, 0:512], win[l, :, 1024:1536].rearrange("(c p) n -> p c n", p=128), writes=[rwb[b]],
              issuer="pool")
        b2 = 1 - b
        k.dma(wbf[b2][:, :, 0:128], win[l, :, 2688:2816].rearrange("(c p) n -> p c n", p=128), writes=[rwb[b2]],
              issuer="pool")
        for i in range(NT):
            vb = i % 2
            pt, c0, rp = g.psb()
            for c in range(16):
                k.mm(pt[:, c0:c0 + 512], u1T[:, c, i * 128:(i + 1) * 128], wbf[b][:, c, 0:512], start=(c == 0),
                     stop=(c == 15), reads=[rwb[b], ru1], writes=[rp])
            k.cp(vst[vb][:, 0:512], pt[:, c0:c0 + 512], [rp], [rvst[vb]])
            pt, c0, rp = g.psb()
            for c in range(16):
                k.mm(pt[:, c0:c0 + 128], u1T[:, c, i * 128:(i + 1) * 128], wbf[b2][:, c, 0:128], start=(c == 0),
                     stop=(c == 15), reads=[rwb[b2], ru1], writes=[rp])
            k.cp(vst[vb][:, 512:640], pt[:, c0:c0 + 128], [rp], [rvst[vb]], eng="act")
            k.dma(g.vtm_d[i * 128:(i + 1) * 128, :], vst[vb][:], reads=[rvst[vb]], writes=[g.rvtm])
        k.barrier()


def epilogue(g):
    k = g.k
    with ExitStack() as st:
        xb = [k.sb("exb%d" % i, [128, 16, 128], F32, st) for i in range(2)]
        ot = [k.sb("eot%d" % i, [128, D], F32, st) for i in range(2)]
        rxb = [R(), R()]
        rot = [R(), R()]
        for i in range(16):
            b = i % 2
            t0 = LC + i * 128
            k.dma(xb[b][:], g.xs_d[:, :, t0:t0 + 128].rearrange("c p t -> p c t"), reads=[g.rxs], writes=[rxb[b]])
            for q in range(4):
                pt, c0, rp = g.psb()
                for cc in range(4):
                    c = q * 4 + cc
                    k.tr(pt[:, c0 + cc * 128:c0 + (cc + 1) * 128], xb[b][:, c, :], g.ident[:],
                         reads=[rxb[b], g.rconst], writes=[rp])
                if q % 2 == 0:
                    k.cp(ot[b][:, q * 512:(q + 1) * 512], pt[:, c0:c0 + 512], [rp], [rot[b]])
                else:
                    k.cp(ot[b][:, q * 512:(q + 1) * 512], pt[:, c0:c0 + 512], [rp], [rot[b]], eng="act")
            k.dma(g.out[i * 128:(i + 1) * 128, :], ot[b][:], reads=[rot[b]])
        k.barrier()


SEGS = [(0, LC), (LC, T)]


def rev_ap(t, a, b, p0=0, p1=128, width=T):
    return bass.AP(t, p0 * width + (b - 1), [[width, p1 - p0], [-1, b - a]])


def small_consts(g, st):
    k = g.k
    if not hasattr(g, "_od128"):
        pass
    od = k.sb("od128", [128, 128], F32, st)
    r = R()
    k.memset(od[:], 1.0 / 128.0, writes=[r], eng="dve")
    return od, r


def rope(g, st_tiles, xT, rx, cosT, sinT, Pb, rtab, outb, rout):
    k = g.k
    xb16, rxb16, t1, rt1, t2, rt2 = st_tiles
    for bi, (t0, n) in enumerate(BLKS):
        b = bi % 2
        k.act(xb16[b][:, 0:n], xT[:, t0:t0 + n], AF.Copy, reads=[rx], writes=[rxb16[b]])
        pt, c0, rp = g.psb()
        k.mm(pt[:, c0:c0 + n], Pb[:], xb16[b][:, 0:n], reads=[rtab, rxb16[b]], writes=[rp])
        k.tt(t1[b][:, 0:n], xT[:, t0:t0 + n], cosT[:, t0:t0 + n], ALU.mult, reads=[rx, rtab], writes=[rt1[b]],
             eng="pool")
        k.tt(t2[b][:, 0:n], pt[:, c0:c0 + n], sinT[:, t0:t0 + n], ALU.mult, reads=[rp, rtab], writes=[rt2[b]])
        k.tt(outb[:, t0:t0 + n], t1[b][:, 0:n], t2[b][:, 0:n], ALU.add, reads=[rt1[b], rt2[b]], writes=[rout])


def rope_tiles(g, st, pfx):
    k = g.k
    xb16 = [k.sb(pfx + "xb16_%d" % i, [128, 512], BF16, st) for i in range(2)]
    t1 = [k.sb(pfx + "t1_%d" % i, [128, 512], F32, st) for i in range(2)]
    t2 = [k.sb(pfx + "t2_%d" % i, [128, 512], F32, st) for i in range(2)]
    return (xb16, [R(), R()], t1, [R(), R()], t2, [R(), R()])


def mix_ret(g, l):
    k, W, C = g.k, g.W, g.C
    with ExitStack() as st:
        cosT = k.sb("rcos", [128, T], F32, st)
        sinT = k.sb("rsin", [128, T], F32, st)
        Pf = k.sb("rPf", [128, 128], F32, st)
        Pb = k.sb("rPb", [128, 128], BF16, st)
        tab = k.sb("rtab", [128, 6, 128], F32, st)
        cj = k.sb("rcj", [128, 2], F32, st)
        lg = k.sb("rlg", [128, 8], F32, st)
        gnw = k.sb("rgnw", [128, 4], F32, st)
        gnb = k.sb("rgnb", [128, 4], F32, st)
        epsc = k.sb("repsc", [128, 1], F32, st)
        rtab = R()
        k.dma(cosT[:], C["ret_cos"], writes=[rtab])
        k.dma(sinT[:], C["ret_sin"], writes=[rtab])
        k.dma(Pf[:], C["ret_P"], writes=[rtab])
        k.dma(tab[:], C["ret_tab"], writes=[rtab])
        k.dma(cj[:], C["ret_cj"], writes=[rtab])
        k.dma(lg[:], W["ret_decay_logit"][l].rearrange("a b -> (a b)").partition_broadcast(128), writes=[rtab])
        k.dma(gnw[:], W["ret_gn_w"][l].rearrange("(h p) -> p h", p=128), writes=[rtab], allow_slow_non_contiguous=True)
        k.dma(gnb[:], W["ret_gn_b"][l].rearrange("(h p) -> p h", p=128), writes=[rtab], allow_slow_non_contiguous=True)
        k.cp(Pb[:], Pf[:], [rtab], [rtab])
        k.memset(epsc[:], 1e-5, writes=[rtab], eng="dve")
        k.act(lg[:], lg[:], AF.Exp, scale=-1.0, reads=[rtab], writes=[rtab])
        k.ts(lg[:], lg[:], 1.0, None, ALU.add, reads=[rtab], writes=[rtab])
        k.act(lg[:], lg[:], AF.Ln, reads=[rtab], writes=[rtab])
        k.ts(lg[:], lg[:], -1.0, None, ALU.mult, reads=[rtab], writes=[rtab])
        od, rod = small_consts(g, st)
        rt = rope_tiles(g, st, "r")
        qT = k.sb("rqT", [128, T], F32, st)
        kT = k.sb("rkT", [128, T], F32, st)
        gT = k.sb("rgT", [128, T], F32, st)
        qTr = k.sb("rqTr", [128, T], BF16, st)
        kTr = k.sb("rkTr", [128, T], BF16, st)
        sg = k.sb("rsg", [128, T], BF16, st)
        ktm = k.sb("rktm", [128, NT, 128], BF16, st)
        vtm = k.sb("rvtm", [128, NT, 128], BF16, st)
        Sall = [k.sb("rSall%d" % d, [128, NT, 128], BF16, st) for d in range(2)]
        S = k.sb("rS", [128, 128], F32, st)
        DT = k.sb("rDT", [128, 128], F32, st)
        tmpa = k.sb("rtmpa", [128, 128], F32, st)
        Gd = [k.sb("rG%d" % d, [128, 128], BF16, st) for d in range(2)]
        Gt = k.sb("rGt", [128, 128], F32, st)
        cdir = k.sb("rcdir", [128, 4], F32, st)
        kw = [k.sb("rkw%d" % i, [128, 128], BF16, st) for i in range(2)]
        Pm = [k.sb("rPm%d" % i, [128, 128], BF16, st) for i in range(2)]
        qw = [[k.sb("rqw%d_%d" % (d, i), [128, 128], BF16, st) for i in range(2)] for d in range(2)]
        y = k.sb("ry", [128, T], F32, st)
        hn = [k.sb("rhn%d" % i, [128, 512], F32, st) for i in range(4)]
        yo = [k.sb("ryo%d" % i, [128, 512], BF16, st) for i in range(2)]
        rq, rk, rg_, rqr, rkr, rsg, rktm, rvtm = [R() for _ in range(8)]
        rSall = [R(), R()]
        rS, rDT, rtmpa, rGt, rcd, ry = [R() for _ in range(6)]
        rG = [R(), R()]
        rkw = [R(), R()]
        rPm = [R(), R()]
        rqw = [[R(), R()], [R(), R()]]
        rhn = [R() for _ in range(4)]
        ryo = [R(), R()]
        g.rybr = getattr(g, "rybr", None) or R("ybr")
        for h in range(4):
            k.dma(qT[:], g.p_d[CH_ID["ret_q%d" % h]], reads=[g.rp_d], writes=[rq])
            k.dma(kT[:], g.p_d[CH_ID["ret_k%d" % h]], reads=[g.rp_d], writes=[rk])
            k.dma(gT[:], g.p_d[CH_ID["ret_g%d" % h]], reads=[g.rp_d], writes=[rg_])
            k.dma(vtm[:], g.vtm_d[:, h * 128:(h + 1) * 128].rearrange("(i p) e -> p i e", p=128), reads=[g.rvtm],
                  writes=[rvtm])
            rope(g, rt, qT, rq, cosT, sinT, Pb, rtab, qTr, rqr)
            rope(g, rt, kT, rk, cosT, sinT, Pb, rtab, kTr, rkr)
            k.act(sg[:], gT[:], AF.Silu, reads=[rg_], writes=[rsg])
            for i in range(NT):
                pt, c0, rp = g.psb()
                ptb = pt.bitcast(BF16)
                k.tr(ptb[:, 2 * c0:2 * c0 + 128], kTr[:, i * 128:(i + 1) * 128], g.identb[:], reads=[rkr, g.rconst],
                     writes=[rp])
                if i % 2:
                    k.cp(ktm[:, i, :], ptb[:, 2 * c0:2 * c0 + 128], [rp], [rktm])
                else:
                    k.cp(ktm[:, i, :], ptb[:, 2 * c0:2 * c0 + 128], [rp], [rktm], eng="act")
            lgf = lg[:, h:h + 1]
            lgb = lg[:, 4 + h:5 + h]
            k.act(DT[:], tab[:, 0, :], AF.Exp, scale=lgf, reads=[rtab], writes=[rDT])
            k.tt(DT[:], DT[:], tab[:, 2, :], ALU.mult, reads=[rDT, rtab], writes=[rDT])
            k.act(tmpa[:], tab[:, 1, :], AF.Exp, scale=lgb, reads=[rtab], writes=[rtmpa])
            k.tt(tmpa[:], tmpa[:], tab[:, 3, :], ALU.mult, reads=[rtmpa, rtab], writes=[rtmpa])
            k.tt(DT[:], DT[:], tmpa[:], ALU.add, reads=[rDT, rtmpa], writes=[rDT])
            for d in range(2):
                k.act(Gt[:], tab[:, 4 + d, :], AF.Exp, scale=(lgf if d == 0 else lgb), reads=[rtab], writes=[rGt])
                k.ts(Gd[d][:], Gt[:], 128.0 ** -0.5, None, ALU.mult, reads=[rGt], writes=[rG[d]])
                k.act(cdir[:, d:d + 1], cj[:, d:d + 1], AF.Exp, scale=(lgf if d == 0 else lgb), reads=[rtab],
                      writes=[rcd])
                k.act(cdir[:, 2 + d:3 + d], (lgf if d == 0 else lgb), AF.Exp, scale=128.0, reads=[rtab],
                      writes=[rcd])
            orders = [list(range(NT)), [1, 0] + list(range(NT - 1, 1, -1))]
            ci = 0
            for d in range(2):
                k.memset(S[:], 0.0, writes=[rS], eng="dve")
                order = orders[d]
                for oi, n in enumerate(order):
                    k.cp(Sall[d][:, n, :], S[:], [rS], [rSall[d]], eng="act")
                    if oi == len(order) - 1:
                        break
                    b = ci % 2
                    ci += 1
                    k.ts(kw[b][:], ktm[:, n, :], cdir[:, d:d + 1], None, ALU.mult, reads=[rktm, rcd],
                         writes=[rkw[b]], eng="pool")
                    pt, c0, rp = g.psb()
                    k.mm(pt[:, c0:c0 + 128], kw[b][:], vtm[:, n, :], reads=[rkw[b], rvtm], writes=[rp])
                    k.stt(S[:], S[:], cdir[:, 2 + d:3 + d], pt[:, c0:c0 + 128], ALU.mult, ALU.add,
                          reads=[rS, rcd, rp], writes=[rS])
            pt = None
            for n in range(NT):
                b = n % 2
                ts_ = slice(n * 128, (n + 1) * 128)
                ps_, cs_, rps = g.psb()
                k.mm(ps_[:, cs_:cs_ + 128], kTr[:, ts_], qTr[:, ts_], reads=[rkr, rqr], writes=[rps])
                k.tt(Pm[b][:], ps_[:, cs_:cs_ + 128], DT[:], ALU.mult, reads=[rps, rDT], writes=[rPm[b]])
                for d in range(2):
                    k.tt(qw[d][b][:], qTr[:, ts_], Gd[d][:], ALU.mult, reads=[rqr, rG[d]], writes=[rqw[d][b]],
                         eng="pool")
                if n % 4 == 0:
                    pt, c0, rp = g.psb()
                o = c0 + (n % 4) * 128
                k.mm(pt[:, o:o + 128], vtm[:, n, :], Pm[b][:], start=True, stop=False, reads=[rvtm, rPm[b]],
                     writes=[rp])
                k.mm(pt[:, o:o + 128], Sall[0][:, n, :], qw[0][b][:], start=False, stop=False,
                     reads=[rSall[0], rqw[0][b]], writes=[rp])
                k.mm(pt[:, o:o + 128], Sall[1][:, n, :], qw[1][b][:], start=False, stop=True,
                     reads=[rSall[1], rqw[1][b]], writes=[rp])
                if n % 4 == 3 or n == NT - 1:
                    n0 = (n // 4) * 4
                    w_ = (n - n0 + 1) * 128
                    k.cp(y[:, n0 * 128:n0 * 128 + w_], pt[:, c0:c0 + w_], [rp], [ry], eng="act")
            for bi, (t0, n) in enumerate(BLKS):
                b = bi % 2
                sl = slice(t0, t0 + n)
                p1, c1, rp1 = g.psb()
                k.mm(p1[:, c1:c1 + n], od[:], y[:, sl], reads=[rod, ry], writes=[rp1])
                k.act(hn[0][:, 0:n], y[:, sl], AF.Square, reads=[ry], writes=[rhn[0]])
                p2, c2, rp2 = g.psb()
                k.mm(p2[:, c2:c2 + n], od[:], hn[0][:, 0:n], reads=[rod, rhn[0]], writes=[rp2])
                k.cp(hn[1][:, 0:n], p1[:, c1:c1 + n], [rp1], [rhn[1]], eng="act")
                k.tt(hn[2][:, 0:n], hn[1][:, 0:n], hn[1][:, 0:n], ALU.mult, reads=[rhn[1]], writes=[rhn[2]],
                     eng="pool")
                k.tt(hn[2][:, 0:n], p2[:, c2:c2 + n], hn[2][:, 0:n], ALU.subtract, reads=[rp2, rhn[2]],
                     writes=[rhn[2]])
                k.act(hn[2][:, 0:n], hn[2][:, 0:n], AF.Sqrt, bias=epsc[:, 0:1], scale=1.0, reads=[rhn[2], rtab],
                      writes=[rhn[2]])
                k.op("dve", lambda e, o_=hn[2][:, 0:n]: e.reciprocal(o_, o_), [rhn[2]], [rhn[2]])
                k.tt(hn[3][:, 0:n], y[:, sl], hn[1][:, 0:n], ALU.subtract, reads=[ry, rhn[1]], writes=[rhn[3]])
                k.tt(hn[3][:, 0:n], hn[3][:, 0:n], hn[2][:, 0:n], ALU.mult, reads=[rhn[3], rhn[2]],
                     writes=[rhn[3]])
                k.act(hn[3][:, 0:n], hn[3][:, 0:n], AF.Identity, bias=gnb[:, h:h + 1], scale=gnw[:, h:h + 1],
                      reads=[rhn[3], rtab], writes=[rhn[3]])
                k.tt(yo[b][:, 0:n], hn[3][:, 0:n], sg[:, sl], ALU.mult, reads=[rhn[3], rsg], writes=[ryo[b]])
                k.dma(g.ybr_d[0 + h, :, sl], yo[b][:, 0:n], reads=[ryo[b]], writes=[g.rybr])
        k.barrier()


def mix_gqa(g, l):
    k, W, C = g.k, g.W, g.C
    with ExitStack() as st:
        cosT = k.sb("gcos", [128, T], F32, st)
        sinT = k.sb("gsin", [128, T], F32, st)
        Pf = k.sb("gPf", [128, 128], F32, st)
        Pb = k.sb("gPb", [128, 128], BF16, st)
        mkf = k.sb("gmkf", [128, 2, 128], F32, st)
        mk = k.sb("gmk", [128, 2, 128], BF16, st)
        esk = k.sb("gesk", [128, 8], F32, st)
        rtab = R()
        k.dma(cosT[:], C["gqa_cos"], writes=[rtab])
        k.dma(sinT[:], C["gqa_sin"], writes=[rtab])
        k.dma(Pf[:], C["gqa_P"], writes=[rtab])
        k.dma(mkf[:], C["gqa_mask"], writes=[rtab])
        k.dma(esk[:], W["gqa_sink"][l].partition_broadcast(128), writes=[rtab])
        k.cp(Pb[:], Pf[:], [rtab], [rtab])
        k.cp(mk[:], mkf[:], [rtab], [rtab])
        k.act(esk[:], esk[:], AF.Exp, reads=[rtab], writes=[rtab])
        rt = rope_tiles(g, st, "g")
        xT = [k.sb("gxT%d" % i, [128, T], F32, st) for i in range(2)]
        rx = [R(), R()]
        qTr = [k.sb("gqTr%d" % c, [128, T], BF16, st) for c in range(4)]
        rqr = [R() for _ in range(4)]
        K2T = [k.sb("gK2T%d" % c, [128, T], BF16, st) for c in range(2)]
        rk2 = [R(), R()]
        V2 = [k.sb("gV2%d" % c, [128, NT, 128], BF16, st) for c in range(2)]
        rv2 = [R(), R()]
        yg = [k.sb("gyg%d" % c, [128, T], BF16, st) for c in range(4)]
        ryg = [R() for _ in range(4)]
        E = [k.sb("gE%d" % i, [128, 640], BF16, st) for i in range(3)]
        rE = [R() for _ in range(3)]
        rd = [k.sb("grd%d" % i, [128, 128], F32, st) for i in range(2)]
        rrd = [R(), R()]
        xi = 0
        for c in range(4):
            b = xi % 2
            xi += 1
            k.dma(xT[b][:], g.p_d[CH_ID["gqa_q%d" % c]], reads=[g.rp_d], writes=[rx[b]])
            rope(g, rt, xT[b], rx[b], cosT, sinT, Pb, rtab, qTr[c], rqr[c])
        for c in range(2):
            b = xi % 2
            xi += 1
            k.dma(xT[b][:], g.p_d[CH_ID["gqa_k%d" % c]], reads=[g.rp_d], writes=[rx[b]])
            rope(g, rt, xT[b], rx[b], cosT, sinT, Pb, rtab, K2T[c], rk2[c])
            for hh in range(2):
                k.dma(V2[c][:, :, hh * 64:(hh + 1) * 64],
                      g.vtm_d[:, 512 + c * 64:512 + (c + 1) * 64].rearrange("(i p) e -> p i e", p=128),
                      reads=[g.rvtm], writes=[rv2[c]])
        it = 0
        for h in range(8):
            gk = h // 4
            c = h // 2
            hp = h % 2
            prt = slice(hp * 64, hp * 64 + 64)
            for qt in range(NT):
                if qt < 2:
                    keys = [(0, None), (1, None)]
                else:
                    keys = [(0, None), (1, None)]
                    for s in (qt - 1, qt, qt + 1):
                        if 2 <= s <= NT - 1:
                            keys.append((s, s - qt))
                nk = len(keys)
                qs = slice(qt * 128, (qt + 1) * 128)
                pd2, rpd = g.psd()
                for idx, (s, rel_) in enumerate(keys):
                    k.mm(pd2[:, idx * 128:(idx + 1) * 128], K2T[gk][prt, s * 128:(s + 1) * 128], qTr[c][prt, qs],
                         reads=[rk2[gk], rqr[c]], writes=[rpd[idx // 4]])
                e = it % 3
                it += 1
                k.act(E[e][:, 0:nk * 128], pd2[:, 0:nk * 128], AF.Exp, scale=0.125, reads=rpd, writes=[rE[e]])
                for idx, (s, rel_) in enumerate(keys):
                    if rel_ == -1 or rel_ == 1:
                        mi = 0 if rel_ == -1 else 1
                        k.tt(E[e][:, idx * 128:(idx + 1) * 128], E[e][:, idx * 128:(idx + 1) * 128], mk[:, mi, :],
                             ALU.mult, reads=[rE[e], rtab], writes=[rE[e]], eng="pool")
                pt, c0, rp = g.psb()
                for idx, (s, rel_) in enumerate(keys):
                    k.mm(pt[:, c0:c0 + 128], V2[gk][:, s, :], E[e][:, idx * 128:(idx + 1) * 128], start=(idx == 0),
                         stop=(idx == nk - 1), reads=[rv2[gk], rE[e]], writes=[rp])
                for idx, (s, rel_) in enumerate(keys):
                    k.mm(pt[:, c0 + 128:c0 + 256], g.onesb[:], E[e][:, idx * 128:(idx + 1) * 128],
                         start=(idx == 0), stop=(idx == nk - 1), reads=[g.rconst, rE[e]], writes=[rp])
                b = it % 2
                k.ts(rd[b][:], pt[:, c0 + 128:c0 + 256], esk[:, h:h + 1], None, ALU.add, reads=[rp, rtab],
                     writes=[rrd[b]])
                k.op("dve", lambda e_, o_=rd[b][:]: e_.reciprocal(o_, o_), [rrd[b]], [rrd[b]])
                k.tt(yg[c][prt, qs], pt[prt, c0:c0 + 128], rd[b][prt, :], ALU.mult, reads=[rp, rrd[b]],
                     writes=[ryg[c]])
        g.rybr = getattr(g, "rybr", None) or R("ybr")
        for c in range(4):
            k.dma(g.ybr_d[4 + c], yg[c][:], reads=[ryg[c]], writes=[g.rybr])
        k.barrier()


def mix_lru(g, l):
    k, W, C = g.k, g.W, g.C
    with ExitStack() as st:
        cw = k.sb("lcw", [128, 4, 4], F32, st)
        cb = k.sb("lcb", [128, 4], F32, st)
        gb = k.sb("lgb", [128, 2, 2, 4], F32, st)
        lm = k.sb("llm", [128, 2, 4], F32, st)
        sp16 = k.sb("lsp16", [128, 2, 4], F32, st)
        onec = k.sb("lonec", [128, 1], F32, st)
        bd = k.sb("lbd", [128, 16, 128], F32, st)
        bdb = k.sb("lbdb", [128, 16, 128], BF16, st)
        rc = R()
        rbd = R()
        for j_ in range(4):
            k.dma(cw[:, :, j_], W["lru_conv_w"][l, j_].rearrange("(c p) -> p c", p=128), writes=[rc],
                  allow_slow_non_contiguous=True)
        k.dma(cb[:], W["lru_conv_b"][l].rearrange("(c p) -> p c", p=128), writes=[rc],
              allow_slow_non_contiguous=True)
        for a_ in range(2):
            for b_ in range(2):
                k.dma(gb[:, a_, b_, :], W["lru_gate_b"][l, a_, b_].rearrange("(c p) -> p c", p=128), writes=[rc],
                      allow_slow_non_contiguous=True)
            k.dma(lm[:, a_, :], W["lru_lambda"][l, a_].rearrange("(c p) -> p c", p=128), writes=[rc],
                  allow_slow_non_contiguous=True)
        k.memset(onec[:], 1.0, writes=[rc], eng="dve")
        k.act(lm[:], lm[:], AF.Exp, scale=-1.0, reads=[rc], writes=[rc])
        k.ts(lm[:], lm[:], 1.0, None, ALU.add, reads=[rc], writes=[rc])
        k.act(lm[:], lm[:], AF.Ln, reads=[rc], writes=[rc])
        k.ts(sp16[:], lm[:], -16.0, None, ALU.mult, reads=[rc], writes=[rc])
        k.ts(lm[:], lm[:], -8.0, None, ALU.mult, reads=[rc], writes=[rc])
        k.memset(bd[:], 0.0, writes=[rbd], eng="dve")
        for d in range(2):
            for gt_ in range(2):
                for c in range(4):
                    idx = (d * 2 + gt_) * 4 + c
                    for hh in range(2):
                        k.dma(bd[hh * 64:(hh + 1) * 64, idx, hh * 64:(hh + 1) * 64],
                              W["lru_gate_w"][l, d, gt_, 2 * c + hh], writes=[rbd])
        k.cp(bdb[:], bd[:], [rbd], [rbd])
        x = k.sb("lx", [128, T], F32, st)
        gt = k.sb("lgt", [128, T], F32, st)
        xc = k.sb("lxc", [128, T], F32, st)
        xcb = k.sb("lxcb", [128, T], BF16, st)
        rg = k.sb("lrg", [128, T], F32, st)
        ig = k.sb("lig", [128, T], F32, st)
        aa = k.sb("laa", [128, T], F32, st)
        bt = k.sb("lbt", [128, T], F32, st)
        hh_ = [k.sb("lh%d" % d, [128, T], F32, st) for d in range(2)]
        yo = k.sb("lyo", [128, T], BF16, st)
        rx, rgt, rxc, rxcb, rrg, rig, raa, rbt, ryo = [R() for _ in range(9)]
        rh = [R(), R()]
        g.rybr = getattr(g, "rybr", None) or R("ybr")
        for c in range(4):
            k.dma(x[:], g.p_d[CH_ID["lru_x%d" % c]], reads=[g.rp_d], writes=[rx])
            k.dma(gt[:], g.p_d[CH_ID["lru_g%d" % c]], reads=[g.rp_d], writes=[rgt])
            for (a, b) in SEGS:
                k.ts(xc[:, a:b], x[:, a:b], cw[:, c, 2:3], cb[:, c:c + 1], ALU.mult, ALU.add, reads=[rx, rc],
                     writes=[rxc])
                k.stt(xc[:, a + 2:b], x[:, a:b - 2], cw[:, c, 0:1], xc[:, a + 2:b], ALU.mult, ALU.add,
                      reads=[rx, rc, rxc], writes=[rxc])
                k.stt(xc[:, a + 1:b], x[:, a:b - 1], cw[:, c, 1:2], xc[:, a + 1:b], ALU.mult, ALU.add,
                      reads=[rx, rc, rxc], writes=[rxc])
                k.stt(xc[:, a:b - 1], x[:, a + 1:b], cw[:, c, 3:4], xc[:, a:b - 1], ALU.mult, ALU.add,
                      reads=[rx, rc, rxc], writes=[rxc])
            k.act(xcb[:], xc[:], AF.Copy, reads=[rxc], writes=[rxcb])
            for d in range(2):
                for (t0, n) in BLKS:
                    sl = slice(t0, t0 + n)
                    p1, c1, rp1 = g.psb()
                    k.mm(p1[:, c1:c1 + n], bdb[:, (d * 2 + 0) * 4 + c, :], xcb[:, sl], reads=[rbd, rxcb],
                         writes=[rp1])
                    k.act(rg[:, sl], p1[:, c1:c1 + n], AF.Sigmoid, bias=gb[:, d, 0, c:c + 1], scale=1.0,
                          reads=[rp1, rc], writes=[rrg])
                    p2, c2, rp2 = g.psb()
                    k.mm(p2[:, c2:c2 + n], bdb[:, (d * 2 + 1) * 4 + c, :], xcb[:, sl], reads=[rbd, rxcb],
                         writes=[rp2])
                    k.act(ig[:, sl], p2[:, c2:c2 + n], AF.Sigmoid, bias=gb[:, d, 1, c:c + 1], scale=1.0,
                          reads=[rp2, rc], writes=[rig])
                k.act(aa[:], rg[:], AF.Exp, scale=lm[:, d, c:c + 1], reads=[rrg, rc], writes=[raa])
                k.act(bt[:], rg[:], AF.Exp, scale=sp16[:, d, c:c + 1], reads=[rrg, rc], writes=[rbt])
                k.act(bt[:], bt[:], AF.Sqrt, bias=onec[:, 0:1], scale=-1.0, reads=[rbt, rc], writes=[rbt])
                k.tt(ig[:], ig[:], xc[:], ALU.mult, reads=[rig, rxc], writes=[rig], eng="pool")
                k.tt(bt[:], bt[:], ig[:], ALU.mult, reads=[rbt, rig], writes=[rbt])
                h_ = hh_[d]
                if d == 0:
                    k.op("dve", lambda e, o_=h_[:], a_=aa[:], b_=bt[:]: e.tensor_tensor_scan(o_, a_, b_, 0.0, ALU.mult, ALU.add),
                         [raa, rbt], [rh[d]])
                else:
                    k.op("dve", lambda e, o_=rev_ap(h_, 0, LC), a_=rev_ap(aa, 0, LC), b_=rev_ap(bt, 0, LC):
                         e.tensor_tensor_scan(o_, a_, b_, 0.0, ALU.mult, ALU.add), [raa, rbt], [rh[d]])
                    k.op("dve", lambda e, o_=rev_ap(h_, LC, T), a_=rev_ap(aa, LC, T), b_=rev_ap(bt, LC, T), i_=h_[:, 0:1]:
                         e.tensor_tensor_scan(o_, a_, b_, i_, ALU.mult, ALU.add), [raa, rbt, rh[d]], [rh[d]])
            k.act(gt[:], gt[:], AF.Gelu, reads=[rgt], writes=[rgt])
            k.tt(hh_[0][:], hh_[0][:], hh_[1][:], ALU.add, reads=[rh[0], rh[1]], writes=[rh[0]], eng="pool")
            k.tt(yo[:], hh_[0][:], gt[:], ALU.mult, reads=[rh[0], rgt], writes=[ryo])
            k.dma(g.ybr_d[12 + c], yo[:], reads=[ryo], writes=[g.rybr])
        k.barrier()


DECAY_SCALE = math.exp(-0.5)
RW_Q = ["r", "k", "kk", "b", "v", "lwf", "lwb", "g"]
MU_COLS = [(c * 128, 128) for c in range(12)] + [(1536, 96), (1632, 96), (1728, 96), (1824, 128), (1952, 128)]


def shift_mix(g, x, rx, tmp, rtmp, out, rout, M, om, hm, rmu):
    k = g.k
    for (a, b) in SEGS:
        k.cp(tmp[0:M, a:b - 1], x[0:M, a + 1:b], [rx], [rtmp], eng="pool")
        k.memset(tmp[0:M, b - 1:b], 0.0, writes=[rtmp], eng="pool")
        k.tt(tmp[0:M, a + 1:b], tmp[0:M, a + 1:b], x[0:M, a:b - 1], ALU.add, reads=[rtmp, rx], writes=[rtmp])
    k.ts(out[0:M, :], x[0:M, :], om, None, ALU.mult, reads=[rx, rmu], writes=[rout])
    k.stt(out[0:M, :], tmp[0:M, :], hm, out[0:M, :], ALU.mult, ALU.add, reads=[rtmp, rmu, rout], writes=[rout])


def dv(arr, d, seg, p0=0, p1=128):
    a, b = SEGS[seg]
    nch = (b - a) // C_RW
    if d == 0:
        return arr[p0:p1, a:b].rearrange("p (n c) -> p n c", c=C_RW)
    return bass.AP(arr, p0 * T + (b - 1), [[T, p1 - p0], [-C_RW, nch], [-1, C_RW]])


def chs(seg):
    a, b = SEGS[seg]
    return slice(a // C_RW, b // C_RW)


def mix_rwkv(g, l):
    rwkv_prep(g, l)
    rwkv_scan(g, l)


def rwkv_prep(g, l):
    k, W, C = g.k, g.W, g.C
    with ExitStack() as st:
        mu = k.sb("wmu", [128, 17], F32, st)
        om = k.sb("wom", [128, 17], F32, st)
        hm = k.sb("whm", [128, 17], F32, st)
        rmu = R()
        k.memset(mu[:], 0.0, writes=[rmu], eng="dve")
        for i, (c0, w) in enumerate(MU_COLS):
            k.dma(mu[0:w, i:i + 1], W["rwkv_mu"][l, c0:c0 + w].rearrange("(p o) -> p o", o=1), writes=[rmu])
        k.ts(om[:], mu[:], -1.0, 1.0, ALU.mult, ALU.add, reads=[rmu], writes=[rmu])
        k.ts(hm[:], mu[:], 0.5, None, ALU.mult, reads=[rmu], writes=[rmu])
        par = k.sb("wpar", [128, 8, 4], F32, st)
        rpar = R()
        srcs = [W["rwkv_w0"][l, 0], W["rwkv_w0"][l, 1], W["rwkv_a0"][l], W["rwkv_k_k"][l], W["rwkv_k_a"][l]]
        for i, s in enumerate(srcs):
            k.dma(par[:, i, :], s.rearrange("(c p) -> p c", p=128), writes=[rpar], allow_slow_non_contiguous=True)
        k.ts(par[:, 5, :], par[:, 4, :], -1.0, 1.0, ALU.mult, ALU.add, reads=[rpar], writes=[rpar])
        wup = k.sb("wwup", [96, 2, 512], BF16, st)
        aup = k.sb("waup", [96, 512], BF16, st)
        gup = k.sb("wgup", [128, 2, 512], BF16, st)
        bo = k.sb("wbo", [128, 128], F32, st)
        rw = R()
        for d in range(2):
            k.dma(wup[:, d, :], W["rwkv_w_up"][l, d], writes=[rw], issuer="pool")
        k.dma(aup[:], W["rwkv_a_up"][l], writes=[rw], issuer="pool")
        k.dma(gup[:], W["rwkv_g_up"][l].rearrange("(c p) n -> p c n", p=128), writes=[rw], issuer="pool")
        k.dma(bo[:], C["blk_ones"], writes=[rw])
        x = k.sb("wx", [128, T], F32, st)
        tmp = k.sb("wtmp", [128, T], F32, st)
        xs_ = k.sb("wxs", [128, T], F32, st)
        rx, rtmp, rxs = R(), R(), R()
        twd = [k.sb("wtwd%d" % d, [96, T], BF16, st) for d in range(2)]
        adb = k.sb("wadb", [96, T], BF16, st)
        sgd = k.sb("wsgd", [128, 2, T], BF16, st)
        rlo = R()
        for i, (name, M) in enumerate([("rw_wdf", 96), ("rw_wdb", 96), ("rw_ad", 96), ("rw_gd0", 128), ("rw_gd1", 128)]):
            mi = 12 + i
            k.dma(x[0:M, :], g.p_d[CH_ID[name], 0:M, :], reads=[g.rp_d], writes=[rx])
            shift_mix(g, x, rx, tmp, rtmp, xs_, rxs, M, om[0:M, mi:mi + 1], hm[0:M, mi:mi + 1], rmu)
            if i < 2:
                k.act(twd[i][:], xs_[0:96, :], AF.Tanh, reads=[rxs], writes=[rlo])
            elif i == 2:
                k.act(adb[:], xs_[0:96, :], AF.Copy, reads=[rxs], writes=[rlo])
            else:
                k.act(sgd[:, i - 3, :], xs_[:], AF.Sigmoid, reads=[rxs], writes=[rlo])
        names = ["r", "k", "v", "lwf", "lwb", "a", "gq", "kk", "t"]
        A = {n: k.sb("wA_" + n, [128, T], F32, st) for n in names}
        RA = {n: R() for n in names}
        g.rrw = getattr(g, "rrw", None) or R("rw_d")
        for c in range(4):
            for qi, (nm, pfx) in enumerate([("r", "rw_r"), ("k", "rw_k"), ("v", "rw_v")]):
                mi = qi * 4 + c
                k.dma(x[:], g.p_d[CH_ID["%s%d" % (pfx, c)]], reads=[g.rp_d], writes=[rx])
                shift_mix(g, x, rx, tmp, rtmp, A[nm], RA[nm], 128, om[:, mi:mi + 1], hm[:, mi:mi + 1], rmu)
            cs_ = slice(c * 128, (c + 1) * 128)
            for (t0, n) in BLKS:
                sl = slice(t0, t0 + n)
                for d in range(2):
                    pt, c0, rp = g.psb()
                    k.mm(pt[:, c0:c0 + n], wup[:, d, cs_], twd[d][:, sl], reads=[rw, rlo], writes=[rp])
                    nm = "lwf" if d == 0 else "lwb"
                    k.act(A[nm][:, sl], pt[:, c0:c0 + n], AF.Sigmoid, bias=par[:, d, c:c + 1], scale=1.0,
                          reads=[rp, rpar], writes=[RA[nm]])
                pt, c0, rp = g.psb()
                k.mm(pt[:, c0:c0 + n], aup[:, cs_], adb[:, sl], reads=[rw, rlo], writes=[rp])
                k.act(A["a"][:, sl], pt[:, c0:c0 + n], AF.Sigmoid, bias=par[:, 2, c:c + 1], scale=1.0,
                      reads=[rp, rpar], writes=[RA["a"]])
                pt, c0, rp = g.psb()
                k.mm(pt[:, c0:c0 + n], gup[:, 0, cs_], sgd[:, 0, sl], start=True, stop=False, reads=[rw, rlo],
                     writes=[rp])
                k.mm(pt[:, c0:c0 + n], gup[:, 1, cs_], sgd[:, 1, sl], start=False, stop=True, reads=[rw, rlo],
                     writes=[rp])
                k.cp(A["gq"][:, sl], pt[:, c0:c0 + n], [rp], [RA["gq"]])
            for nm in ("lwf", "lwb"):
                k.ts(A[nm][:], A[nm][:], -DECAY_SCALE, None, ALU.mult, reads=[RA[nm]], writes=[RA[nm]], eng="pool")
            k.ts(A["kk"][:], A["k"][:], par[:, 3, c:c + 1], None, ALU.mult, reads=[RA["k"], rpar], writes=[RA["kk"]])
            k.act(A["t"][:], A["kk"][:], AF.Square, reads=[RA["kk"]], writes=[RA["t"]])
            for (t0, n) in BLKS:
                sl = slice(t0, t0 + n)
                pt, c0, rp = g.psb()
                k.mm(pt[:, c0:c0 + n], bo[:], A["t"][:, sl], reads=[rw, RA["t"]], writes=[rp])
                k.act(tmp[:, sl], pt[:, c0:c0 + n], AF.Sqrt, reads=[rp], writes=[rtmp])
            k.ts(tmp[:], tmp[:], 1e-12, None, ALU.max, reads=[rtmp], writes=[rtmp])
            k.op("dve", lambda e, o_=tmp[:]: e.reciprocal(o_, o_), [rtmp], [rtmp])
            k.tt(A["kk"][:], A["kk"][:], tmp[:], ALU.mult, reads=[RA["kk"], rtmp], writes=[RA["kk"]])
            k.ts(A["t"][:], A["a"][:], par[:, 4, c:c + 1], par[:, 5, c:c + 1], ALU.mult, ALU.add,
                 reads=[RA["a"], rpar, RA["t"]], writes=[RA["t"]])
            k.tt(A["k"][:], A["k"][:], A["t"][:], ALU.mult, reads=[RA["k"], RA["t"]], writes=[RA["k"]], eng="pool")
            k.tt(A["a"][:], A["a"][:], A["kk"][:], ALU.mult, reads=[RA["a"], RA["kk"]], writes=[RA["a"]])
            for qi, nm in enumerate(["r", "k", "kk", "a", "v", "lwf", "lwb", "gq"]):
                k.dma(g.rw_d[qi, c], A[nm][:], reads=[RA[nm]], writes=[g.rrw])
        k.barrier()


def rwkv_scan(g, l):
    k, W, C = g.k, g.W, g.C
    with ExitStack() as st:
        mkf = k.sb("smkf", [128, 5, 128], F32, st)
        mk = k.sb("smk", [128, 5, 128], BF16, st)
        ist = k.sb("sist", [128, 64], F32, st)
        istb = k.sb("sistb", [128, 64], BF16, st)
        bo64 = k.sb("sbo64", [128, 128], F32, st)
        bo = k.sb("sbo", [128, 128], F32, st)
        par = k.sb("spar", [128, 3, 4], F32, st)
        epsc = k.sb("sepsc", [128, 1], F32, st)
        rcs = R()
        k.dma(mkf[:], C["rw_mask"], writes=[rcs])
        k.dma(ist[:], C["ist"], writes=[rcs])
        k.dma(bo[:], C["blk_ones"], writes=[rcs])
        k.cp(mk[:], mkf[:], [rcs], [rcs])
        k.cp(istb[:], ist[:], [rcs], [rcs])
        k.ts(bo64[:], bo[:], 1.0 / 64.0, None, ALU.mult, reads=[rcs], writes=[rcs])
        k.memset(epsc[:], 64e-5, writes=[rcs], eng="dve")
        for i, s in enumerate([W["rwkv_ln_w"][l], W["rwkv_ln_b"][l], W["rwkv_r_k"][l].rearrange("h d -> (h d)")]):
            k.dma(par[:, i, :], s.rearrange("(c p) -> p c", p=128), writes=[rcs], allow_slow_non_contiguous=True)
        nat = {n: k.sb("sN_" + n, [128, T], F32, st) for n in ["r", "k", "kk", "b", "v", "lw"]}
        rnat = {n: R() for n in nat}
        yd = [k.sb("syd%d" % d, [128, T], F32, st) for d in range(2)]
        ryd = [R(), R()]
        cum = k.sb("scum", [128, NCH, C_RW], F32, st)
        lwd = k.sb("slwd", [128, NCH, C_RW], F32, st)
        c0t = k.sb("sc0", [128, NCH], F32, st)
        eL = k.sb("seL", [128, NCH, C_RW], F32, st)
        eLx = k.sb("seLx", [128, NCH, C_RW], F32, st)
        rcum, rlwd, rc0, reL, reLx = [R() for _ in range(5)]
        enL, renL = cum, rcum
        tq, rtq = lwd, rlwd
        BD = {n: k.sb("sBD_" + n, [128, NCH, 128], BF16, st) for n in ["R", "A", "B", "K", "V", "BH", "KH"]}
        rBD = {n: R() for n in BD}
        for n in BD:
            k.memset(BD[n][:], 0.0, writes=[rBD[n]], eng="pool")
        Ybd = [k.sb("sYbd%d" % i, [128, 128], F32, st) for i in range(2)]
        rYbd = [R(), R()]
        for i in range(2):
            k.memset(Ybd[i][:], 0.0, writes=[rYbd[i]], eng="pool")
        H = k.sb("sH", [128, 64], F32, st)
        Hb = k.sb("sHb", [128, 64], BF16, st)
        rH, rHb = R(), R()
        NB = 3
        M2 = [k.sb("sM2_%d" % i, [128, 256], BF16, st) for i in range(NB * 2)]
        rM2 = [R() for _ in range(NB * 2)]
        XT = [k.sb("sXT_%d" % i, [128, 128], BF16, st) for i in range(NB * 2)]
        rXT = [R() for _ in range(NB * 2)]
        A3 = [k.sb("sA3_%d" % i, [128, 384], BF16, st) for i in range(NB)]
        rA3 = [R() for _ in range(NB)]
        Vst = [k.sb("sVst_%d" % i, [128, 64], BF16, st) for i in range(NB)]
        rVst = [R() for _ in range(NB)]
        BK = [k.sb("sBK_%d" % i, [128, 256], BF16, st) for i in range(NB)]
        rBK = [R() for _ in range(NB)]
        Bm = [k.sb("sBm_%d" % i, [128, 64], BF16, st) for i in range(2)]
        rBm = [R(), R()]
        Ub = [k.sb("sUb_%d" % i, [128, 64], BF16, st) for i in range(2)]
        rUb = [R(), R()]
        hn = [k.sb("shn%d" % i, [128, 512], F32, st) for i in range(4)]
        rhn = [R() for _ in range(4)]
        gq = eLx[:].rearrange("p n c -> p (n c)")
        rgq = reLx
        yo = [k.sb("syo%d" % i, [128, 512], BF16, st) for i in range(2)]
        ryo = [R(), R()]
        g.rybr = getattr(g, "rybr", None) or R("ybr")
        ev = 0
        for c in range(4):
            for qi, nm in enumerate(["r", "k", "kk", "b", "v"]):
                k.dma(nat[nm][:], g.rw_d[qi, c], reads=[g.rrw], writes=[rnat[nm]])
            for d in range(2):
                lw = nat["lw"]
                rlw = rnat["lw"]
                k.dma(lw[:], g.rw_d[5 + d, c], reads=[g.rrw], writes=[rlw])
                cumf = cum[:].rearrange("p n c -> p (n c)")
                for seg in range(2):
                    a, b = SEGS[seg]
                    k.cp(lwd[:, chs(seg), :], dv(lw, d, seg), [rlw], [rlwd], eng="pool")
                lwdf = lwd[:].rearrange("p n c -> p (n c)")
                k.op("dve", lambda e, o_=cumf, a_=g.ones[:, 0:1].to_broadcast([128, T]), b_=lwdf:
                     e.tensor_tensor_scan(o_, a_, b_, 0.0, ALU.mult, ALU.add), [rlwd, g.rconst], [rcum])
                k.tt(c0t[:], cum[:, :, 0], lwd[:, :, 0], ALU.subtract, reads=[rcum, rlwd], writes=[rc0])
                c0b = c0t[:].rearrange("p (n o) -> p n o", o=1).to_broadcast([128, NCH, C_RW])
                k.tt(cum[:], cum[:], c0b, ALU.subtract, reads=[rcum, rc0], writes=[rcum])
                k.tt(lwd[:], cum[:], lwd[:], ALU.subtract, reads=[rcum, rlwd], writes=[rlwd], eng="pool")
                k.act(eL[:], cum[:], AF.Exp, reads=[rcum], writes=[reL])
                k.act(eLx[:], lwd[:], AF.Exp, reads=[rlwd], writes=[reLx])
                k.act(cum[:], cum[:], AF.Exp, scale=-1.0, reads=[rcum], writes=[rcum])
                WCb = eL[:, :, C_RW - 1:C_RW].to_broadcast([128, NCH, C_RW])
                for seg in range(2):
                    cs_ = chs(seg)
                    for hh in range(2):
                        p0, p1 = hh * 64, hh * 64 + 64
                        ps_ = slice(p0, p1)
                        k.tt(BD["R"][ps_, cs_, ps_], dv(nat["r"], d, seg, p0, p1), eL[ps_, cs_, :], ALU.mult,
                             reads=[rnat["r"], reL], writes=[rBD["R"]])
                        k.stt(BD["A"][ps_, cs_, ps_], dv(nat["kk"], d, seg, p0, p1), -1.0, eLx[ps_, cs_, :],
                              ALU.mult, ALU.mult, reads=[rnat["kk"], reLx], writes=[rBD["A"]])
                        k.cp(BD["V"][ps_, cs_, ps_], dv(nat["v"], d, seg, p0, p1), [rnat["v"]], [rBD["V"]],
                             eng="act")
                    for (src, nb, nh) in (("b", "B", "BH"), ("k", "K", "KH")):
                        k.tt(tq[:, cs_, :], dv(nat[src], d, seg), enL[:, cs_, :], ALU.mult, reads=[rnat[src], renL],
                             writes=[rtq])
                        for hh in range(2):
                            ps_ = slice(hh * 64, hh * 64 + 64)
                            k.cp(BD[nb][ps_, cs_, ps_], tq[ps_, cs_, :], [rtq], [rBD[nb]], eng="act")
                            k.tt(BD[nh][ps_, cs_, ps_], tq[ps_, cs_, :], WCb[ps_, cs_, :], ALU.mult,
                                 reads=[rtq, reL], writes=[rBD[nh]], eng="pool")
                k.memset(H[:], 0.0, writes=[rH], eng="dve")
                k.memset(Hb[:], 0.0, writes=[rHb], eng="dve")
                fin = {}
                def par_part(n):
                        i3 = n % NB
                        Ab, Bb, Kb, Rb = BD["A"][:, n, :], BD["B"][:, n, :], BD["K"][:, n, :], BD["R"][:, n, :]
                        Vb, BHb, KHb = BD["V"][:, n, :], BD["BH"][:, n, :], BD["KH"][:, n, :]
                        pa, ca, rpa = g.psb()
                        k.mm(pa[:, ca:ca + 128], Ab, Bb, reads=[rBD["A"], rBD["B"]], writes=[rpa])
                        k.mm(pa[:, ca + 128:ca + 256], Bb, Ab, reads=[rBD["A"], rBD["B"]], writes=[rpa])
                        mi = (n % NB) * 2
                        k.tt(M2[mi][:], pa[:, ca:ca + 256], mk[:, 0:2, :].rearrange("p a b -> p (a b)"), ALU.mult,
                             reads=[rpa, rcs], writes=[rM2[mi]])
                        xi = (n % NB) * 2
                        k.tt(XT[xi][:], M2[mi][:, 128:256], g.identb[:], ALU.add, reads=[rM2[mi], g.rconst],
                             writes=[rXT[xi]], eng="pool")
                        curM, rcurM, curX, rcurX = M2[mi], rM2[mi], XT[xi], rXT[xi]
                        for s in range(5):
                            nm_, rnm_ = (M2[mi + 1], rM2[mi + 1]) if curM is M2[mi] else (M2[mi], rM2[mi])
                            nx_, rnx_ = (XT[xi + 1], rXT[xi + 1]) if curX is XT[xi] else (XT[xi], rXT[xi])
                            pm, cm, rpm = g.psb()
                            k.mm(pm[:, cm:cm + 128], curM[:, 128:256], curM[:, 0:128], reads=[rcurM], writes=[rpm])
                            wcols = 128
                            if s < 4:
                                k.mm(pm[:, cm + 128:cm + 256], curM[:, 0:128], curM[:, 128:256], reads=[rcurM],
                                     writes=[rpm])
                                wcols = 256
                            k.act(nm_[:, 0:wcols], pm[:, cm:cm + wcols], AF.Copy, reads=[rpm], writes=[rnm_])
                            px, cx, rpx = g.psb()
                            k.mm(px[:, cx:cx + 128], nm_[:, 0:128], curX[:], reads=[rnm_, rcurX], writes=[rpx])
                            k.tt(nx_[:], px[:, cx:cx + 128], curX[:], ALU.add, reads=[rpx, rcurX], writes=[rnx_])
                            curM, rcurM, curX, rcurX = nm_, rnm_, nx_, rnx_
                        pb, cb_, rpb = g.psb()
                        k.mm(pb[:, cb_:cb_ + 128], Kb, Ab, reads=[rBD["K"], rBD["A"]], writes=[rpb])
                        k.mm(pb[:, cb_ + 128:cb_ + 256], Bb, Rb, reads=[rBD["B"], rBD["R"]], writes=[rpb])
                        k.mm(pb[:, cb_ + 256:cb_ + 384], Kb, Rb, reads=[rBD["K"], rBD["R"]], writes=[rpb])
                        k.tt(A3[i3][:], pb[:, cb_:cb_ + 384], mk[:, 2:5, :].rearrange("p a b -> p (a b)"), ALU.mult,
                             reads=[rpb, rcs], writes=[rA3[i3]])
                        pv, cv, rpv = g.psb()
                        k.mm(pv[:, cv:cv + 64], Vb, istb[:], reads=[rBD["V"], rcs], writes=[rpv])
                        k.act(Vst[i3][:], pv[:, cv:cv + 64], AF.Copy, reads=[rpv], writes=[rVst[i3]])
                        ptt, ct, rpt = g.psb()
                        ptb = ptt.bitcast(BF16)
                        k.tr(ptb[:, 2 * ct:2 * ct + 128], BHb, g.identb[:], reads=[rBD["BH"], g.rconst], writes=[rpt])
                        k.tr(ptb[:, 2 * ct + 128:2 * ct + 256], KHb, g.identb[:], reads=[rBD["KH"], g.rconst],
                             writes=[rpt])
                        k.act(BK[i3][:], ptb[:, 2 * ct:2 * ct + 256], AF.Copy, reads=[rpt], writes=[rBK[i3]])
                        fin[n] = (curX, rcurX)
                def seq_part(n):
                        i3 = n % NB
                        Ab, Rb = BD["A"][:, n, :], BD["R"][:, n, :]
                        curX, rcurX = fin.pop(n)
                        b2 = n % 2
                        p1_, c1, rp1 = g.psb()
                        k.mm(p1_[:, c1:c1 + 64], Ab, Hb[:], start=True, stop=False, reads=[rBD["A"], rHb], writes=[rp1])
                        k.mm(p1_[:, c1:c1 + 64], A3[i3][:, 0:128], Vst[i3][:], start=False, stop=True,
                             reads=[rA3[i3], rVst[i3]], writes=[rp1])
                        k.act(Bm[b2][:], p1_[:, c1:c1 + 64], AF.Copy, reads=[rp1], writes=[rBm[b2]])
                        p2_, c2, rp2 = g.psb()
                        k.mm(p2_[:, c2:c2 + 64], curX[:], Bm[b2][:], reads=[rcurX, rBm[b2]], writes=[rp2])
                        k.cp(Ub[b2][:], p2_[:, c2:c2 + 64], [rp2], [rUb[b2]])
                        py, cy, rpy = g.psb()
                        k.mm(py[:, cy:cy + 64], Rb, Hb[:], start=True, stop=False, reads=[rBD["R"], rHb], writes=[rpy])
                        k.mm(py[:, cy:cy + 64], A3[i3][:, 128:256], Ub[b2][:], start=False, stop=False,
                             reads=[rA3[i3], rUb[b2]], writes=[rpy])
                        k.mm(py[:, cy:cy + 64], A3[i3][:, 256:384], Vst[i3][:], start=False, stop=True,
                             reads=[rA3[i3], rVst[i3]], writes=[rpy])
                        k.cp(Ybd[b2][0:64, 0:64], py[0:64, cy:cy + 64], [rpy], [rYbd[b2]], eng="act")
                        k.cp(Ybd[b2][64:128, 64:128], py[64:128, cy:cy + 64], [rpy], [rYbd[b2]], eng="act")
                        ph, ch_, rph = g.psb()
                        k.mm(ph[:, ch_:ch_ + 64], BK[i3][:, 0:128], Ub[b2][:], start=True, stop=False,
                             reads=[rBK[i3], rUb[b2]], writes=[rph])
                        k.mm(ph[:, ch_:ch_ + 64], BK[i3][:, 128:256], Vst[i3][:], start=False, stop=True,
                             reads=[rBK[i3], rVst[i3]], writes=[rph])
                        k.stt(H[:], H[:], eL[:, n, C_RW - 1:C_RW], ph[:, ch_:ch_ + 64], ALU.mult, ALU.add,
                              reads=[rH, reL, rph], writes=[rH])
                        k.cp(Hb[:], H[:], [rH], [rHb])
                        po, co, rpo = g.psb()
                        k.mm(po[:, co:co + 64], Ybd[b2][:], ist[:], reads=[rYbd[b2], rcs], writes=[rpo])
                        if d == 0:
                            dst = yd[d][:, n * 64:(n + 1) * 64]
                        elif n < 4:
                            dst = rev_ap(yd[d], LC - (n + 1) * 64, LC - n * 64)
                        else:
                            dst = rev_ap(yd[d], T - (n - 3) * 64, T - (n - 4) * 64)
                        k.cp(dst, po[:, co:co + 64], [rpo], [ryd[d]], eng="pool" if False else "dve")
                par_part(0)
                for n in range(NCH):
                    if n + 1 < NCH:
                        par_part(n + 1)
                    seq_part(n)
            k.dma(gq, g.rw_d[7, c], reads=[g.rrw], writes=[rgq])
            k.tt(yd[0][:], yd[0][:], yd[1][:], ALU.add, reads=[ryd[0], ryd[1]], writes=[ryd[0]], eng="pool")
            k.stt(yd[1][:], nat["r"][:], par[:, 2, c:c + 1], nat["k"][:], ALU.mult, ALU.mult,
                  reads=[rnat["r"], rnat["k"], rcs, ryd[1]], writes=[ryd[1]])
            y = yd[0]
            ry = ryd[0]
            for bi, (t0, n) in enumerate(BLKS):
                b = bi % 2
                sl = slice(t0, t0 + n)
                p1, c1, rp1 = g.psb()
                k.mm(p1[:, c1:c1 + n], bo64[:], y[:, sl], reads=[rcs, ry], writes=[rp1])
                k.act(hn[0][:, 0:n], y[:, sl], AF.Square, reads=[ry], writes=[rhn[0]])
                p2, c2, rp2 = g.psb()
                k.mm(p2[:, c2:c2 + n], bo64[:], hn[0][:, 0:n], reads=[rcs, rhn[0]], writes=[rp2])
                k.cp(hn[1][:, 0:n], p1[:, c1:c1 + n], [rp1], [rhn[1]], eng="act")
                k.tt(hn[2][:, 0:n], hn[1][:, 0:n], hn[1][:, 0:n], ALU.mult, reads=[rhn[1]], writes=[rhn[2]],
                     eng="pool")
                k.tt(hn[2][:, 0:n], p2[:, c2:c2 + n], hn[2][:, 0:n], ALU.subtract, reads=[rp2, rhn[2]],
                     writes=[rhn[2]])
                k.act(hn[2][:, 0:n], hn[2][:, 0:n], AF.Sqrt, bias=epsc[:, 0:1], scale=1.0, reads=[rhn[2], rcs],
                      writes=[rhn[2]])
                k.op("dve", lambda e, o_=hn[2][:, 0:n]: e.reciprocal(o_, o_), [rhn[2]], [rhn[2]])
                k.tt(hn[3][:, 0:n], y[:, sl], hn[1][:, 0:n], ALU.subtract, reads=[ry, rhn[1]], writes=[rhn[3]])
                k.tt(hn[3][:, 0:n], hn[3][:, 0:n], hn[2][:, 0:n], ALU.mult, reads=[rhn[3], rhn[2]],
                     writes=[rhn[3]])
                k.act(hn[3][:, 0:n], hn[3][:, 0:n], AF.Identity, bias=par[:, 1, c:c + 1], scale=par[:, 0, c:c + 1],
                      reads=[rhn[3], rcs], writes=[rhn[3]])
                p3, c3, rp3 = g.psb()
                k.mm(p3[:, c3:c3 + n], bo[:], yd[1][:, sl], reads=[rcs, ryd[1]], writes=[rp3])
                k.tt(hn[0][:, 0:n], p3[:, c3:c3 + n], nat["v"][:, sl], ALU.mult, reads=[rp3, rnat["v"], rhn[0]],
                     writes=[rhn[0]])
                k.tt(hn[3][:, 0:n], hn[3][:, 0:n], hn[0][:, 0:n], ALU.add, reads=[rhn[3], rhn[0]],
                     writes=[rhn[3]], eng="pool")
                k.tt(yo[b][:, 0:n], hn[3][:, 0:n], gq[:, sl], ALU.mult, reads=[rhn[3], rgq], writes=[ryo[b]])
                k.dma(g.ybr_d[8 + c, :, sl], yo[b][:, 0:n], reads=[ryo[b]], writes=[g.rybr])
        k.barrier()


LN_EPS = 1e-5


def ln_block(g, z, rz, n, scr, rscr, lnw, lnb, rpar, od, rod, epsc, st_tiles):
    k = g.k
    mean, rmean, rstd, rrstd = st_tiles
    pm, cm, rpm = g.psb()
    for c in range(16):
        k.mm(pm[:, cm:cm + n], od[:], z[:, c, 0:n], start=(c == 0), stop=(c == 15), reads=[rod, rz], writes=[rpm])
    for c in range(16):
        k.act(scr[:, c, 0:n], z[:, c, 0:n], AF.Square, reads=[rz], writes=[rscr])
    pv, cv, rpv = g.psb()
    for c in range(16):
        k.mm(pv[:, cv:cv + n], od[:], scr[:, c, 0:n], start=(c == 0), stop=(c == 15), reads=[rod, rscr],
             writes=[rpv])
    k.cp(mean[:, 0:n], pm[:, cm:cm + n], [rpm], [rmean], eng="act")
    k.tt(rstd[:, 0:n], mean[:, 0:n], mean[:, 0:n], ALU.mult, reads=[rmean], writes=[rrstd], eng="pool")
    k.tt(rstd[:, 0:n], pv[:, cv:cv + n], rstd[:, 0:n], ALU.subtract, reads=[rpv, rrstd], writes=[rrstd])
    k.act(rstd[:, 0:n], rstd[:, 0:n], AF.Sqrt, bias=epsc[:, 0:1], scale=1.0, reads=[rrstd, rpar], writes=[rrstd])
    k.op("dve", lambda e, o_=rstd[:, 0:n]: e.reciprocal(o_, o_), [rrstd], [rrstd])
    for c in range(16):
        k.tt(scr[:, c, 0:n], z[:, c, 0:n], mean[:, 0:n], ALU.subtract, reads=[rz, rmean], writes=[rscr])
        k.tt(scr[:, c, 0:n], scr[:, c, 0:n], rstd[:, 0:n], ALU.mult, reads=[rscr, rrstd], writes=[rscr], eng="pool")
        k.act(z[:, c, 0:n], scr[:, c, 0:n], AF.Identity, bias=lnb[:, c:c + 1], scale=lnw[:, c:c + 1],
              reads=[rscr, rpar], writes=[rz])


def phase_merge(g, l, last):
    k, W, C = g.k, g.W, g.C
    halves = [[0, 1, 2], [3, 4]]
    if last:
        halves = [[1, 2], [3, 4]]
    g.ru2 = getattr(g, "ru2", None) or R("u2_d")
    g.rcmbd = getattr(g, "rcmbd", None) or R("cmb_d")
    with ExitStack() as st:
        bbg = k.sb("mbbg", [128, 64], F32, st)
        lnw = k.sb("mlnw", [128, 16], F32, st)
        lnb = k.sb("mlnb", [128, 16], F32, st)
        wr = k.sb("mwr", [128, 16, 36], F32, st)
        br = k.sb("mbr", [36, 1], F32, st)
        od = k.sb("mod2048", [128, 128], F32, st)
        epsc = k.sb("mepsc", [128, 1], F32, st)
        rpar = R()
        rod = R()
        k.dma(bbg[:], W["b_bgate"][l].rearrange("(j p) -> p j", p=128), writes=[rpar], allow_slow_non_contiguous=True)
        k.dma(lnw[:], W["ln1_w"][l].rearrange("(c p) -> p c", p=128), writes=[rpar], allow_slow_non_contiguous=True)
        k.dma(lnb[:], W["ln1_b"][l].rearrange("(c p) -> p c", p=128), writes=[rpar], allow_slow_non_contiguous=True)
        k.dma(wr[:, :, 0:4], W["moe_w_grp"][l].rearrange("(c p) n -> p c n", p=128), writes=[rpar])
        k.dma(wr[:, :, 4:36], W["moe_w_exp"][l].rearrange("(c p) n -> p c n", p=128), writes=[rpar])
        k.dma(br[0:4, :], W["moe_b_grp"][l].rearrange("(p o) -> p o", o=1), writes=[rpar])
        k.dma(br[4:36, :], W["moe_b_exp"][l].rearrange("(p o) -> p o", o=1), writes=[rpar])
        k.memset(od[:], 1.0 / 2048.0, writes=[rod], eng="dve")
        k.memset(epsc[:], LN_EPS, writes=[rpar], eng="dve")
        mg = k.sb("mmg", [128, 16, 1280], BF16, st)
        rmg = R()
        wi = 0
        for half in halves:
            blks = [BLKS[i] for i in half]
            p0 = blks[0][0]
            np_ = sum(n for _, n in blks)
            with ExitStack() as st1:
                u1 = k.sb("mu1", [128, 16, 1280], BF16, st1)
                yb = k.sb("myb", [128, 16, 1280], BF16, st1)
                ru1, ryb = R(), R()
                xs_ = [k.sb("mxs%d" % i, [128, 512], F32, st1) for i in range(3)]
                rxs_ = [R() for _ in range(3)]
                wbg = [k.sb("mwbg%d" % i, [128, 16, 512], BF16, st1) for i in range(2)]
                rwbg = [R(), R()]
                wbr = [k.sb("mwbr%d" % i, [128, 4, 512], BF16, st1) for i in range(2)]
                rwbr = [R(), R()]
                gtt = [k.sb("mgt%d" % i, [128, 512], F32, st1) for i in range(2)]
                rgt = [R(), R()]
                tt_ = [k.sb("mtt%d" % i, [128, 512], F32, st1) for i in range(2)]
                rtt = [R(), R()]
                macc = [[k.sb("mmacc%d_%d" % (cc, bi), [128, 512], F32, st1) for bi in range(len(blks))]
                        for cc in range(4)]
                rmacc = [[R() for bi in range(len(blks))] for cc in range(4)]
                k.dma(yb[:, :, 0:np_], g.ybr_d[:, :, p0:p0 + np_].rearrange("i p t -> p i t"), reads=[g.rybr],
                      writes=[ryb])
                xi = 0
                for (t0, n) in blks:
                    j = 1 if t0 == 0 else 0
                    o = t0 - p0
                    for c in range(16):
                        b = xi % 3
                        xi += 1
                        k.dma(xs_[b][:, 0:n], g.xs_d[c, :, t0:t0 + n], reads=[g.rxs], writes=[rxs_[b]])
                        k.act(u1[:, c, o:o + n], xs_[b][:, 0:n], AF.Identity, bias=mod(g, l, 0, c, j),
                              scale=mod(g, l, 1, c, j), reads=[rxs_[b], g.rmod], writes=[ru1])
                gi = 0
                for c4 in range(4):
                    for kbr in range(4):
                        b = wi % 2
                        wi += 1
                        k.dma(wbg[b][:], W["w_bgate"][l, :, kbr * 2048 + c4 * 512:kbr * 2048 + (c4 + 1) * 512].rearrange(
                            "(cc p) n -> p cc n", p=128), writes=[rwbg[b]], issuer="pool")
                        k.dma(wbr[b][:], W["w_branch"][l, kbr, :, c4 * 512:(c4 + 1) * 512].rearrange(
                            "(cc p) n -> p cc n", p=128), writes=[rwbr[b]], issuer="pool")
                        for bi, (t0, n) in enumerate(blks):
                            o = t0 - p0
                            for cc in range(4):
                                c = c4 * 4 + cc
                                pg, cg, rpg = g.psb()
                                for kc in range(16):
                                    k.mm(pg[:, cg:cg + n], wbg[b][:, kc, cc * 128:(cc + 1) * 128], u1[:, kc, o:o + n],
                                         start=(kc == 0), stop=(kc == 15), reads=[rwbg[b], ru1], writes=[rpg])
                                pp, cp_, rpp = g.psb()
                                for q4 in range(4):
                                    k.mm(pp[:, cp_:cp_ + n], wbr[b][:, q4, cc * 128:(cc + 1) * 128],
                                         yb[:, kbr * 4 + q4, o:o + n], start=(q4 == 0), stop=(q4 == 3),
                                         reads=[rwbr[b], ryb], writes=[rpp])
                                gb_ = gi % 2
                                gi += 1
                                k.act(gtt[gb_][:, 0:n], pg[:, cg:cg + n], AF.Sigmoid,
                                      bias=bbg[:, kbr * 16 + c:kbr * 16 + c + 1], scale=1.0, reads=[rpg, rpar],
                                      writes=[rgt[gb_]])
                                ma, rma = macc[cc][bi], rmacc[cc][bi]
                                if kbr == 0:
                                    k.tt(ma[:, 0:n], pp[:, cp_:cp_ + n], gtt[gb_][:, 0:n], ALU.mult,
                                         reads=[rpp, rgt[gb_]], writes=[rma])
                                else:
                                    k.tt(tt_[gb_][:, 0:n], pp[:, cp_:cp_ + n], gtt[gb_][:, 0:n], ALU.mult,
                                         reads=[rpp, rgt[gb_]], writes=[rtt[gb_]])
                                    if kbr < 3:
                                        k.tt(ma[:, 0:n], ma[:, 0:n], tt_[gb_][:, 0:n], ALU.add,
                                             reads=[rma, rtt[gb_]], writes=[rma], eng="pool")
                                    else:
                                        k.tt(mg[:, c, o:o + n], ma[:, 0:n], tt_[gb_][:, 0:n], ALU.add,
                                             reads=[rma, rtt[gb_]], writes=[rmg], eng="pool")
                k.barrier()
            with ExitStack() as st2:
                xb = k.sb("mxb", [128, 16, 512], F32, st2)
                z = k.sb("mz", [128, 16, 512], F32, st2)
                u2b = k.sb("mu2b", [128, 16, 512], BF16, st2)
                rxb, rz, ru2b = R(), R(), R()
                wo = [k.sb("mwo%d" % i, [128, 16, 512], BF16, st2) for i in range(2)]
                rwo = [R(), R()]
                mean = k.sb("mmean", [128, 512], F32, st2)
                rstd = k.sb("mrstd", [128, 512], F32, st2)
                lgt = k.sb("mlgt", [36, 512], F32, st2)
                rlgt = R()
                lnt = (mean, R(), rstd, R())
                L = k.sb("mL", [128, 36], F32, st2)
                sm = k.sb("msm", [128, 16], F32, st2)
                gh = k.sb("mgh", [128, 4], F32, st2)
                mk1 = k.sb("mmk1", [128, 32], F32, st2)
                mk2 = k.sb("mmk2", [128, 32], F32, st2)
                oh1 = k.sb("moh1", [128, 32], F32, st2)
                oh2 = k.sb("moh2", [128, 32], F32, st2)
                cmb = k.sb("mcmb", [128, 32], F32, st2)
                cmbT = k.sb("mcmbT", [32, 512], F32, st2)
                rL, rsm, rgh, rmk1, rmk2, roh1, roh2, rcmb, rcmbT = [R() for _ in range(9)]
                for (t0, n) in blks:
                    j = 1 if t0 == 0 else 0
                    o = t0 - p0
                    k.dma(xb[:, :, 0:n], g.xs_d[:, :, t0:t0 + n].rearrange("c p t -> p c t"), reads=[g.rxs],
                          writes=[rxb])
                    k.ts(xb[:, :, 0:n], xb[:, :, 0:n], ALPHA, None, ALU.mult, reads=[rxb], writes=[rxb], eng="pool")
                    for grp in range(4):
                        b = wi % 2
                        wi += 1
                        k.dma(wo[b][:], W["w_out"][l, :, grp * 512:(grp + 1) * 512].rearrange("(cc p) n -> p cc n", p=128),
                              writes=[rwo[b]], issuer="pool")
                        for nn in range(4):
                            ni = grp * 4 + nn
                            po, co, rpo = g.psb()
                            for c in range(16):
                                k.mm(po[:, co:co + n], wo[b][:, c, nn * 128:(nn + 1) * 128], mg[:, c, o:o + n],
                                     start=(c == 0), stop=(c == 15), reads=[rwo[b], rmg], writes=[rpo])
                            k.stt(z[:, ni, 0:n], po[:, co:co + n], mod(g, l, 2, ni, j), xb[:, ni, 0:n], ALU.mult,
                                  ALU.add, reads=[rpo, g.rmod, rxb], writes=[rz])
                    ln_block(g, z, rz, n, xb, rxb, lnw, lnb, rpar, od, rod, epsc, lnt)
                    k.dma(g.xs_d[:, :, t0:t0 + n].rearrange("c p t -> p c t"), z[:, :, 0:n], reads=[rz],
                          writes=[g.rxs])
                    for c in range(16):
                        k.act(xb[:, c, 0:n], z[:, c, 0:n], AF.Identity, bias=mod(g, l, 3, c, j),
                              scale=mod(g, l, 4, c, j), reads=[rz, g.rmod], writes=[rxb])
                    k.cp(u2b[:, :, 0:n], xb[:, :, 0:n], [rxb], [ru2b], eng="pool")
                    k.dma(g.u2_d[:, :, t0:t0 + n].rearrange("c p t -> p c t"), u2b[:, :, 0:n], reads=[ru2b],
                          writes=[g.ru2])
                    pl_, cl, rpl = g.psb()
                    for c in range(16):
                        k.mm(pl_[0:36, cl:cl + n], wr[:, c, :], xb[:, c, 0:n], start=(c == 0), stop=(c == 15),
                             reads=[rpar, rxb], writes=[rpl])
                    k.act(lgt[:, 0:n], pl_[0:36, cl:cl + n], AF.Identity, bias=br[:, 0:1], scale=1.0,
                          reads=[rpl, rpar], writes=[rlgt])
                    for ti in range(n // 128):
                        tsl = slice(ti * 128, (ti + 1) * 128)
                        pt, ct, rpt = g.psb()
                        k.tr(pt[:, ct:ct + 36], lgt[:, tsl], g.ident[0:36, 0:36], reads=[rlgt, g.rconst],
                             writes=[rpt])
                        k.cp(L[:], pt[:, ct:ct + 36], [rpt], [rL])
                        D_ = "dve"
                        k.op(D_, lambda e: e.tensor_reduce(sm[:, 0:1], L[:, 0:4], AX.X, ALU.max), [rL], [rsm])
                        k.ts(gh[:], L[:, 0:4], sm[:, 0:1], None, ALU.subtract, reads=[rL, rsm], writes=[rgh])
                        k.act(gh[:], gh[:], AF.Exp, reads=[rgh], writes=[rgh])
                        k.op(D_, lambda e: e.tensor_reduce(sm[:, 1:2], gh[:], AX.X, ALU.add), [rgh], [rsm])
                        k.op(D_, lambda e: e.reciprocal(sm[:, 2:3], sm[:, 1:2]), [rsm], [rsm])
                        k.ts(gh[:], L[:, 0:4], sm[:, 0:1], None, ALU.is_equal, reads=[rL, rsm, rgh], writes=[rgh])
                        k.ts(gh[:], gh[:], -1.0, 1e30, ALU.add, ALU.mult, reads=[rgh], writes=[rgh])
                        k.tt(mk1[:].rearrange("p (a b) -> p a b", b=8), L[:, 4:36].rearrange("p (a b) -> p a b", b=8),
                             gh[:].rearrange("p (a o) -> p a o", o=1).to_broadcast([128, 4, 8]), ALU.add,
                             reads=[rL, rgh], writes=[rmk1])
                        k.op(D_, lambda e: e.tensor_reduce(sm[:, 3:4], mk1[:], AX.X, ALU.max), [rmk1], [rsm])
                        k.ts(oh1[:], mk1[:], sm[:, 3:4], None, ALU.is_equal, reads=[rmk1, rsm], writes=[roh1])
                        k.stt(mk2[:], oh1[:], -1e30, mk1[:], ALU.mult, ALU.add, reads=[roh1, rmk1], writes=[rmk2])
                        k.op(D_, lambda e: e.tensor_reduce(sm[:, 4:5], mk2[:], AX.X, ALU.max), [rmk2], [rsm])
                        k.ts(oh2[:], mk2[:], sm[:, 4:5], None, ALU.is_equal, reads=[rmk2, rsm], writes=[roh2])
                        k.tt(sm[:, 5:6], sm[:, 4:5], sm[:, 3:4], ALU.subtract, reads=[rsm], writes=[rsm])
                        k.act(sm[:, 6:7], sm[:, 5:6], AF.Exp, reads=[rsm], writes=[rsm])
                        k.ts(sm[:, 7:8], sm[:, 6:7], 1.0, None, ALU.add, reads=[rsm], writes=[rsm])
                        k.op(D_, lambda e: e.reciprocal(sm[:, 8:9], sm[:, 7:8]), [rsm], [rsm])
                        k.tt(sm[:, 9:10], sm[:, 8:9], sm[:, 2:3], ALU.mult, reads=[rsm], writes=[rsm])
                        k.tt(sm[:, 10:11], sm[:, 9:10], sm[:, 6:7], ALU.mult, reads=[rsm], writes=[rsm])
                        k.ts(cmb[:], oh1[:], sm[:, 9:10], None, ALU.mult, reads=[roh1, rsm], writes=[rcmb])
                        k.stt(cmb[:], oh2[:], sm[:, 10:11], cmb[:], ALU.mult, ALU.add, reads=[roh2, rsm, rcmb],
                              writes=[rcmb])
                        pt2, ct2, rpt2 = g.psb()
                        k.tr(pt2[0:32, ct2:ct2 + 128], cmb[:], g.ident[:], reads=[rcmb, g.rconst], writes=[rpt2])
                        k.cp(cmbT[:, tsl], pt2[0:32, ct2:ct2 + 128], [rpt2], [rcmbT], eng="act")
                    k.dma(g.cmb_d[:, t0:t0 + n], cmbT[:, 0:n], reads=[rcmbT], writes=[g.rcmbd])
                k.barrier()
        k.barrier()


def phase_moe(g, l, last, n_exp=32):
    k, W, C = g.k, g.W, g.C
    parts = [[0, 1, 2], [3, 4]]
    if last:
        parts = [[1, 2], [3, 4]]
    with ExitStack() as st:
        u2 = k.sb("eu2", [128, 16, 1280], BF16, st)
        acc = k.sb("eacc", [128, 16, 1280], F32, st)
        ru2s, racc = R(), R()
        lnw = k.sb("elnw", [128, 16], F32, st)
        lnb = k.sb("elnb", [128, 16], F32, st)
        od = k.sb("eod", [128, 128], F32, st)
        epsc = k.sb("eepsc", [128, 1], F32, st)
        rpar, rod = R(), R()
        k.dma(lnw[:], W["ln2_w"][l].rearrange("(c p) -> p c", p=128), writes=[rpar], allow_slow_non_contiguous=True)
        k.dma(lnb[:], W["ln2_b"][l].rearrange("(c p) -> p c", p=128), writes=[rpar], allow_slow_non_contiguous=True)
        k.memset(od[:], 1.0 / 2048.0, writes=[rod], eng="dve")
        k.memset(epsc[:], LN_EPS, writes=[rpar], eng="dve")
        for part in parts:
            blks = [BLKS[i] for i in part]
            p0 = blks[0][0]
            np_ = sum(n for _, n in blks)
            k.dma(u2[:, :, 0:np_], g.u2_d[:, :, p0:p0 + np_].rearrange("c p t -> p c t"), reads=[g.ru2],
                  writes=[ru2s])
            k.memset(acc[:, :, 0:np_], 0.0, writes=[racc], eng="pool")
            with ExitStack() as st2:
                w13 = [k.sb("ew13_%d" % i, [128, 16, 2, 128], BF16, st2) for i in range(2)]
                rw13 = [R(), R()]
                w2b = [k.sb("ew2_%d" % i, [128, 4, 2048], BF16, st2) for i in range(2)]
                rw2 = [R(), R()]
                hT = k.sb("ehT", [128, 4, 1280], BF16, st2)
                rhT = R()
                cb = [k.sb("ecb%d" % i, [128, 512], F32, st2) for i in range(3)]
                rcb = [R() for _ in range(3)]
                sg = [k.sb("esg%d" % i, [128, 512], F32, st2) for i in range(2)]
                rsg = [R(), R()]
                wi = 0
                ci = 0
                for e in range(n_exp):
                    eb = e % 2
                    k.dma(w2b[eb][:], W["moe_w2"][l, e].rearrange("(f p) n -> p f n", p=128), writes=[rw2[eb]],
                          issuer="pool")
                    cbs = []
                    for (t0, n) in blks:
                        cix = ci % 3
                        ci += 1
                        k.dma(cb[cix][:, 0:n], g.cmb_d[e, t0:t0 + n].partition_broadcast(128), reads=[g.rcmbd],
                              writes=[rcb[cix]])
                        cbs.append(cix)
                    for f in range(4):
                        b = wi % 2
                        wi += 1
                        k.dma(w13[b][:, :, 0, :],
                              W["moe_w1"][l, e, :, f * 128:(f + 1) * 128].rearrange("(c p) n -> p c n", p=128),
                              writes=[rw13[b]], issuer="pool")
                        k.dma(w13[b][:, :, 1, :],
                              W["moe_w3"][l, e, :, f * 128:(f + 1) * 128].rearrange("(c p) n -> p c n", p=128),
                              writes=[rw13[b]], issuer="pool")
                        for bi, (t0, n) in enumerate(blks):
                            o = t0 - p0
                            p1, c1, rp1 = g.psb()
                            for c in range(16):
                                k.mm(p1[:, c1:c1 + n], w13[b][:, c, 0, :], u2[:, c, o:o + n], start=(c == 0),
                                     stop=(c == 15), reads=[rw13[b], ru2s], writes=[rp1])
                            p3, c3, rp3 = g.psb()
                            for c in range(16):
                                k.mm(p3[:, c3:c3 + n], w13[b][:, c, 1, :], u2[:, c, o:o + n], start=(c == 0),
                                     stop=(c == 15), reads=[rw13[b], ru2s], writes=[rp3])
                            sb_ = (f * 8 + bi) % 2
                            k.act(sg[sb_][:, 0:n], p1[:, c1:c1 + n], AF.Silu, reads=[rp1], writes=[rsg[sb_]])
                            k.tt(sg[sb_][:, 0:n], p3[:, c3:c3 + n], sg[sb_][:, 0:n], ALU.mult, reads=[rp3, rsg[sb_]],
                                 writes=[rsg[sb_]])
                            k.tt(hT[:, f, o:o + n], sg[sb_][:, 0:n], cb[cbs[bi]][:, 0:n], ALU.mult,
                                 reads=[rsg[sb_], rcb[cbs[bi]]], writes=[rhT], eng="pool")
                    for bi, (t0, n) in enumerate(blks):
                        o = t0 - p0
                        for nn in range(16):
                            po, co, rpo = g.psb()
                            for f in range(4):
                                k.mm(po[:, co:co + n], w2b[eb][:, f, nn * 128:(nn + 1) * 128], hT[:, f, o:o + n],
                                     start=(f == 0), stop=(f == 3), reads=[rw2[eb], rhT], writes=[rpo])
                            k.tt(acc[:, nn, o:o + n], po[:, co:co + n], acc[:, nn, o:o + n], ALU.add,
                                 reads=[rpo, racc], writes=[racc])
                k.barrier()
            with ExitStack() as st3:
                xb = k.sb("exb", [128, 16, 512], F32, st3)
                zz = k.sb("ezz", [128, 16, 512], F32, st3)
                mean = k.sb("emean", [128, 512], F32, st3)
                rstd = k.sb("erstd", [128, 512], F32, st3)
                rxb, rzz = R(), R()
                lnt = (mean, R(), rstd, R())
                for (t0, n) in blks:
                    j = 1 if t0 == 0 else 0
                    o = t0 - p0
                    k.dma(xb[:, :, 0:n], g.xs_d[:, :, t0:t0 + n].rearrange("c p t -> p c t"), reads=[g.rxs],
                          writes=[rxb])
                    k.ts(xb[:, :, 0:n], xb[:, :, 0:n], ALPHA, None, ALU.mult, reads=[rxb], writes=[rxb], eng="pool")
                    for c in range(16):
                        k.stt(zz[:, c, 0:n], acc[:, c, o:o + n], mod(g, l, 5, c, j), xb[:, c, 0:n], ALU.mult,
                              ALU.add, reads=[racc, g.rmod, rxb], writes=[rzz])
                    ln_block(g, zz, rzz, n, xb, rxb, lnw, lnb, rpar, od, rod, epsc, lnt)
                    k.dma(g.xs_d[:, :, t0:t0 + n].rearrange("c p t -> p c t"), zz[:, :, 0:n], reads=[rzz],
                          writes=[g.rxs])
                k.barrier()
        k.barrier()

from concourse.bass_utils import run_bass_kernel_spmd

N_CORES = 4


def kernel(**inputs):
    nc = build(nl=DEPTH)
    cs = host_consts()
    in_maps = []
    for b in range(N_CORES):
        m = {}
        for n in W_SHAPES:
            m[n] = np.ascontiguousarray(np.asarray(inputs[n], dtype=np.float32))
        m["xin"] = np.ascontiguousarray(
            np.concatenate([np.asarray(inputs["ctx"][b]), np.asarray(inputs["x"][b])], 0).astype(np.float32))
        m["c2"] = np.ascontiguousarray(
            np.stack([np.asarray(inputs["c"][b]), np.asarray(inputs["c_ctx"])], 0).astype(np.float32))
        for n, v in cs.items():
            m["k_" + n] = v
        in_maps.append(m)
    res = run_bass_kernel_spmd(nc, in_maps, core_ids=list(range(N_CORES)))
    out = np.stack([np.asarray(r["out"], dtype=np.float32) for r in res.results], 0)
    return out
```

```python
import numpy as np
from contextlib import ExitStack
import concourse.bass as bass
import concourse.mybir as mybir

F32 = mybir.dt.float32
BF16 = mybir.dt.bfloat16
AF = mybir.ActivationFunctionType
ALU = mybir.AluOpType
AX = mybir.AxisListType

NDQ = 8


class R:
    __slots__ = ("w", "rd", "name")

    def __init__(self, name=""):
        self.w = None
        self.rd = {}
        self.name = name


class KB:
    def __init__(self, nc):
        self.nc = nc
        self.es = ExitStack()
        self.eng = {"pe": nc.tensor, "act": nc.scalar, "dve": nc.vector, "pool": nc.gpsimd, "sp": nc.sync}
        self.real = list(self.eng.keys())
        self.virt = ["dq%d" % i for i in range(NDQ)]
        self.all = self.real + self.virt + ["cc"]
        self.sem = {}
        for e in self.all:
            self.sem[e] = self.es.enter_context(nc.semaphore("s_" + e))
        self.inc = {e: (1 if (e in self.real or e == "cc") else 16) for e in self.all}
        self.cnt = {e: 0 for e in self.all}
        self.seen = {e: {f: 0 for f in self.all} for e in self.real}
        self.q = {e: [] for e in self.real}
        self.dq_next = 0
        self.n_ops = 0

    def sb(self, name, shape, dtype, stack=None):
        self.uid = getattr(self, "uid", 0) + 1
        t = (stack or self.es).enter_context(self.nc.sbuf_tensor("%s_u%d" % (name, self.uid), list(shape), dtype))
        return t

    def ps(self, name, shape, dtype, stack=None):
        t = (stack or self.es).enter_context(self.nc.psum_tensor(name, list(shape), dtype))
        return t

    def _waits(self, eng, reads, writes):
        needs = {}
        for r in reads:
            if r.w is not None:
                f, n = r.w
                if n > needs.get(f, 0):
                    needs[f] = n
        for w in writes:
            if w.w is not None:
                f, n = w.w
                if f != eng and n > needs.get(f, 0):
                    needs[f] = n
            for f, n in w.rd.items():
                if f != eng and n > needs.get(f, 0):
                    needs[f] = n
        seen = self.seen[eng]
        for f, n in needs.items():
            if seen[f] >= n:
                continue
            seen[f] = n
            self.q[eng].append(("w", self.sem[f], n * self.inc[f]))

    def _commit(self, tag, reads, writes):
        self.cnt[tag] += 1
        n = self.cnt[tag]
        for r in reads:
            if n > r.rd.get(tag, 0):
                r.rd[tag] = n
        for w in writes:
            w.w = (tag, n)
            w.rd = {}
        return n

    def op(self, eng, fn, reads=(), writes=(), inc=True):
        self._waits(eng, reads, writes)
        if inc:
            self._commit(eng, reads, writes)
            self.q[eng].append(("o", fn, self.sem[eng], 1))
        else:
            n = self.cnt[eng] + 1
            for r in reads:
                if n > r.rd.get(eng, 0):
                    r.rd[eng] = n
            for w in writes:
                w.w = (eng, n)
                w.rd = {}
            self.q[eng].append(("n", fn))
        self.n_ops += 1

    def dma(self, out, in_, reads=(), writes=(), issuer="sp", **kw):
        slot = self.virt[self.dq_next]
        self.dq_next = (self.dq_next + 1) % NDQ
        seen = self.seen[issuer]
        if seen[slot] < self.cnt[slot]:
            seen[slot] = self.cnt[slot]
            self.q[issuer].append(("w", self.sem[slot], self.cnt[slot] * 16))
        self._waits(issuer, reads, writes)
        self._commit(slot, reads, writes)
        self.q[issuer].append(("o", (lambda e, o=out, i=in_, k=kw: e.dma_start(out=o, in_=i, **k)), self.sem[slot], 16))
        self.n_ops += 1

    def coll(self, fn, reads=(), writes=()):
        self._waits("pool", reads, writes)
        self._commit("cc", reads, writes)
        self.q["pool"].append(("o", fn, self.sem["cc"], 1))
        self.n_ops += 1

    def barrier(self):
        for e in self.real:
            seen = self.seen[e]
            for f in self.all:
                if f == e:
                    continue
                if seen[f] < self.cnt[f]:
                    seen[f] = self.cnt[f]
                    self.q[e].append(("w", self.sem[f], self.cnt[f] * self.inc[f]))

    def finish(self):
        self.barrier()
        nc = self.nc
        q = self.q

        def replay(name):
            def f(e):
                for it in q[name]:
                    if it[0] == "w":
                        e.wait_ge(it[1], it[2])
                    elif it[0] == "n":
                        it[1](e)
                    else:
                        ins = it[1](e)
                        ins.then_inc(it[2], it[3])
            return f

        with nc.Block() as block:
            block.tensor(replay("pe"))
            block.scalar(replay("act"))
            block.vector(replay("dve"))
            block.gpsimd(replay("pool"))
            block.sync(replay("sp"))
        self.es.close()

    def mm(self, out, lhsT, rhs, start=True, stop=True, reads=(), writes=(), **kw):
        self.op("pe", lambda e: e.matmul(out, lhsT, rhs, start=start, stop=stop, **kw), reads, writes, inc=bool(stop))

    def tr(self, out, in_, ident, reads=(), writes=()):
        self.op("pe", lambda e: e.transpose(out, in_, ident), reads, writes)

    def act(self, out, in_, func, bias=None, scale=None, reads=(), writes=(), eng="act", **kw):
        k = dict(kw)
        if bias is not None:
            k["bias"] = bias
        if scale is not None:
            k["scale"] = scale
        self.op("act", lambda e: e.activation(out, in_, func, **k), reads, writes)

    def tt(self, out, a, b, op, reads=(), writes=(), eng="dve"):
        self.op(eng, lambda e: e.tensor_tensor(out, a, b, op), reads, writes)

    def ts(self, out, a, s1, s2, op0, op1=None, reads=(), writes=(), eng="dve", **kw):
        if op1 is None:
            self.op(eng, lambda e: e.tensor_scalar(out, a, s1, None, op0, **kw), reads, writes)
        else:
            self.op(eng, lambda e: e.tensor_scalar(out, a, s1, s2, op0, op1, **kw), reads, writes)

    def stt(self, out, a, s, b, op0, op1, reads=(), writes=(), eng="dve"):
        self.op(eng, lambda e: e.scalar_tensor_tensor(out, a, s, b, op0, op1), reads, writes)

    def cp(self, out, in_, reads=(), writes=(), eng="dve"):
        if eng == "act":
            self.op("act", lambda e: e.copy(out, in_), reads, writes)
        else:
            self.op(eng, lambda e: e.tensor_copy(out, in_), reads, writes)

    def memset(self, ap, val, writes=(), eng="pool"):
        self.op(eng, lambda e: e.memset(ap, val), (), writes)

import math
import numpy as np
from contextlib import ExitStack
import concourse.bass as bass
import concourse.mybir as mybir

T = 2304
LC = 256
D = 2048
KC = 16
BLKS = [(0, 256), (256, 512), (768, 512), (1280, 512), (1792, 512)]
NT = 18
HALF = 1152
LB = [(0, 256), (256, 512), (768, 384)]


def nat_split(t0, n):
    out = []
    t = t0
    end = t0 + n
    while t < end:
        h = t // HALF
        lt = t - h * HALF
        ln = min(end, (h + 1) * HALF) - t
        out.append((h, lt, ln, t - t0))
        t += ln
    return out
DEPTH = 4
ALPHA = (2.0 * DEPTH) ** 0.25
C_RW = 64
NCH = T // C_RW

CHUNKS = {}
_o = 0
for h in range(4):
    CHUNKS["ret_q%d" % h] = [(0 + h * 128, 128)]
    CHUNKS["ret_k%d" % h] = [(512 + h * 128, 128)]
    CHUNKS["ret_g%d" % h] = [(1536 + h * 128, 128)]
for c in range(4):
    CHUNKS["gqa_q%d" % c] = [(2048 + c * 128, 128)]
for g in range(2):
    CHUNKS["gqa_k%d" % g] = [(2560 + g * 64, 64), (2560 + g * 64, 64)]
RW0 = 2816
for c in range(4):
    CHUNKS["rw_r%d" % c] = [(RW0 + c * 128, 128)]
    CHUNKS["rw_k%d" % c] = [(RW0 + 512 + c * 128, 128)]
    CHUNKS["rw_v%d" % c] = [(RW0 + 1024 + c * 128, 128)]
CHUNKS["rw_wdf"] = [(RW0 + 1536, 96)]
CHUNKS["rw_wdb"] = [(RW0 + 1632, 96)]
CHUNKS["rw_ad"] = [(RW0 + 1728, 96)]
CHUNKS["rw_gd0"] = [(RW0 + 1824, 128)]
CHUNKS["rw_gd1"] = [(RW0 + 1952, 128)]
LR0 = 4896
for c in range(4):
    CHUNKS["lru_x%d" % c] = [(LR0 + c * 128, 128)]
    CHUNKS["lru_g%d" % c] = [(LR0 + 512 + c * 128, 128)]
CH_NAMES = list(CHUNKS.keys())
CH_ID = {n: i for i, n in enumerate(CH_NAMES)}
NCHUNK = len(CH_NAMES)


def ch_width(name):
    return sum(w for _, w in CHUNKS[name])


def host_consts():
    cs = {}
    cs["ident"] = np.eye(128, dtype=np.float32)
    tok = np.arange(2048)
    row = (tok // 64).astype(np.float32)
    col = (tok % 64).astype(np.float32)

    def tables(dh):
        da = dh // 2
        inv = (10000.0 ** (-np.arange(0, da, 2, dtype=np.float32) / da)).astype(np.float32)
        nf = da // 2
        cos = np.ones((dh, T), np.float32)
        sin = np.zeros((dh, T), np.float32)
        for d in range(dh):
            first = d < da
            dd = d if first else d - da
            fi = dd % nf
            pos = row if first else col
            ang = (pos * inv[fi]).astype(np.float32)
            cos[d, LC:] = np.cos(ang)
            sin[d, LC:] = np.sin(ang)
        P = np.zeros((dh, dh), np.float32)
        for m in range(dh):
            dd = m % da
            if dd < nf:
                P[m + nf, m] = -1.0
            else:
                P[m - nf, m] = 1.0
        return cos, sin, P

    c, s, P = tables(128)
    cs["ret_cos"], cs["ret_sin"], cs["ret_P"] = c, s, P
    c, s, P = tables(64)
    cs["gqa_cos"] = np.concatenate([c, c], 0)
    cs["gqa_sin"] = np.concatenate([s, s], 0)
    P2 = np.zeros((128, 128), np.float32)
    P2[:64, :64] = P
    P2[64:, 64:] = P
    cs["gqa_P"] = P2
    j = np.arange(128)[:, None].astype(np.float32)
    i = np.arange(128)[None, :].astype(np.float32)
    sc = 128.0 ** -0.5
    ret = np.zeros((128, 6, 128), np.float32)
    ret[:, 0] = np.maximum(i - j, 0)
    ret[:, 1] = np.maximum(j - i, 0)
    ret[:, 2] = (i >= j) * sc
    ret[:, 3] = (j >= i) * sc
    ret[:, 4] = np.broadcast_to(i + 1.0, (128, 128))
    ret[:, 5] = np.broadcast_to(128.0 - i, (128, 128))
    cs["ret_tab"] = ret
    cj = np.zeros((128, 2), np.float32)
    cj[:, 0] = 127.0 - np.arange(128)
    cj[:, 1] = np.arange(128)
    cs["ret_cj"] = cj
    gm = np.zeros((128, 2, 128), np.float32)
    gm[:, 0] = (j >= i)
    gm[:, 1] = (j <= i)
    cs["gqa_mask"] = gm
    blk = (np.arange(128)[:, None] // 64) == (np.arange(128)[None, :] // 64)
    a = np.arange(128)[:, None] % 64
    b = np.arange(128)[None, :] % 64
    rm = np.zeros((128, 5, 128), np.float32)
    rm[:, 0] = blk & (a > b)
    rm[:, 1] = blk & (b > a)
    rm[:, 2] = blk & (b > a)
    rm[:, 3] = blk & (b >= a)
    rm[:, 4] = blk & (b >= a)
    cs["rw_mask"] = rm
    cs["blk_ones"] = blk.astype(np.float32)
    ist = np.zeros((128, 64), np.float32)
    ist[np.arange(128), np.arange(128) % 64] = 1.0
    cs["ist"] = ist
    return cs


CONST_SHAPES = None


W_SHAPES = {
    "w_ada": [4, 2048, 12288], "b_ada": [4, 12288], "w_in": [4, 2048, 5920],
    "ret_decay_logit": [4, 2, 4], "ret_gn_w": [4, 512], "ret_gn_b": [4, 512], "gqa_sink": [4, 8],
    "rwkv_mu": [4, 2080], "rwkv_w0": [4, 2, 512], "rwkv_w_up": [4, 2, 96, 512], "rwkv_a0": [4, 512],
    "rwkv_a_up": [4, 96, 512], "rwkv_g_up": [4, 256, 512], "rwkv_k_k": [4, 512], "rwkv_k_a": [4, 512],
    "rwkv_r_k": [4, 8, 64], "rwkv_ln_w": [4, 512], "rwkv_ln_b": [4, 512],
    "lru_conv_w": [4, 4, 512], "lru_conv_b": [4, 512], "lru_gate_w": [4, 2, 2, 8, 64, 64],
    "lru_gate_b": [4, 2, 2, 512], "lru_lambda": [4, 2, 512],
    "w_branch": [4, 4, 512, 2048], "w_bgate": [4, 2048, 8192], "b_bgate": [4, 8192], "w_out": [4, 2048, 2048],
    "ln1_w": [4, 2048], "ln1_b": [4, 2048], "ln2_w": [4, 2048], "ln2_b": [4, 2048],
    "moe_w_grp": [4, 2048, 4], "moe_b_grp": [4, 4], "moe_w_exp": [4, 2048, 32], "moe_b_exp": [4, 32],
    "moe_w1": [4, 32, 2048, 512], "moe_w3": [4, 32, 2048, 512], "moe_w2": [4, 32, 512, 2048],
}


class Ctx:
    pass


def build(nl=4, dump=(), stop=None, n_exp=32, rgroups=None):
    nc = bass.Bass("TRN2", target_bir_lowering=False)
    g = Ctx()
    g.nc = nc
    g.nl = nl
    g.rgroups = rgroups or [[0, 1], [2, 3], [4, 5], [6, 7]]
    W = {}
    for n, s in W_SHAPES.items():
        W[n] = nc.dram_tensor(n, [nl] + list(s[1:]), F32, kind="ExternalInput").ap()
    g.W = W
    g.xin = nc.dram_tensor("xin", [T, D], F32, kind="ExternalInput").ap()
    g.c2 = nc.dram_tensor("c2", [3, D], F32, kind="ExternalInput").ap()
    g.xown_in = nc.dram_tensor("xown_in", [HALF, D], F32, kind="ExternalInput").ap()
    g.sel_in = nc.dram_tensor("sel", [128, 2], F32, kind="ExternalInput").ap()
    cs = host_consts()
    g.C = {n: nc.dram_tensor("k_" + n, list(v.shape), F32, kind="ExternalInput").ap() for n, v in cs.items()}
    g.out = nc.dram_tensor("out", [2048, D], F32, kind="ExternalOutput").ap()

    def scratch(name, shape, dt):
        kind = "ExternalOutput" if name in dump else "Internal"
        return nc.dram_tensor(name, shape, dt, kind=kind).ap()

    g.xs2_t = nc.dram_tensor("xs2", [2 * KC * 128, HALF], F32)
    g.xown_t = nc.dram_tensor("xown", [KC * 128, HALF], F32)
    g.xs4 = g.xs2_t.ap().rearrange("(c h p) t -> h c p t", c=KC, h=2)
    g.xo3 = g.xown_t.ap().rearrange("(c p) t -> c p t", c=KC)
    g.x1o3 = scratch("x1own", [KC, 128, HALF], F32)
    g.p_d = scratch("p_d", [NCHUNK, 128, T], F32)
    g.vtm_d = scratch("vtm_d", [T, 640], BF16)
    g.ybr4 = scratch("ybr2", [2, 16, 128, HALF], BF16)
    g.rw_d = scratch("rw_d", [8, 4, 128, T], F32)
    g.u2_d = scratch("u2_d", [KC, 128, HALF], BF16)
    g.cmb_d = scratch("cmb_d", [32, HALF], F32)

    k = KB(nc)
    g.k = k
    g.ident = k.sb("ident", [128, 128], F32)
    g.identb = k.sb("identb", [128, 128], BF16)
    g.ones = k.sb("ones", [128, 512], F32)
    g.onesb = k.sb("onesb", [128, 128], BF16)
    g.modT = k.sb("modT", [128, 4, 96, 3], F32)
    g.sel = k.sb("sel", [128, 2], F32)
    g.rconst = R("const")
    g.rmod = R("mod")
    k.dma(g.ident[:], g.C["ident"], writes=[g.rconst])
    k.dma(g.sel[:], g.sel_in, writes=[g.rconst])
    k.cp(g.identb[:], g.ident[:], [g.rconst], [g.rconst])
    k.memset(g.ones[:], 1.0, writes=[g.rconst], eng="dve")
    k.memset(g.onesb[:], 1.0, writes=[g.rconst], eng="dve")
    g.pd = [k.ps("pd%d" % i, [128, 1024], F32) for i in range(4)]
    g.pr = [[R("ps%d_%d" % (i, h)) for h in range(2)] for i in range(4)]
    g.pi = 0

    def psb():
        i = g.pi
        g.pi = (g.pi + 1) % 8
        t = g.pd[i // 2]
        h = i % 2
        return t, h * 512, g.pr[i // 2][h]

    def psd():
        if g.pi % 2:
            g.pi = (g.pi + 1) % 8
        i = g.pi // 2
        g.pi = (g.pi + 2) % 8
        return g.pd[i], g.pr[i]

    g.psb = psb
    g.psd = psd

    prologue(g)
    if stop == "prologue":
        return finish(g)
    for l in range(nl):
        last = (l == DEPTH - 1)
        phase_inproj(g, l)
        if stop == "inproj":
            return finish(g)
        mix_ret(g, l)
        if stop == "ret":
            return finish(g)
        mix_gqa(g, l)
        if stop == "gqa":
            return finish(g)
        mix_lru(g, l)
        if stop == "lru":
            return finish(g)
        mix_rwkv(g, l)
        if stop == "rwkv":
            return finish(g)
        phase_merge(g, l, last)
        if stop == "merge":
            return finish(g)
        phase_moe(g, l, last, n_exp)
    epilogue(g)
    return finish(g)


def finish(g):
    g.k.finish()
    return g.nc


def mod(g, l, which, c, j):
    return g.modT[:, l, which * 16 + c, j:j + 1]


def prologue(g):
    k, nc, W = g.k, g.nc, g.W
    with ExitStack() as st:
        c2s = k.sb("c2s", [3, D], F32, st)
        scT = k.sb("scT", [128, 16, 3], F32, st)
        modtm = k.sb("modtm", [3, 12288], F32, st)
        bada = k.sb("bada", [3, 12288], F32, st)
        wst = [k.sb("wst%d" % i, [128, 16, 512], F32, st) for i in range(2)]
        rw = [R() for _ in range(2)]
        rc2, rscT, rmt, rba = R(), R(), R(), R()
        k.dma(c2s[:], g.c2, writes=[rc2])
        k.act(c2s[:], c2s[:], AF.Silu, reads=[rc2], writes=[rc2])
        pt, c0, rp = g.psb()
        for c in range(16):
            k.tr(pt[:, c0 + c * 3:c0 + c * 3 + 3], c2s[0:3, c * 128:(c + 1) * 128], g.ident[0:3, 0:3],
                 reads=[rc2, g.rconst], writes=[rp])
        k.cp(scT[:].rearrange("p c j -> p (c j)"), pt[:, c0:c0 + 48], [rp], [rscT])
        gi = 0
        for l in range(g.nl):
            k.dma(bada[:], W["b_ada"][l, :].partition_broadcast(3), writes=[rba])
            for grp in range(24):
                b = gi % 2
                gi += 1
                k.dma(wst[b][:], W["w_ada"][l, :, grp * 512:(grp + 1) * 512].rearrange("(c p) n -> p c n", p=128),
                      writes=[rw[b]])
                pt, c0, rp = g.psb()
                for c in range(16):
                    k.mm(pt[0:3, c0:c0 + 512], scT[:, c, :], wst[b][:, c, :], start=(c == 0), stop=(c == 15),
                         reads=[rscT, rw[b]], writes=[rp])
                k.tt(modtm[:, grp * 512:(grp + 1) * 512], pt[0:3, c0:c0 + 512], bada[:, grp * 512:(grp + 1) * 512],
                     ALU.add, reads=[rp, rba], writes=[rmt])
            pt, c0, rp = g.psb()
            for j in range(96):
                k.tr(pt[:, c0 + j * 3:c0 + j * 3 + 3], modtm[0:3, j * 128:(j + 1) * 128], g.ident[0:3, 0:3],
                     reads=[rmt, g.rconst], writes=[rp])
            k.cp(g.modT[:, l, :, :].rearrange("p c j -> p (c j)"), pt[:, c0:c0 + 288], [rp], [g.rmod])
            for which in (1, 4):
                sl = g.modT[:, l, which * 16:(which + 1) * 16, :]
                k.ts(sl, sl, 1.0, None, ALU.add, reads=[g.rmod], writes=[g.rmod])
        k.barrier()
    with ExitStack() as st:
        xt = [k.sb("xt%d" % i, [128, D], F32, st) for i in range(2)]
        xT = [k.sb("xT%d" % i, [128, 16, 128], F32, st) for i in range(2)]
        rxt = [R(), R()]
        rxT = [R(), R()]
        g.rxs = R("xs_d")
        g.rxo = R("xown")
        for i in range(NT + HALF // 128):
            b = i % 2
            if i < NT:
                k.dma(xt[b][:], g.xin[i * 128:(i + 1) * 128, :], writes=[rxt[b]])
            else:
                k.dma(xt[b][:], g.xown_in[(i - NT) * 128:(i - NT + 1) * 128, :], writes=[rxt[b]])
            for q in range(4):
                pt, c0, rp = g.psb()
                for cc in range(4):
                    c = q * 4 + cc
                    k.tr(pt[:, c0 + cc * 128:c0 + (cc + 1) * 128], xt[b][:, c * 128:(c + 1) * 128], g.ident[:],
                         reads=[rxt[b], g.rconst], writes=[rp])
                dst = xT[b][:, q * 4:(q + 1) * 4, :].rearrange("p c t -> p (c t)")
                if q % 2 == 0:
                    k.cp(dst, pt[:, c0:c0 + 512], [rp], [rxT[b]])
                else:
                    k.cp(dst, pt[:, c0:c0 + 512], [rp], [rxT[b]], eng="act")
            if i < NT:
                h_, lt_ = (i * 128) // HALF, (i * 128) % HALF
                k.dma(g.xs4[h_, :, :, lt_:lt_ + 128].rearrange("c p t -> p c t"), xT[b][:], reads=[rxT[b]],
                      writes=[g.rxs])
            else:
                lt_ = (i - NT) * 128
                k.dma(g.xo3[:, :, lt_:lt_ + 128].rearrange("c p t -> p c t"), xT[b][:], reads=[rxT[b]],
                      writes=[g.rxo])
        k.barrier()


def phase_inproj(g, l):
    k, nc, W = g.k, g.nc, g.W
    with ExitStack() as st:
        u1T = k.sb("u1T", [128, 16, T], BF16, st)
        ru1 = R("u1T")
        xb = [k.sb("xb%d" % i, [128, 16, 512], F32, st) for i in range(2)]
        rxb = [R(), R()]
        for bi, (t0, n) in enumerate(BLKS):
            j = 1 if t0 == 0 else 0
            b = bi % 2
            for (h_, lt_, ln_, off_) in nat_split(t0, n):
                k.dma(xb[b][:, :, off_:off_ + ln_], g.xs4[h_, :, :, lt_:lt_ + ln_].rearrange("c p t -> p c t"),
                      reads=[g.rxs], writes=[rxb[b]])
            for c in range(16):
                k.act(u1T[:, c, t0:t0 + n], xb[b][:, c, 0:n], AF.Identity, bias=mod(g, l, 0, c, j),
                      scale=mod(g, l, 1, c, j), reads=[rxb[b], g.rmod], writes=[ru1])
        wbf = [k.sb("wbf%d" % i, [128, 16, 512], BF16, st) for i in range(2)]
        rwb = [R(), R()]
        stg = [k.sb("stg%d" % i, [128, 512], F32, st) for i in range(4)]
        rstg = [R() for _ in range(4)]
        g.rp_d = R("p_d")
        g.rvtm = R("vtm_d")
        win = W["w_in"]
        si = 0
        groups = [CH_NAMES[i:i + 4] for i in range(0, NCHUNK, 4)]
        for gi, grp in enumerate(groups):
            b = gi % 2
            for ci, name in enumerate(grp):
                off = 0
                for (c0_, w_) in CHUNKS[name]:
                    k.dma(wbf[b][:, :, ci * 128 + off:ci * 128 + off + w_],
                          win[l, :, c0_:c0_ + w_].rearrange("(c p) n -> p c n", p=128), writes=[rwb[b]], issuer="pool")
                    off += w_
            for (t0, n) in BLKS:
                for ci, name in enumerate(grp):
                    M = ch_width(name)
                    pt, c0, rp = g.psb()
                    for c in range(16):
                        k.mm(pt[0:M, c0:c0 + n], wbf[b][:, c, ci * 128:ci * 128 + M], u1T[:, c, t0:t0 + n],
                             start=(c == 0), stop=(c == 15), reads=[rwb[b], ru1], writes=[rp])
                    s = si % 4
                    si += 1
                    if si % 2:
                        k.cp(stg[s][0:M, 0:n], pt[0:M, c0:c0 + n], [rp], [rstg[s]])
                    else:
                        k.cp(stg[s][0:M, 0:n], pt[0:M, c0:c0 + n], [rp], [rstg[s]], eng="act")
                    k.dma(g.p_d[CH_ID[name], 0:M, t0:t0 + n], stg[s][0:M, 0:n], reads=[rstg[s]], writes=[g.rp_d])
        vst = [k.sb("vst%d" % i, [128, 640], BF16, st) for i in range(2)]
        rvst = [R(), R()]
        b = len(groups) % 2
        k.dma(wbf[b][:, :, 0:512], win[l, :, 1024:1536].rearrange("(c p) n -> p c n", p=128), writes=[rwb[b]],
              issuer="pool")
        b2 = 1 - b
        k.dma(wbf[b2][:, :, 0:128], win[l, :, 2688:2816].rearrange("(c p) n -> p c n", p=128), writes=[rwb[b2]],
              issuer="pool")
        for i in range(NT):
            vb = i % 2
            pt, c0, rp = g.psb()
            for c in range(16):
                k.mm(pt[:, c0:c0 + 512], u1T[:, c, i * 128:(i + 1) * 128], wbf[b][:, c, 0:512], start=(c == 0),
                     stop=(c == 15), reads=[rwb[b], ru1], writes=[rp])
            k.cp(vst[vb][:, 0:512], pt[:, c0:c0 + 512], [rp], [rvst[vb]])
            pt, c0, rp = g.psb()
            for c in range(16):
                k.mm(pt[:, c0:c0 + 128], u1T[:, c, i * 128:(i + 1) * 128], wbf[b2][:, c, 0:128], start=(c == 0),
                     stop=(c == 15), reads=[rwb[b2], ru1], writes=[rp])
            k.cp(vst[vb][:, 512:640], pt[:, c0:c0 + 128], [rp], [rvst[vb]], eng="act")
            k.dma(g.vtm_d[i * 128:(i + 1) * 128, :], vst[vb][:], reads=[rvst[vb]], writes=[g.rvtm])
        k.barrier()


def epilogue(g):
    k = g.k
    with ExitStack() as st:
        xb = [k.sb("exb%d" % i, [128, 16, 128], F32, st) for i in range(2)]
        ot = [k.sb("eot%d" % i, [128, D], F32, st) for i in range(2)]
        rxb = [R(), R()]
        rot = [R(), R()]
        for i in range(16):
            b = i % 2
            t0 = LC + i * 128
            k.dma(xb[b][:], g.xs4[t0 // HALF, :, :, (t0 % HALF):(t0 % HALF) + 128].rearrange("c p t -> p c t"),
                  reads=[g.rxs], writes=[rxb[b]])
            for q in range(4):
                pt, c0, rp = g.psb()
                for cc in range(4):
                    c = q * 4 + cc
                    k.tr(pt[:, c0 + cc * 128:c0 + (cc + 1) * 128], xb[b][:, c, :], g.ident[:],
                         reads=[rxb[b], g.rconst], writes=[rp])
                if q % 2 == 0:
                    k.cp(ot[b][:, q * 512:(q + 1) * 512], pt[:, c0:c0 + 512], [rp], [rot[b]])
                else:
                    k.cp(ot[b][:, q * 512:(q + 1) * 512], pt[:, c0:c0 + 512], [rp], [rot[b]], eng="act")
            k.dma(g.out[i * 128:(i + 1) * 128, :], ot[b][:], reads=[rot[b]])
        k.barrier()


SEGS = [(0, LC), (LC, T)]


def rev_ap(t, a, b, p0=0, p1=128, width=T):
    return bass.AP(t, p0 * width + (b - 1), [[width, p1 - p0], [-1, b - a]])


def small_consts(g, st):
    k = g.k
    if not hasattr(g, "_od128"):
        pass
    od = k.sb("od128", [128, 128], F32, st)
    r = R()
    k.memset(od[:], 1.0 / 128.0, writes=[r], eng="dve")
    return od, r


def rope(g, st_tiles, xT, rx, cosT, sinT, Pb, rtab, outb, rout):
    k = g.k
    xb16, rxb16, t1, rt1, t2, rt2 = st_tiles
    for bi, (t0, n) in enumerate(BLKS):
        b = bi % 2
        k.act(xb16[b][:, 0:n], xT[:, t0:t0 + n], AF.Copy, reads=[rx], writes=[rxb16[b]])
        pt, c0, rp = g.psb()
        k.mm(pt[:, c0:c0 + n], Pb[:], xb16[b][:, 0:n], reads=[rtab, rxb16[b]], writes=[rp])
        k.tt(t1[b][:, 0:n], xT[:, t0:t0 + n], cosT[:, t0:t0 + n], ALU.mult, reads=[rx, rtab], writes=[rt1[b]],
             eng="pool")
        k.tt(t2[b][:, 0:n], pt[:, c0:c0 + n], sinT[:, t0:t0 + n], ALU.mult, reads=[rp, rtab], writes=[rt2[b]])
        k.tt(outb[:, t0:t0 + n], t1[b][:, 0:n], t2[b][:, 0:n], ALU.add, reads=[rt1[b], rt2[b]], writes=[rout])


def rope_tiles(g, st, pfx):
    k = g.k
    xb16 = [k.sb(pfx + "xb16_%d" % i, [128, 512], BF16, st) for i in range(2)]
    t1 = [k.sb(pfx + "t1_%d" % i, [128, 512], F32, st) for i in range(2)]
    t2 = [k.sb(pfx + "t2_%d" % i, [128, 512], F32, st) for i in range(2)]
    return (xb16, [R(), R()], t1, [R(), R()], t2, [R(), R()])


def mix_ret(g, l):
    k, W, C = g.k, g.W, g.C
    with ExitStack() as st:
        cosT = k.sb("rcos", [128, T], F32, st)
        sinT = k.sb("rsin", [128, T], F32, st)
        Pf = k.sb("rPf", [128, 128], F32, st)
        Pb = k.sb("rPb", [128, 128], BF16, st)
        tab = k.sb("rtab", [128, 6, 128], F32, st)
        cj = k.sb("rcj", [128, 2], F32, st)
        lg = k.sb("rlg", [128, 8], F32, st)
        gnw = k.sb("rgnw", [128, 4], F32, st)
        gnb = k.sb("rgnb", [128, 4], F32, st)
        epsc = k.sb("repsc", [128, 1], F32, st)
        rtab = R()
        k.dma(cosT[:], C["ret_cos"], writes=[rtab])
        k.dma(sinT[:], C["ret_sin"], writes=[rtab])
        k.dma(Pf[:], C["ret_P"], writes=[rtab])
        k.dma(tab[:], C["ret_tab"], writes=[rtab])
        k.dma(cj[:], C["ret_cj"], writes=[rtab])
        k.dma(lg[:], W["ret_decay_logit"][l].rearrange("a b -> (a b)").partition_broadcast(128), writes=[rtab])
        k.dma(gnw[:], W["ret_gn_w"][l].rearrange("(h p) -> p h", p=128), writes=[rtab], allow_slow_non_contiguous=True)
        k.dma(gnb[:], W["ret_gn_b"][l].rearrange("(h p) -> p h", p=128), writes=[rtab], allow_slow_non_contiguous=True)
        k.cp(Pb[:], Pf[:], [rtab], [rtab])
        k.memset(epsc[:], 1e-5, writes=[rtab], eng="dve")
        k.act(lg[:], lg[:], AF.Exp, scale=-1.0, reads=[rtab], writes=[rtab])
        k.ts(lg[:], lg[:], 1.0, None, ALU.add, reads=[rtab], writes=[rtab])
        k.act(lg[:], lg[:], AF.Ln, reads=[rtab], writes=[rtab])
        k.ts(lg[:], lg[:], -1.0, None, ALU.mult, reads=[rtab], writes=[rtab])
        od, rod = small_consts(g, st)
        rt = rope_tiles(g, st, "r")
        qT = k.sb("rqT", [128, T], F32, st)
        kT = k.sb("rkT", [128, T], F32, st)
        gT = k.sb("rgT", [128, T], F32, st)
        qTr = k.sb("rqTr", [128, T], BF16, st)
        kTr = k.sb("rkTr", [128, T], BF16, st)
        sg = k.sb("rsg", [128, T], BF16, st)
        ktm = k.sb("rktm", [128, NT, 128], BF16, st)
        vtm = k.sb("rvtm", [128, NT, 128], BF16, st)
        Sall = [k.sb("rSall%d" % d, [128, NT, 128], BF16, st) for d in range(2)]
        S = k.sb("rS", [128, 128], F32, st)
        DT = k.sb("rDT", [128, 128], F32, st)
        tmpa = k.sb("rtmpa", [128, 128], F32, st)
        Gd = [k.sb("rG%d" % d, [128, 128], BF16, st) for d in range(2)]
        Gt = k.sb("rGt", [128, 128], F32, st)
        cdir = k.sb("rcdir", [128, 4], F32, st)
        kw = [k.sb("rkw%d" % i, [128, 128], BF16, st) for i in range(2)]
        Pm = [k.sb("rPm%d" % i, [128, 128], BF16, st) for i in range(2)]
        qw = [[k.sb("rqw%d_%d" % (d, i), [128, 128], BF16, st) for i in range(2)] for d in range(2)]
        y = k.sb("ry", [128, T], F32, st)
        hn = [k.sb("rhn%d" % i, [128, 512], F32, st) for i in range(4)]
        yo = [k.sb("ryo%d" % i, [128, 512], BF16, st) for i in range(2)]
        rq, rk, rg_, rqr, rkr, rsg, rktm, rvtm = [R() for _ in range(8)]
        rSall = [R(), R()]
        rS, rDT, rtmpa, rGt, rcd, ry = [R() for _ in range(6)]
        rG = [R(), R()]
        rkw = [R(), R()]
        rPm = [R(), R()]
        rqw = [[R(), R()], [R(), R()]]
        rhn = [R() for _ in range(4)]
        ryo = [R(), R()]
        g.rybr = getattr(g, "rybr", None) or R("ybr")
        for h in range(4):
            k.dma(qT[:], g.p_d[CH_ID["ret_q%d" % h]], reads=[g.rp_d], writes=[rq])
            k.dma(kT[:], g.p_d[CH_ID["ret_k%d" % h]], reads=[g.rp_d], writes=[rk])
            k.dma(gT[:], g.p_d[CH_ID["ret_g%d" % h]], reads=[g.rp_d], writes=[rg_])
            k.dma(vtm[:], g.vtm_d[:, h * 128:(h + 1) * 128].rearrange("(i p) e -> p i e", p=128), reads=[g.rvtm],
                  writes=[rvtm])
            rope(g, rt, qT, rq, cosT, sinT, Pb, rtab, qTr, rqr)
            rope(g, rt, kT, rk, cosT, sinT, Pb, rtab, kTr, rkr)
            k.act(sg[:], gT[:], AF.Silu, reads=[rg_], writes=[rsg])
            for i in range(NT):
                pt, c0, rp = g.psb()
                ptb = pt.bitcast(BF16)
                k.tr(ptb[:, 2 * c0:2 * c0 + 128], kTr[:, i * 128:(i + 1) * 128], g.identb[:], reads=[rkr, g.rconst],
                     writes=[rp])
                if i % 2:
                    k.cp(ktm[:, i, :], ptb[:, 2 * c0:2 * c0 + 128], [rp], [rktm])
                else:
                    k.cp(ktm[:, i, :], ptb[:, 2 * c0:2 * c0 + 128], [rp], [rktm], eng="act")
            lgf = lg[:, h:h + 1]
            lgb = lg[:, 4 + h:5 + h]
            k.act(DT[:], tab[:, 0, :], AF.Exp, scale=lgf, reads=[rtab], writes=[rDT])
            k.tt(DT[:], DT[:], tab[:, 2, :], ALU.mult, reads=[rDT, rtab], writes=[rDT])
            k.act(tmpa[:], tab[:, 1, :], AF.Exp, scale=lgb, reads=[rtab], writes=[rtmpa])
            k.tt(tmpa[:], tmpa[:], tab[:, 3, :], ALU.mult, reads=[rtmpa, rtab], writes=[rtmpa])
            k.tt(DT[:], DT[:], tmpa[:], ALU.add, reads=[rDT, rtmpa], writes=[rDT])
            for d in range(2):
                k.act(Gt[:], tab[:, 4 + d, :], AF.Exp, scale=(lgf if d == 0 else lgb), reads=[rtab], writes=[rGt])
                k.ts(Gd[d][:], Gt[:], 128.0 ** -0.5, None, ALU.mult, reads=[rGt], writes=[rG[d]])
                k.act(cdir[:, d:d + 1], cj[:, d:d + 1], AF.Exp, scale=(lgf if d == 0 else lgb), reads=[rtab],
                      writes=[rcd])
                k.act(cdir[:, 2 + d:3 + d], (lgf if d == 0 else lgb), AF.Exp, scale=128.0, reads=[rtab],
                      writes=[rcd])
            orders = [list(range(NT)), [1, 0] + list(range(NT - 1, 1, -1))]
            ci = 0
            for d in range(2):
                k.memset(S[:], 0.0, writes=[rS], eng="dve")
                order = orders[d]
                for oi, n in enumerate(order):
                    k.cp(Sall[d][:, n, :], S[:], [rS], [rSall[d]], eng="act")
                    if oi == len(order) - 1:
                        break
                    b = ci % 2
                    ci += 1
                    k.ts(kw[b][:], ktm[:, n, :], cdir[:, d:d + 1], None, ALU.mult, reads=[rktm, rcd],
                         writes=[rkw[b]], eng="pool")
                    pt, c0, rp = g.psb()
                    k.mm(pt[:, c0:c0 + 128], kw[b][:], vtm[:, n, :], reads=[rkw[b], rvtm], writes=[rp])
                    k.stt(S[:], S[:], cdir[:, 2 + d:3 + d], pt[:, c0:c0 + 128], ALU.mult, ALU.add,
                          reads=[rS, rcd, rp], writes=[rS])
            pt = None
            for n in range(NT):
                b = n % 2
                ts_ = slice(n * 128, (n + 1) * 128)
                ps_, cs_, rps = g.psb()
                k.mm(ps_[:, cs_:cs_ + 128], kTr[:, ts_], qTr[:, ts_], reads=[rkr, rqr], writes=[rps])
                k.tt(Pm[b][:], ps_[:, cs_:cs_ + 128], DT[:], ALU.mult, reads=[rps, rDT], writes=[rPm[b]])
                for d in range(2):
                    k.tt(qw[d][b][:], qTr[:, ts_], Gd[d][:], ALU.mult, reads=[rqr, rG[d]], writes=[rqw[d][b]],
                         eng="pool")
                if n % 4 == 0:
                    pt, c0, rp = g.psb()
                o = c0 + (n % 4) * 128
                k.mm(pt[:, o:o + 128], vtm[:, n, :], Pm[b][:], start=True, stop=False, reads=[rvtm, rPm[b]],
                     writes=[rp])
                k.mm(pt[:, o:o + 128], Sall[0][:, n, :], qw[0][b][:], start=False, stop=False,
                     reads=[rSall[0], rqw[0][b]], writes=[rp])
                k.mm(pt[:, o:o + 128], Sall[1][:, n, :], qw[1][b][:], start=False, stop=True,
                     reads=[rSall[1], rqw[1][b]], writes=[rp])
                if n % 4 == 3 or n == NT - 1:
                    n0 = (n // 4) * 4
                    w_ = (n - n0 + 1) * 128
                    k.cp(y[:, n0 * 128:n0 * 128 + w_], pt[:, c0:c0 + w_], [rp], [ry], eng="act")
            for bi, (t0, n) in enumerate(BLKS):
                b = bi % 2
                sl = slice(t0, t0 + n)
                p1, c1, rp1 = g.psb()
                k.mm(p1[:, c1:c1 + n], od[:], y[:, sl], reads=[rod, ry], writes=[rp1])
                k.act(hn[0][:, 0:n], y[:, sl], AF.Square, reads=[ry], writes=[rhn[0]])
                p2, c2, rp2 = g.psb()
                k.mm(p2[:, c2:c2 + n], od[:], hn[0][:, 0:n], reads=[rod, rhn[0]], writes=[rp2])
                k.cp(hn[1][:, 0:n], p1[:, c1:c1 + n], [rp1], [rhn[1]], eng="act")
                k.tt(hn[2][:, 0:n], hn[1][:, 0:n], hn[1][:, 0:n], ALU.mult, reads=[rhn[1]], writes=[rhn[2]],
                     eng="pool")
                k.tt(hn[2][:, 0:n], p2[:, c2:c2 + n], hn[2][:, 0:n], ALU.subtract, reads=[rp2, rhn[2]],
                     writes=[rhn[2]])
                k.act(hn[2][:, 0:n], hn[2][:, 0:n], AF.Sqrt, bias=epsc[:, 0:1], scale=1.0, reads=[rhn[2], rtab],
                      writes=[rhn[2]])
                k.op("dve", lambda e, o_=hn[2][:, 0:n]: e.reciprocal(o_, o_), [rhn[2]], [rhn[2]])
                k.tt(hn[3][:, 0:n], y[:, sl], hn[1][:, 0:n], ALU.subtract, reads=[ry, rhn[1]], writes=[rhn[3]])
                k.tt(hn[3][:, 0:n], hn[3][:, 0:n], hn[2][:, 0:n], ALU.mult, reads=[rhn[3], rhn[2]],
                     writes=[rhn[3]])
                k.act(hn[3][:, 0:n], hn[3][:, 0:n], AF.Identity, bias=gnb[:, h:h + 1], scale=gnw[:, h:h + 1],
                      reads=[rhn[3], rtab], writes=[rhn[3]])
                k.tt(yo[b][:, 0:n], hn[3][:, 0:n], sg[:, sl], ALU.mult, reads=[rhn[3], rsg], writes=[ryo[b]])
                for (h_, lt_, ln_, off_) in nat_split(t0, n):
                    k.dma(g.ybr4[h_, 0 + h, :, lt_:lt_ + ln_], yo[b][:, off_:off_ + ln_], reads=[ryo[b]],
                          writes=[g.rybr])
        k.barrier()


def mix_gqa(g, l):
    k, W, C = g.k, g.W, g.C
    with ExitStack() as st:
        cosT = k.sb("gcos", [128, T], F32, st)
        sinT = k.sb("gsin", [128, T], F32, st)
        Pf = k.sb("gPf", [128, 128], F32, st)
        Pb = k.sb("gPb", [128, 128], BF16, st)
        mkf = k.sb("gmkf", [128, 2, 128], F32, st)
        mk = k.sb("gmk", [128, 2, 128], BF16, st)
        esk = k.sb("gesk", [128, 8], F32, st)
        rtab = R()
        k.dma(cosT[:], C["gqa_cos"], writes=[rtab])
        k.dma(sinT[:], C["gqa_sin"], writes=[rtab])
        k.dma(Pf[:], C["gqa_P"], writes=[rtab])
        k.dma(mkf[:], C["gqa_mask"], writes=[rtab])
        k.dma(esk[:], W["gqa_sink"][l].partition_broadcast(128), writes=[rtab])
        k.cp(Pb[:], Pf[:], [rtab], [rtab])
        k.cp(mk[:], mkf[:], [rtab], [rtab])
        k.act(esk[:], esk[:], AF.Exp, reads=[rtab], writes=[rtab])
        rt = rope_tiles(g, st, "g")
        xT = [k.sb("gxT%d" % i, [128, T], F32, st) for i in range(2)]
        rx = [R(), R()]
        qTr = [k.sb("gqTr%d" % c, [128, T], BF16, st) for c in range(4)]
        rqr = [R() for _ in range(4)]
        K2T = [k.sb("gK2T%d" % c, [128, T], BF16, st) for c in range(2)]
        rk2 = [R(), R()]
        V2 = [k.sb("gV2%d" % c, [128, NT, 128], BF16, st) for c in range(2)]
        rv2 = [R(), R()]
        yg = [k.sb("gyg%d" % c, [128, T], BF16, st) for c in range(4)]
        ryg = [R() for _ in range(4)]
        E = [k.sb("gE%d" % i, [128, 640], BF16, st) for i in range(3)]
        rE = [R() for _ in range(3)]
        rd = [k.sb("grd%d" % i, [128, 128], F32, st) for i in range(2)]
        rrd = [R(), R()]
        xi = 0
        for c in range(4):
            b = xi % 2
            xi += 1
            k.dma(xT[b][:], g.p_d[CH_ID["gqa_q%d" % c]], reads=[g.rp_d], writes=[rx[b]])
            rope(g, rt, xT[b], rx[b], cosT, sinT, Pb, rtab, qTr[c], rqr[c])
        for c in range(2):
            b = xi % 2
            xi += 1
            k.dma(xT[b][:], g.p_d[CH_ID["gqa_k%d" % c]], reads=[g.rp_d], writes=[rx[b]])
            rope(g, rt, xT[b], rx[b], cosT, sinT, Pb, rtab, K2T[c], rk2[c])
            for hh in range(2):
                k.dma(V2[c][:, :, hh * 64:(hh + 1) * 64],
                      g.vtm_d[:, 512 + c * 64:512 + (c + 1) * 64].rearrange("(i p) e -> p i e", p=128),
                      reads=[g.rvtm], writes=[rv2[c]])
        it = 0
        for h in range(8):
            gk = h // 4
            c = h // 2
            hp = h % 2
            prt = slice(hp * 64, hp * 64 + 64)
            for qt in range(NT):
                if qt < 2:
                    keys = [(0, None), (1, None)]
                else:
                    keys = [(0, None), (1, None)]
                    for s in (qt - 1, qt, qt + 1):
                        if 2 <= s <= NT - 1:
                            keys.append((s, s - qt))
                nk = len(keys)
                qs = slice(qt * 128, (qt + 1) * 128)
                pd2, rpd = g.psd()
                for idx, (s, rel_) in enumerate(keys):
                    k.mm(pd2[:, idx * 128:(idx + 1) * 128], K2T[gk][prt, s * 128:(s + 1) * 128], qTr[c][prt, qs],
                         reads=[rk2[gk], rqr[c]], writes=[rpd[idx // 4]])
                e = it % 3
                it += 1
                k.act(E[e][:, 0:nk * 128], pd2[:, 0:nk * 128], AF.Exp, scale=0.125, reads=rpd, writes=[rE[e]])
                for idx, (s, rel_) in enumerate(keys):
                    if rel_ == -1 or rel_ == 1:
                        mi = 0 if rel_ == -1 else 1
                        k.tt(E[e][:, idx * 128:(idx + 1) * 128], E[e][:, idx * 128:(idx + 1) * 128], mk[:, mi, :],
                             ALU.mult, reads=[rE[e], rtab], writes=[rE[e]], eng="pool")
                pt, c0, rp = g.psb()
                for idx, (s, rel_) in enumerate(keys):
                    k.mm(pt[:, c0:c0 + 128], V2[gk][:, s, :], E[e][:, idx * 128:(idx + 1) * 128], start=(idx == 0),
                         stop=(idx == nk - 1), reads=[rv2[gk], rE[e]], writes=[rp])
                for idx, (s, rel_) in enumerate(keys):
                    k.mm(pt[:, c0 + 128:c0 + 256], g.onesb[:], E[e][:, idx * 128:(idx + 1) * 128],
                         start=(idx == 0), stop=(idx == nk - 1), reads=[g.rconst, rE[e]], writes=[rp])
                b = it % 2
                k.ts(rd[b][:], pt[:, c0 + 128:c0 + 256], esk[:, h:h + 1], None, ALU.add, reads=[rp, rtab],
                     writes=[rrd[b]])
                k.op("dve", lambda e_, o_=rd[b][:]: e_.reciprocal(o_, o_), [rrd[b]], [rrd[b]])
                k.tt(yg[c][prt, qs], pt[prt, c0:c0 + 128], rd[b][prt, :], ALU.mult, reads=[rp, rrd[b]],
                     writes=[ryg[c]])
        g.rybr = getattr(g, "rybr", None) or R("ybr")
        for c in range(4):
            for h_ in range(2):
                k.dma(g.ybr4[h_, 4 + c], yg[c][:, h_ * HALF:(h_ + 1) * HALF], reads=[ryg[c]], writes=[g.rybr])
        k.barrier()


def mix_lru(g, l):
    k, W, C = g.k, g.W, g.C
    with ExitStack() as st:
        cw = k.sb("lcw", [128, 4, 4], F32, st)
        cb = k.sb("lcb", [128, 4], F32, st)
        gb = k.sb("lgb", [128, 2, 2, 4], F32, st)
        lm = k.sb("llm", [128, 2, 4], F32, st)
        sp16 = k.sb("lsp16", [128, 2, 4], F32, st)
        onec = k.sb("lonec", [128, 1], F32, st)
        bd = k.sb("lbd", [128, 16, 128], F32, st)
        bdb = k.sb("lbdb", [128, 16, 128], BF16, st)
        rc = R()
        rbd = R()
        for j_ in range(4):
            k.dma(cw[:, :, j_], W["lru_conv_w"][l, j_].rearrange("(c p) -> p c", p=128), writes=[rc],
                  allow_slow_non_contiguous=True)
        k.dma(cb[:], W["lru_conv_b"][l].rearrange("(c p) -> p c", p=128), writes=[rc],
              allow_slow_non_contiguous=True)
        for a_ in range(2):
            for b_ in range(2):
                k.dma(gb[:, a_, b_, :], W["lru_gate_b"][l, a_, b_].rearrange("(c p) -> p c", p=128), writes=[rc],
                      allow_slow_non_contiguous=True)
            k.dma(lm[:, a_, :], W["lru_lambda"][l, a_].rearrange("(c p) -> p c", p=128), writes=[rc],
                  allow_slow_non_contiguous=True)
        k.memset(onec[:], 1.0, writes=[rc], eng="dve")
        k.act(lm[:], lm[:], AF.Exp, scale=-1.0, reads=[rc], writes=[rc])
        k.ts(lm[:], lm[:], 1.0, None, ALU.add, reads=[rc], writes=[rc])
        k.act(lm[:], lm[:], AF.Ln, reads=[rc], writes=[rc])
        k.ts(sp16[:], lm[:], -16.0, None, ALU.mult, reads=[rc], writes=[rc])
        k.ts(lm[:], lm[:], -8.0, None, ALU.mult, reads=[rc], writes=[rc])
        k.memset(bd[:], 0.0, writes=[rbd], eng="dve")
        for d in range(2):
            for gt_ in range(2):
                for c in range(4):
                    idx = (d * 2 + gt_) * 4 + c
                    for hh in range(2):
                        k.dma(bd[hh * 64:(hh + 1) * 64, idx, hh * 64:(hh + 1) * 64],
                              W["lru_gate_w"][l, d, gt_, 2 * c + hh], writes=[rbd])
        k.cp(bdb[:], bd[:], [rbd], [rbd])
        x = k.sb("lx", [128, T], F32, st)
        gt = k.sb("lgt", [128, T], F32, st)
        xc = k.sb("lxc", [128, T], F32, st)
        xcb = k.sb("lxcb", [128, T], BF16, st)
        rg = k.sb("lrg", [128, T], F32, st)
        ig = k.sb("lig", [128, T], F32, st)
        aa = k.sb("laa", [128, T], F32, st)
        bt = k.sb("lbt", [128, T], F32, st)
        hh_ = [k.sb("lh%d" % d, [128, T], F32, st) for d in range(2)]
        yo = k.sb("lyo", [128, T], BF16, st)
        rx, rgt, rxc, rxcb, rrg, rig, raa, rbt, ryo = [R() for _ in range(9)]
        rh = [R(), R()]
        g.rybr = getattr(g, "rybr", None) or R("ybr")
        for c in range(4):
            k.dma(x[:], g.p_d[CH_ID["lru_x%d" % c]], reads=[g.rp_d], writes=[rx])
            k.dma(gt[:], g.p_d[CH_ID["lru_g%d" % c]], reads=[g.rp_d], writes=[rgt])
            for (a, b) in SEGS:
                k.ts(xc[:, a:b], x[:, a:b], cw[:, c, 2:3], cb[:, c:c + 1], ALU.mult, ALU.add, reads=[rx, rc],
                     writes=[rxc])
                k.stt(xc[:, a + 2:b], x[:, a:b - 2], cw[:, c, 0:1], xc[:, a + 2:b], ALU.mult, ALU.add,
                      reads=[rx, rc, rxc], writes=[rxc])
                k.stt(xc[:, a + 1:b], x[:, a:b - 1], cw[:, c, 1:2], xc[:, a + 1:b], ALU.mult, ALU.add,
                      reads=[rx, rc, rxc], writes=[rxc])
                k.stt(xc[:, a:b - 1], x[:, a + 1:b], cw[:, c, 3:4], xc[:, a:b - 1], ALU.mult, ALU.add,
                      reads=[rx, rc, rxc], writes=[rxc])
            k.act(xcb[:], xc[:], AF.Copy, reads=[rxc], writes=[rxcb])
            for d in range(2):
                for (t0, n) in BLKS:
                    sl = slice(t0, t0 + n)
                    p1, c1, rp1 = g.psb()
                    k.mm(p1[:, c1:c1 + n], bdb[:, (d * 2 + 0) * 4 + c, :], xcb[:, sl], reads=[rbd, rxcb],
                         writes=[rp1])
                    k.act(rg[:, sl], p1[:, c1:c1 + n], AF.Sigmoid, bias=gb[:, d, 0, c:c + 1], scale=1.0,
                          reads=[rp1, rc], writes=[rrg])
                    p2, c2, rp2 = g.psb()
                    k.mm(p2[:, c2:c2 + n], bdb[:, (d * 2 + 1) * 4 + c, :], xcb[:, sl], reads=[rbd, rxcb],
                         writes=[rp2])
                    k.act(ig[:, sl], p2[:, c2:c2 + n], AF.Sigmoid, bias=gb[:, d, 1, c:c + 1], scale=1.0,
                          reads=[rp2, rc], writes=[rig])
                k.act(aa[:], rg[:], AF.Exp, scale=lm[:, d, c:c + 1], reads=[rrg, rc], writes=[raa])
                k.act(bt[:], rg[:], AF.Exp, scale=sp16[:, d, c:c + 1], reads=[rrg, rc], writes=[rbt])
                k.act(bt[:], bt[:], AF.Sqrt, bias=onec[:, 0:1], scale=-1.0, reads=[rbt, rc], writes=[rbt])
                k.tt(ig[:], ig[:], xc[:], ALU.mult, reads=[rig, rxc], writes=[rig], eng="pool")
                k.tt(bt[:], bt[:], ig[:], ALU.mult, reads=[rbt, rig], writes=[rbt])
                h_ = hh_[d]
                if d == 0:
                    k.op("dve", lambda e, o_=h_[:], a_=aa[:], b_=bt[:]: e.tensor_tensor_scan(o_, a_, b_, 0.0, ALU.mult, ALU.add),
                         [raa, rbt], [rh[d]])
                else:
                    k.op("dve", lambda e, o_=rev_ap(h_, 0, LC), a_=rev_ap(aa, 0, LC), b_=rev_ap(bt, 0, LC):
                         e.tensor_tensor_scan(o_, a_, b_, 0.0, ALU.mult, ALU.add), [raa, rbt], [rh[d]])
                    k.op("dve", lambda e, o_=rev_ap(h_, LC, T), a_=rev_ap(aa, LC, T), b_=rev_ap(bt, LC, T), i_=h_[:, 0:1]:
                         e.tensor_tensor_scan(o_, a_, b_, i_, ALU.mult, ALU.add), [raa, rbt, rh[d]], [rh[d]])
            k.act(gt[:], gt[:], AF.Gelu, reads=[rgt], writes=[rgt])
            k.tt(hh_[0][:], hh_[0][:], hh_[1][:], ALU.add, reads=[rh[0], rh[1]], writes=[rh[0]], eng="pool")
            k.tt(yo[:], hh_[0][:], gt[:], ALU.mult, reads=[rh[0], rgt], writes=[ryo])
            for h_ in range(2):
                k.dma(g.ybr4[h_, 12 + c], yo[:, h_ * HALF:(h_ + 1) * HALF], reads=[ryo], writes=[g.rybr])
        k.barrier()


DECAY_SCALE = math.exp(-0.5)
RW_Q = ["r", "k", "kk", "b", "v", "lwf", "lwb", "g"]
MU_COLS = [(c * 128, 128) for c in range(12)] + [(1536, 96), (1632, 96), (1728, 96), (1824, 128), (1952, 128)]


def shift_mix(g, x, rx, tmp, rtmp, out, rout, M, om, hm, rmu):
    k = g.k
    for (a, b) in SEGS:
        k.cp(tmp[0:M, a:b - 1], x[0:M, a + 1:b], [rx], [rtmp], eng="pool")
        k.memset(tmp[0:M, b - 1:b], 0.0, writes=[rtmp], eng="pool")
        k.tt(tmp[0:M, a + 1:b], tmp[0:M, a + 1:b], x[0:M, a:b - 1], ALU.add, reads=[rtmp, rx], writes=[rtmp])
    k.ts(out[0:M, :], x[0:M, :], om, None, ALU.mult, reads=[rx, rmu], writes=[rout])
    k.stt(out[0:M, :], tmp[0:M, :], hm, out[0:M, :], ALU.mult, ALU.add, reads=[rtmp, rmu, rout], writes=[rout])


def dv(arr, d, seg, p0=0, p1=128):
    a, b = SEGS[seg]
    nch = (b - a) // C_RW
    if d == 0:
        return arr[p0:p1, a:b].rearrange("p (n c) -> p n c", c=C_RW)
    return bass.AP(arr, p0 * T + (b - 1), [[T, p1 - p0], [-C_RW, nch], [-1, C_RW]])


def chs(seg):
    a, b = SEGS[seg]
    return slice(a // C_RW, b // C_RW)


def mix_rwkv(g, l):
    rwkv_prep(g, l)
    rwkv_scan(g, l)


def rwkv_prep(g, l):
    k, W, C = g.k, g.W, g.C
    with ExitStack() as st:
        mu = k.sb("wmu", [128, 17], F32, st)
        om = k.sb("wom", [128, 17], F32, st)
        hm = k.sb("whm", [128, 17], F32, st)
        rmu = R()
        k.memset(mu[:], 0.0, writes=[rmu], eng="dve")
        for i, (c0, w) in enumerate(MU_COLS):
            k.dma(mu[0:w, i:i + 1], W["rwkv_mu"][l, c0:c0 + w].rearrange("(p o) -> p o", o=1), writes=[rmu])
        k.ts(om[:], mu[:], -1.0, 1.0, ALU.mult, ALU.add, reads=[rmu], writes=[rmu])
        k.ts(hm[:], mu[:], 0.5, None, ALU.mult, reads=[rmu], writes=[rmu])
        par = k.sb("wpar", [128, 8, 4], F32, st)
        rpar = R()
        srcs = [W["rwkv_w0"][l, 0], W["rwkv_w0"][l, 1], W["rwkv_a0"][l], W["rwkv_k_k"][l], W["rwkv_k_a"][l]]
        for i, s in enumerate(srcs):
            k.dma(par[:, i, :], s.rearrange("(c p) -> p c", p=128), writes=[rpar], allow_slow_non_contiguous=True)
        k.ts(par[:, 5, :], par[:, 4, :], -1.0, 1.0, ALU.mult, ALU.add, reads=[rpar], writes=[rpar])
        wup = k.sb("wwup", [96, 2, 512], BF16, st)
        aup = k.sb("waup", [96, 512], BF16, st)
        gup = k.sb("wgup", [128, 2, 512], BF16, st)
        bo = k.sb("wbo", [128, 128], F32, st)
        rw = R()
        for d in range(2):
            k.dma(wup[:, d, :], W["rwkv_w_up"][l, d], writes=[rw], issuer="pool")
        k.dma(aup[:], W["rwkv_a_up"][l], writes=[rw], issuer="pool")
        k.dma(gup[:], W["rwkv_g_up"][l].rearrange("(c p) n -> p c n", p=128), writes=[rw], issuer="pool")
        k.dma(bo[:], C["blk_ones"], writes=[rw])
        x = k.sb("wx", [128, T], F32, st)
        tmp = k.sb("wtmp", [128, T], F32, st)
        xs_ = k.sb("wxs", [128, T], F32, st)
        rx, rtmp, rxs = R(), R(), R()
        twd = [k.sb("wtwd%d" % d, [96, T], BF16, st) for d in range(2)]
        adb = k.sb("wadb", [96, T], BF16, st)
        sgd = k.sb("wsgd", [128, 2, T], BF16, st)
        rlo = R()
        for i, (name, M) in enumerate([("rw_wdf", 96), ("rw_wdb", 96), ("rw_ad", 96), ("rw_gd0", 128), ("rw_gd1", 128)]):
            mi = 12 + i
            k.dma(x[0:M, :], g.p_d[CH_ID[name], 0:M, :], reads=[g.rp_d], writes=[rx])
            shift_mix(g, x, rx, tmp, rtmp, xs_, rxs, M, om[0:M, mi:mi + 1], hm[0:M, mi:mi + 1], rmu)
            if i < 2:
                k.act(twd[i][:], xs_[0:96, :], AF.Tanh, reads=[rxs], writes=[rlo])
            elif i == 2:
                k.act(adb[:], xs_[0:96, :], AF.Copy, reads=[rxs], writes=[rlo])
            else:
                k.act(sgd[:, i - 3, :], xs_[:], AF.Sigmoid, reads=[rxs], writes=[rlo])
        names = ["r", "k", "v", "lwf", "lwb", "a", "gq", "kk", "t"]
        A = {n: k.sb("wA_" + n, [128, T], F32, st) for n in names}
        RA = {n: R() for n in names}
        g.rrw = getattr(g, "rrw", None) or R("rw_d")
        for c in range(4):
            for qi, (nm, pfx) in enumerate([("r", "rw_r"), ("k", "rw_k"), ("v", "rw_v")]):
                mi = qi * 4 + c
                k.dma(x[:], g.p_d[CH_ID["%s%d" % (pfx, c)]], reads=[g.rp_d], writes=[rx])
                shift_mix(g, x, rx, tmp, rtmp, A[nm], RA[nm], 128, om[:, mi:mi + 1], hm[:, mi:mi + 1], rmu)
            cs_ = slice(c * 128, (c + 1) * 128)
            for (t0, n) in BLKS:
                sl = slice(t0, t0 + n)
                for d in range(2):
                    pt, c0, rp = g.psb()
                    k.mm(pt[:, c0:c0 + n], wup[:, d, cs_], twd[d][:, sl], reads=[rw, rlo], writes=[rp])
                    nm = "lwf" if d == 0 else "lwb"
                    k.act(A[nm][:, sl], pt[:, c0:c0 + n], AF.Sigmoid, bias=par[:, d, c:c + 1], scale=1.0,
                          reads=[rp, rpar], writes=[RA[nm]])
                pt, c0, rp = g.psb()
                k.mm(pt[:, c0:c0 + n], aup[:, cs_], adb[:, sl], reads=[rw, rlo], writes=[rp])
                k.act(A["a"][:, sl], pt[:, c0:c0 + n], AF.Sigmoid, bias=par[:, 2, c:c + 1], scale=1.0,
                      reads=[rp, rpar], writes=[RA["a"]])
                pt, c0, rp = g.psb()
                k.mm(pt[:, c0:c0 + n], gup[:, 0, cs_], sgd[:, 0, sl], start=True, stop=False, reads=[rw, rlo],
                     writes=[rp])
                k.mm(pt[:, c0:c0 + n], gup[:, 1, cs_], sgd[:, 1, sl], start=False, stop=True, reads=[rw, rlo],
                     writes=[rp])
                k.cp(A["gq"][:, sl], pt[:, c0:c0 + n], [rp], [RA["gq"]])
            for nm in ("lwf", "lwb"):
                k.ts(A[nm][:], A[nm][:], -DECAY_SCALE, None, ALU.mult, reads=[RA[nm]], writes=[RA[nm]], eng="pool")
            k.ts(A["kk"][:], A["k"][:], par[:, 3, c:c + 1], None, ALU.mult, reads=[RA["k"], rpar], writes=[RA["kk"]])
            k.act(A["t"][:], A["kk"][:], AF.Square, reads=[RA["kk"]], writes=[RA["t"]])
            for (t0, n) in BLKS:
                sl = slice(t0, t0 + n)
                pt, c0, rp = g.psb()
                k.mm(pt[:, c0:c0 + n], bo[:], A["t"][:, sl], reads=[rw, RA["t"]], writes=[rp])
                k.act(tmp[:, sl], pt[:, c0:c0 + n], AF.Sqrt, reads=[rp], writes=[rtmp])
            k.ts(tmp[:], tmp[:], 1e-12, None, ALU.max, reads=[rtmp], writes=[rtmp])
            k.op("dve", lambda e, o_=tmp[:]: e.reciprocal(o_, o_), [rtmp], [rtmp])
            k.tt(A["kk"][:], A["kk"][:], tmp[:], ALU.mult, reads=[RA["kk"], rtmp], writes=[RA["kk"]])
            k.ts(A["t"][:], A["a"][:], par[:, 4, c:c + 1], par[:, 5, c:c + 1], ALU.mult, ALU.add,
                 reads=[RA["a"], rpar, RA["t"]], writes=[RA["t"]])
            k.tt(A["k"][:], A["k"][:], A["t"][:], ALU.mult, reads=[RA["k"], RA["t"]], writes=[RA["k"]], eng="pool")
            k.tt(A["a"][:], A["a"][:], A["kk"][:], ALU.mult, reads=[RA["a"], RA["kk"]], writes=[RA["a"]])
            for qi, nm in enumerate(["r", "k", "kk", "a", "v", "lwf", "lwb", "gq"]):
                k.dma(g.rw_d[qi, c], A[nm][:], reads=[RA[nm]], writes=[g.rrw])
        k.barrier()


def rwkv_scan(g, l):
    k, W, C = g.k, g.W, g.C
    with ExitStack() as st:
        mkf = k.sb("smkf", [128, 5, 128], F32, st)
        mk = k.sb("smk", [128, 5, 128], BF16, st)
        ist = k.sb("sist", [128, 64], F32, st)
        istb = k.sb("sistb", [128, 64], BF16, st)
        bo64 = k.sb("sbo64", [128, 128], F32, st)
        bo = k.sb("sbo", [128, 128], F32, st)
        par = k.sb("spar", [128, 3, 4], F32, st)
        epsc = k.sb("sepsc", [128, 1], F32, st)
        rcs = R()
        k.dma(mkf[:], C["rw_mask"], writes=[rcs])
        k.dma(ist[:], C["ist"], writes=[rcs])
        k.dma(bo[:], C["blk_ones"], writes=[rcs])
        k.cp(mk[:], mkf[:], [rcs], [rcs])
        k.cp(istb[:], ist[:], [rcs], [rcs])
        k.ts(bo64[:], bo[:], 1.0 / 64.0, None, ALU.mult, reads=[rcs], writes=[rcs])
        k.memset(epsc[:], 64e-5, writes=[rcs], eng="dve")
        for i, s in enumerate([W["rwkv_ln_w"][l], W["rwkv_ln_b"][l], W["rwkv_r_k"][l].rearrange("h d -> (h d)")]):
            k.dma(par[:, i, :], s.rearrange("(c p) -> p c", p=128), writes=[rcs], allow_slow_non_contiguous=True)
        nat = {n: k.sb("sN_" + n, [128, T], F32, st) for n in ["r", "k", "kk", "b", "v", "lw"]}
        rnat = {n: R() for n in nat}
        yd = [k.sb("syd%d" % d, [128, T], F32, st) for d in range(2)]
        ryd = [R(), R()]
        cum = k.sb("scum", [128, NCH, C_RW], F32, st)
        lwd = k.sb("slwd", [128, NCH, C_RW], F32, st)
        c0t = k.sb("sc0", [128, NCH], F32, st)
        eL = k.sb("seL", [128, NCH, C_RW], F32, st)
        eLx = k.sb("seLx", [128, NCH, C_RW], F32, st)
        rcum, rlwd, rc0, reL, reLx = [R() for _ in range(5)]
        enL, renL = cum, rcum
        tq, rtq = lwd, rlwd
        BD = {n: k.sb("sBD_" + n, [128, NCH, 128], BF16, st) for n in ["R", "A", "B", "K", "V", "BH", "KH"]}
        rBD = {n: R() for n in BD}
        for n in BD:
            k.memset(BD[n][:], 0.0, writes=[rBD[n]], eng="pool")
        Ybd = [k.sb("sYbd%d" % i, [128, 128], F32, st) for i in range(2)]
        rYbd = [R(), R()]
        for i in range(2):
            k.memset(Ybd[i][:], 0.0, writes=[rYbd[i]], eng="pool")
        H = k.sb("sH", [128, 64], F32, st)
        Hb = k.sb("sHb", [128, 64], BF16, st)
        rH, rHb = R(), R()
        NB = 3
        M2 = [k.sb("sM2_%d" % i, [128, 256], BF16, st) for i in range(NB * 2)]
        rM2 = [R() for _ in range(NB * 2)]
        XT = [k.sb("sXT_%d" % i, [128, 128], BF16, st) for i in range(NB * 2)]
        rXT = [R() for _ in range(NB * 2)]
        A3 = [k.sb("sA3_%d" % i, [128, 384], BF16, st) for i in range(NB)]
        rA3 = [R() for _ in range(NB)]
        Vst = [k.sb("sVst_%d" % i, [128, 64], BF16, st) for i in range(NB)]
        rVst = [R() for _ in range(NB)]
        BK = [k.sb("sBK_%d" % i, [128, 256], BF16, st) for i in range(NB)]
        rBK = [R() for _ in range(NB)]
        Bm = [k.sb("sBm_%d" % i, [128, 64], BF16, st) for i in range(2)]
        rBm = [R(), R()]
        Ub = [k.sb("sUb_%d" % i, [128, 64], BF16, st) for i in range(2)]
        rUb = [R(), R()]
        hn = [k.sb("shn%d" % i, [128, 512], F32, st) for i in range(4)]
        rhn = [R() for _ in range(4)]
        gq = eLx[:].rearrange("p n c -> p (n c)")
        rgq = reLx
        yo = [k.sb("syo%d" % i, [128, 512], BF16, st) for i in range(2)]
        ryo = [R(), R()]
        g.rybr = getattr(g, "rybr", None) or R("ybr")
        ev = 0
        for c in range(4):
            for qi, nm in enumerate(["r", "k", "kk", "b", "v"]):
                k.dma(nat[nm][:], g.rw_d[qi, c], reads=[g.rrw], writes=[rnat[nm]])
            for d in range(2):
                lw = nat["lw"]
                rlw = rnat["lw"]
                k.dma(lw[:], g.rw_d[5 + d, c], reads=[g.rrw], writes=[rlw])
                cumf = cum[:].rearrange("p n c -> p (n c)")
                for seg in range(2):
                    a, b = SEGS[seg]
                    k.cp(lwd[:, chs(seg), :], dv(lw, d, seg), [rlw], [rlwd], eng="pool")
                lwdf = lwd[:].rearrange("p n c -> p (n c)")
                k.op("dve", lambda e, o_=cumf, a_=g.ones[:, 0:1].to_broadcast([128, T]), b_=lwdf:
                     e.tensor_tensor_scan(o_, a_, b_, 0.0, ALU.mult, ALU.add), [rlwd, g.rconst], [rcum])
                k.tt(c0t[:], cum[:, :, 0], lwd[:, :, 0], ALU.subtract, reads=[rcum, rlwd], writes=[rc0])
                c0b = c0t[:].rearrange("p (n o) -> p n o", o=1).to_broadcast([128, NCH, C_RW])
                k.tt(cum[:], cum[:], c0b, ALU.subtract, reads=[rcum, rc0], writes=[rcum])
                k.tt(lwd[:], cum[:], lwd[:], ALU.subtract, reads=[rcum, rlwd], writes=[rlwd], eng="pool")
                k.act(eL[:], cum[:], AF.Exp, reads=[rcum], writes=[reL])
                k.act(eLx[:], lwd[:], AF.Exp, reads=[rlwd], writes=[reLx])
                k.act(cum[:], cum[:], AF.Exp, scale=-1.0, reads=[rcum], writes=[rcum])
                WCb = eL[:, :, C_RW - 1:C_RW].to_broadcast([128, NCH, C_RW])
                for seg in range(2):
                    cs_ = chs(seg)
                    for hh in range(2):
                        p0, p1 = hh * 64, hh * 64 + 64
                        ps_ = slice(p0, p1)
                        k.tt(BD["R"][ps_, cs_, ps_], dv(nat["r"], d, seg, p0, p1), eL[ps_, cs_, :], ALU.mult,
                             reads=[rnat["r"], reL], writes=[rBD["R"]])
                        k.stt(BD["A"][ps_, cs_, ps_], dv(nat["kk"], d, seg, p0, p1), -1.0, eLx[ps_, cs_, :],
                              ALU.mult, ALU.mult, reads=[rnat["kk"], reLx], writes=[rBD["A"]])
                        k.cp(BD["V"][ps_, cs_, ps_], dv(nat["v"], d, seg, p0, p1), [rnat["v"]], [rBD["V"]],
                             eng="act")
                    for (src, nb, nh) in (("b", "B", "BH"), ("k", "K", "KH")):
                        k.tt(tq[:, cs_, :], dv(nat[src], d, seg), enL[:, cs_, :], ALU.mult, reads=[rnat[src], renL],
                             writes=[rtq])
                        for hh in range(2):
                            ps_ = slice(hh * 64, hh * 64 + 64)
                            k.cp(BD[nb][ps_, cs_, ps_], tq[ps_, cs_, :], [rtq], [rBD[nb]], eng="act")
                            k.tt(BD[nh][ps_, cs_, ps_], tq[ps_, cs_, :], WCb[ps_, cs_, :], ALU.mult,
                                 reads=[rtq, reL], writes=[rBD[nh]], eng="pool")
                k.memset(H[:], 0.0, writes=[rH], eng="dve")
                k.memset(Hb[:], 0.0, writes=[rHb], eng="dve")
                fin = {}
                def par_part(n):
                        i3 = n % NB
                        Ab, Bb, Kb, Rb = BD["A"][:, n, :], BD["B"][:, n, :], BD["K"][:, n, :], BD["R"][:, n, :]
                        Vb, BHb, KHb = BD["V"][:, n, :], BD["BH"][:, n, :], BD["KH"][:, n, :]
                        pa, ca, rpa = g.psb()
                        k.mm(pa[:, ca:ca + 128], Ab, Bb, reads=[rBD["A"], rBD["B"]], writes=[rpa])
                        k.mm(pa[:, ca + 128:ca + 256], Bb, Ab, reads=[rBD["A"], rBD["B"]], writes=[rpa])
                        mi = (n % NB) * 2
                        k.tt(M2[mi][:], pa[:, ca:ca + 256], mk[:, 0:2, :].rearrange("p a b -> p (a b)"), ALU.mult,
                             reads=[rpa, rcs], writes=[rM2[mi]])
                        xi = (n % NB) * 2
                        k.tt(XT[xi][:], M2[mi][:, 128:256], g.identb[:], ALU.add, reads=[rM2[mi], g.rconst],
                             writes=[rXT[xi]], eng="pool")
                        curM, rcurM, curX, rcurX = M2[mi], rM2[mi], XT[xi], rXT[xi]
                        for s in range(5):
                            nm_, rnm_ = (M2[mi + 1], rM2[mi + 1]) if curM is M2[mi] else (M2[mi], rM2[mi])
                            nx_, rnx_ = (XT[xi + 1], rXT[xi + 1]) if curX is XT[xi] else (XT[xi], rXT[xi])
                            pm, cm, rpm = g.psb()
                            k.mm(pm[:, cm:cm + 128], curM[:, 128:256], curM[:, 0:128], reads=[rcurM], writes=[rpm])
                            wcols = 128
                            if s < 4:
                                k.mm(pm[:, cm + 128:cm + 256], curM[:, 0:128], curM[:, 128:256], reads=[rcurM],
                                     writes=[rpm])
                                wcols = 256
                            k.act(nm_[:, 0:wcols], pm[:, cm:cm + wcols], AF.Copy, reads=[rpm], writes=[rnm_])
                            px, cx, rpx = g.psb()
                            k.mm(px[:, cx:cx + 128], nm_[:, 0:128], curX[:], reads=[rnm_, rcurX], writes=[rpx])
                            k.tt(nx_[:], px[:, cx:cx + 128], curX[:], ALU.add, reads=[rpx, rcurX], writes=[rnx_])
                            curM, rcurM, curX, rcurX = nm_, rnm_, nx_, rnx_
                        pb, cb_, rpb = g.psb()
                        k.mm(pb[:, cb_:cb_ + 128], Kb, Ab, reads=[rBD["K"], rBD["A"]], writes=[rpb])
                        k.mm(pb[:, cb_ + 128:cb_ + 256], Bb, Rb, reads=[rBD["B"], rBD["R"]], writes=[rpb])
                        k.mm(pb[:, cb_ + 256:cb_ + 384], Kb, Rb, reads=[rBD["K"], rBD["R"]], writes=[rpb])
                        k.tt(A3[i3][:], pb[:, cb_:cb_ + 384], mk[:, 2:5, :].rearrange("p a b -> p (a b)"), ALU.mult,
                             reads=[rpb, rcs], writes=[rA3[i3]])
                        pv, cv, rpv = g.psb()
                        k.mm(pv[:, cv:cv + 64], Vb, istb[:], reads=[rBD["V"], rcs], writes=[rpv])
                        k.act(Vst[i3][:], pv[:, cv:cv + 64], AF.Copy, reads=[rpv], writes=[rVst[i3]])
                        ptt, ct, rpt = g.psb()
                        ptb = ptt.bitcast(BF16)
                        k.tr(ptb[:, 2 * ct:2 * ct + 128], BHb, g.identb[:], reads=[rBD["BH"], g.rconst], writes=[rpt])
                        k.tr(ptb[:, 2 * ct + 128:2 * ct + 256], KHb, g.identb[:], reads=[rBD["KH"], g.rconst],
                             writes=[rpt])
                        k.act(BK[i3][:], ptb[:, 2 * ct:2 * ct + 256], AF.Copy, reads=[rpt], writes=[rBK[i3]])
                        fin[n] = (curX, rcurX)
                def seq_part(n):
                        i3 = n % NB
                        Ab, Rb = BD["A"][:, n, :], BD["R"][:, n, :]
                        curX, rcurX = fin.pop(n)
                        b2 = n % 2
                        p1_, c1, rp1 = g.psb()
                        k.mm(p1_[:, c1:c1 + 64], Ab, Hb[:], start=True, stop=False, reads=[rBD["A"], rHb], writes=[rp1])
                        k.mm(p1_[:, c1:c1 + 64], A3[i3][:, 0:128], Vst[i3][:], start=False, stop=True,
                             reads=[rA3[i3], rVst[i3]], writes=[rp1])
                        k.act(Bm[b2][:], p1_[:, c1:c1 + 64], AF.Copy, reads=[rp1], writes=[rBm[b2]])
                        p2_, c2, rp2 = g.psb()
                        k.mm(p2_[:, c2:c2 + 64], curX[:], Bm[b2][:], reads=[rcurX, rBm[b2]], writes=[rp2])
                        k.cp(Ub[b2][:], p2_[:, c2:c2 + 64], [rp2], [rUb[b2]])
                        py, cy, rpy = g.psb()
                        k.mm(py[:, cy:cy + 64], Rb, Hb[:], start=True, stop=False, reads=[rBD["R"], rHb], writes=[rpy])
                        k.mm(py[:, cy:cy + 64], A3[i3][:, 128:256], Ub[b2][:], start=False, stop=False,
                             reads=[rA3[i3], rUb[b2]], writes=[rpy])
                        k.mm(py[:, cy:cy + 64], A3[i3][:, 256:384], Vst[i3][:], start=False, stop=True,
                             reads=[rA3[i3], rVst[i3]], writes=[rpy])
                        k.cp(Ybd[b2][0:64, 0:64], py[0:64, cy:cy + 64], [rpy], [rYbd[b2]], eng="act")
                        k.cp(Ybd[b2][64:128, 64:128], py[64:128, cy:cy + 64], [rpy], [rYbd[b2]], eng="act")
                        ph, ch_, rph = g.psb()
                        k.mm(ph[:, ch_:ch_ + 64], BK[i3][:, 0:128], Ub[b2][:], start=True, stop=False,
                             reads=[rBK[i3], rUb[b2]], writes=[rph])
                        k.mm(ph[:, ch_:ch_ + 64], BK[i3][:, 128:256], Vst[i3][:], start=False, stop=True,
                             reads=[rBK[i3], rVst[i3]], writes=[rph])
                        k.stt(H[:], H[:], eL[:, n, C_RW - 1:C_RW], ph[:, ch_:ch_ + 64], ALU.mult, ALU.add,
                              reads=[rH, reL, rph], writes=[rH])
                        k.cp(Hb[:], H[:], [rH], [rHb])
                        po, co, rpo = g.psb()
                        k.mm(po[:, co:co + 64], Ybd[b2][:], ist[:], reads=[rYbd[b2], rcs], writes=[rpo])
                        if d == 0:
                            dst = yd[d][:, n * 64:(n + 1) * 64]
                        elif n < 4:
                            dst = rev_ap(yd[d], LC - (n + 1) * 64, LC - n * 64)
                        else:
                            dst = rev_ap(yd[d], T - (n - 3) * 64, T - (n - 4) * 64)
                        k.cp(dst, po[:, co:co + 64], [rpo], [ryd[d]], eng="pool" if False else "dve")
                par_part(0)
                for n in range(NCH):
                    if n + 1 < NCH:
                        par_part(n + 1)
                    seq_part(n)
            k.dma(gq, g.rw_d[7, c], reads=[g.rrw], writes=[rgq])
            k.tt(yd[0][:], yd[0][:], yd[1][:], ALU.add, reads=[ryd[0], ryd[1]], writes=[ryd[0]], eng="pool")
            k.stt(yd[1][:], nat["r"][:], par[:, 2, c:c + 1], nat["k"][:], ALU.mult, ALU.mult,
                  reads=[rnat["r"], rnat["k"], rcs, ryd[1]], writes=[ryd[1]])
            y = yd[0]
            ry = ryd[0]
            for bi, (t0, n) in enumerate(BLKS):
                b = bi % 2
                sl = slice(t0, t0 + n)
                p1, c1, rp1 = g.psb()
                k.mm(p1[:, c1:c1 + n], bo64[:], y[:, sl], reads=[rcs, ry], writes=[rp1])
                k.act(hn[0][:, 0:n], y[:, sl], AF.Square, reads=[ry], writes=[rhn[0]])
                p2, c2, rp2 = g.psb()
                k.mm(p2[:, c2:c2 + n], bo64[:], hn[0][:, 0:n], reads=[rcs, rhn[0]], writes=[rp2])
                k.cp(hn[1][:, 0:n], p1[:, c1:c1 + n], [rp1], [rhn[1]], eng="act")
                k.tt(hn[2][:, 0:n], hn[1][:, 0:n], hn[1][:, 0:n], ALU.mult, reads=[rhn[1]], writes=[rhn[2]],
                     eng="pool")
                k.tt(hn[2][:, 0:n], p2[:, c2:c2 + n], hn[2][:, 0:n], ALU.subtract, reads=[rp2, rhn[2]],
                     writes=[rhn[2]])
                k.act(hn[2][:, 0:n], hn[2][:, 0:n], AF.Sqrt, bias=epsc[:, 0:1], scale=1.0, reads=[rhn[2], rcs],
                      writes=[rhn[2]])
                k.op("dve", lambda e, o_=hn[2][:, 0:n]: e.reciprocal(o_, o_), [rhn[2]], [rhn[2]])
                k.tt(hn[3][:, 0:n], y[:, sl], hn[1][:, 0:n], ALU.subtract, reads=[ry, rhn[1]], writes=[rhn[3]])
                k.tt(hn[3][:, 0:n], hn[3][:, 0:n], hn[2][:, 0:n], ALU.mult, reads=[rhn[3], rhn[2]],
                     writes=[rhn[3]])
                k.act(hn[3][:, 0:n], hn[3][:, 0:n], AF.Identity, bias=par[:, 1, c:c + 1], scale=par[:, 0, c:c + 1],
                      reads=[rhn[3], rcs], writes=[rhn[3]])
                p3, c3, rp3 = g.psb()
                k.mm(p3[:, c3:c3 + n], bo[:], yd[1][:, sl], reads=[rcs, ryd[1]], writes=[rp3])
                k.tt(hn[0][:, 0:n], p3[:, c3:c3 + n], nat["v"][:, sl], ALU.mult, reads=[rp3, rnat["v"], rhn[0]],
                     writes=[rhn[0]])
                k.tt(hn[3][:, 0:n], hn[3][:, 0:n], hn[0][:, 0:n], ALU.add, reads=[rhn[3], rhn[0]],
                     writes=[rhn[3]], eng="pool")
                k.tt(yo[b][:, 0:n], hn[3][:, 0:n], gq[:, sl], ALU.mult, reads=[rhn[3], rgq], writes=[ryo[b]])
                for (h_, lt_, ln_, off_) in nat_split(t0, n):
                    k.dma(g.ybr4[h_, 8 + c, :, lt_:lt_ + ln_], yo[b][:, off_:off_ + ln_], reads=[ryo[b]],
                          writes=[g.rybr])
        k.barrier()


LN_EPS = 1e-5


def ln_block(g, z, rz, n, scr, rscr, lnw, lnb, rpar, od, rod, epsc, st_tiles):
    k = g.k
    mean, rmean, rstd, rrstd = st_tiles
    pm, cm, rpm = g.psb()
    for c in range(16):
        k.mm(pm[:, cm:cm + n], od[:], z[:, c, 0:n], start=(c == 0), stop=(c == 15), reads=[rod, rz], writes=[rpm])
    for c in range(16):
        k.act(scr[:, c, 0:n], z[:, c, 0:n], AF.Square, reads=[rz], writes=[rscr])
    pv, cv, rpv = g.psb()
    for c in range(16):
        k.mm(pv[:, cv:cv + n], od[:], scr[:, c, 0:n], start=(c == 0), stop=(c == 15), reads=[rod, rscr],
             writes=[rpv])
    k.cp(mean[:, 0:n], pm[:, cm:cm + n], [rpm], [rmean], eng="act")
    k.tt(rstd[:, 0:n], mean[:, 0:n], mean[:, 0:n], ALU.mult, reads=[rmean], writes=[rrstd], eng="pool")
    k.tt(rstd[:, 0:n], pv[:, cv:cv + n], rstd[:, 0:n], ALU.subtract, reads=[rpv, rrstd], writes=[rrstd])
    k.act(rstd[:, 0:n], rstd[:, 0:n], AF.Sqrt, bias=epsc[:, 0:1], scale=1.0, reads=[rrstd, rpar], writes=[rrstd])
    k.op("dve", lambda e, o_=rstd[:, 0:n]: e.reciprocal(o_, o_), [rrstd], [rrstd])
    for c in range(16):
        k.tt(scr[:, c, 0:n], z[:, c, 0:n], mean[:, 0:n], ALU.subtract, reads=[rz, rmean], writes=[rscr])
        k.tt(scr[:, c, 0:n], scr[:, c, 0:n], rstd[:, 0:n], ALU.mult, reads=[rscr, rrstd], writes=[rscr], eng="pool")
        k.act(z[:, c, 0:n], scr[:, c, 0:n], AF.Identity, bias=lnb[:, c:c + 1], scale=lnw[:, c:c + 1],
              reads=[rscr, rpar], writes=[rz])


def phase_merge(g, l, last):
    k, W, C = g.k, g.W, g.C
    halves = [[0, 1, 2]]
    g.ru2 = getattr(g, "ru2", None) or R("u2_d")
    g.rcmbd = getattr(g, "rcmbd", None) or R("cmb_d")
    with ExitStack() as st:
        bbg = k.sb("mbbg", [128, 64], F32, st)
        lnw = k.sb("mlnw", [128, 16], F32, st)
        lnb = k.sb("mlnb", [128, 16], F32, st)
        wr = k.sb("mwr", [128, 16, 36], F32, st)
        br = k.sb("mbr", [36, 1], F32, st)
        od = k.sb("mod2048", [128, 128], F32, st)
        epsc = k.sb("mepsc", [128, 1], F32, st)
        rpar = R()
        rod = R()
        k.dma(bbg[:], W["b_bgate"][l].rearrange("(j p) -> p j", p=128), writes=[rpar], allow_slow_non_contiguous=True)
        k.dma(lnw[:], W["ln1_w"][l].rearrange("(c p) -> p c", p=128), writes=[rpar], allow_slow_non_contiguous=True)
        k.dma(lnb[:], W["ln1_b"][l].rearrange("(c p) -> p c", p=128), writes=[rpar], allow_slow_non_contiguous=True)
        k.dma(wr[:, :, 0:4], W["moe_w_grp"][l].rearrange("(c p) n -> p c n", p=128), writes=[rpar])
        k.dma(wr[:, :, 4:36], W["moe_w_exp"][l].rearrange("(c p) n -> p c n", p=128), writes=[rpar])
        k.dma(br[0:4, :], W["moe_b_grp"][l].rearrange("(p o) -> p o", o=1), writes=[rpar])
        k.dma(br[4:36, :], W["moe_b_exp"][l].rearrange("(p o) -> p o", o=1), writes=[rpar])
        k.memset(od[:], 1.0 / 2048.0, writes=[rod], eng="dve")
        k.memset(epsc[:], LN_EPS, writes=[rpar], eng="dve")
        mg = k.sb("mmg", [128, 16, HALF], BF16, st)
        rmg = R()
        wi = 0
        for half in halves:
            blks = list(LB)
            p0 = 0
            np_ = HALF
            with ExitStack() as st1:
                u1 = k.sb("mu1", [128, 16, HALF], BF16, st1)
                yb = k.sb("myb", [128, 16, HALF], BF16, st1)
                ysa = [k.sb("mysa%d" % i, [128, HALF], BF16, st1) for i in range(2)]
                ysb = [k.sb("mysb%d" % i, [128, HALF], BF16, st1) for i in range(2)]
                rysa, rysb = [R(), R()], [R(), R()]
                ru1, ryb = R(), R()
                xs_ = [k.sb("mxs%d" % i, [128, 512], F32, st1) for i in range(3)]
                rxs_ = [R() for _ in range(3)]
                wbg = [k.sb("mwbg%d" % i, [128, 16, 512], BF16, st1) for i in range(2)]
                rwbg = [R(), R()]
                wbr = [k.sb("mwbr%d" % i, [128, 4, 512], BF16, st1) for i in range(2)]
                rwbr = [R(), R()]
                gtt = [k.sb("mgt%d" % i, [128, 512], F32, st1) for i in range(2)]
                rgt = [R(), R()]
                tt_ = [k.sb("mtt%d" % i, [128, 512], F32, st1) for i in range(2)]
                rtt = [R(), R()]
                macc = [[k.sb("mmacc%d_%d" % (cc, bi), [128, 512], F32, st1) for bi in range(len(blks))]
                        for cc in range(4)]
                rmacc = [[R() for bi in range(len(blks))] for cc in range(4)]
                for idx in range(16):
                    sb_ = idx % 2
                    k.dma(ysa[sb_][:], g.ybr4[0, idx], reads=[g.rybr], writes=[rysa[sb_]])
                    k.dma(ysb[sb_][:], g.ybr4[1, idx], reads=[g.rybr], writes=[rysb[sb_]])
                    k.ts(yb[:, idx, :], ysa[sb_][:], g.sel[:, 0:1], None, ALU.mult, reads=[rysa[sb_], g.rconst],
                         writes=[ryb])
                    k.stt(yb[:, idx, :], ysb[sb_][:], g.sel[:, 1:2], yb[:, idx, :], ALU.mult, ALU.add,
                          reads=[rysb[sb_], g.rconst, ryb], writes=[ryb])
                xi = 0
                for (t0, n) in blks:
                    j = 2 if t0 == 0 else 0
                    o = t0 - p0
                    for c in range(16):
                        b = xi % 3
                        xi += 1
                        k.dma(xs_[b][:, 0:n], g.xo3[c, :, t0:t0 + n], reads=[g.rxo], writes=[rxs_[b]])
                        k.act(u1[:, c, o:o + n], xs_[b][:, 0:n], AF.Identity, bias=mod(g, l, 0, c, j),
                              scale=mod(g, l, 1, c, j), reads=[rxs_[b], g.rmod], writes=[ru1])
                gi = 0
                for c4 in range(4):
                    for kbr in range(4):
                        b = wi % 2
                        wi += 1
                        k.dma(wbg[b][:], W["w_bgate"][l, :, kbr * 2048 + c4 * 512:kbr * 2048 + (c4 + 1) * 512].rearrange(
                            "(cc p) n -> p cc n", p=128), writes=[rwbg[b]], issuer="pool")
                        k.dma(wbr[b][:], W["w_branch"][l, kbr, :, c4 * 512:(c4 + 1) * 512].rearrange(
                            "(cc p) n -> p cc n", p=128), writes=[rwbr[b]], issuer="pool")
                        for bi, (t0, n) in enumerate(blks):
                            o = t0 - p0
                            for cc in range(4):
                                c = c4 * 4 + cc
                                pg, cg, rpg = g.psb()
                                for kc in range(16):
                                    k.mm(pg[:, cg:cg + n], wbg[b][:, kc, cc * 128:(cc + 1) * 128], u1[:, kc, o:o + n],
                                         start=(kc == 0), stop=(kc == 15), reads=[rwbg[b], ru1], writes=[rpg])
                                pp, cp_, rpp = g.psb()
                                for q4 in range(4):
                                    k.mm(pp[:, cp_:cp_ + n], wbr[b][:, q4, cc * 128:(cc + 1) * 128],
                                         yb[:, kbr * 4 + q4, o:o + n], start=(q4 == 0), stop=(q4 == 3),
                                         reads=[rwbr[b], ryb], writes=[rpp])
                                gb_ = gi % 2
                                gi += 1
                                k.act(gtt[gb_][:, 0:n], pg[:, cg:cg + n], AF.Sigmoid,
                                      bias=bbg[:, kbr * 16 + c:kbr * 16 + c + 1], scale=1.0, reads=[rpg, rpar],
                                      writes=[rgt[gb_]])
                                ma, rma = macc[cc][bi], rmacc[cc][bi]
                                if kbr == 0:
                                    k.tt(ma[:, 0:n], pp[:, cp_:cp_ + n], gtt[gb_][:, 0:n], ALU.mult,
                                         reads=[rpp, rgt[gb_]], writes=[rma])
                                else:
                                    k.tt(tt_[gb_][:, 0:n], pp[:, cp_:cp_ + n], gtt[gb_][:, 0:n], ALU.mult,
                                         reads=[rpp, rgt[gb_]], writes=[rtt[gb_]])
                                    if kbr < 3:
                                        k.tt(ma[:, 0:n], ma[:, 0:n], tt_[gb_][:, 0:n], ALU.add,
                                             reads=[rma, rtt[gb_]], writes=[rma], eng="pool")
                                    else:
                                        k.tt(mg[:, c, o:o + n], ma[:, 0:n], tt_[gb_][:, 0:n], ALU.add,
                                             reads=[rma, rtt[gb_]], writes=[rmg], eng="pool")
                k.barrier()
            with ExitStack() as st2:
                xb = k.sb("mxb", [128, 16, 512], F32, st2)
                z = k.sb("mz", [128, 16, 512], F32, st2)
                u2b = k.sb("mu2b", [128, 16, 512], BF16, st2)
                rxb, rz, ru2b = R(), R(), R()
                wo = [k.sb("mwo%d" % i, [128, 16, 512], BF16, st2) for i in range(2)]
                rwo = [R(), R()]
                mean = k.sb("mmean", [128, 512], F32, st2)
                rstd = k.sb("mrstd", [128, 512], F32, st2)
                lgt = k.sb("mlgt", [36, 512], F32, st2)
                rlgt = R()
                lnt = (mean, R(), rstd, R())
                L = k.sb("mL", [128, 36], F32, st2)
                sm = k.sb("msm", [128, 16], F32, st2)
                gh = k.sb("mgh", [128, 4], F32, st2)
                mk1 = k.sb("mmk1", [128, 32], F32, st2)
                mk2 = k.sb("mmk2", [128, 32], F32, st2)
                oh1 = k.sb("moh1", [128, 32], F32, st2)
                oh2 = k.sb("moh2", [128, 32], F32, st2)
                cmb = k.sb("mcmb", [128, 32], F32, st2)
                cmbT = k.sb("mcmbT", [32, 512], F32, st2)
                rL, rsm, rgh, rmk1, rmk2, roh1, roh2, rcmb, rcmbT = [R() for _ in range(9)]
                g.rx1 = getattr(g, "rx1", None) or R("x1own")
                for (t0, n) in blks:
                    j = 2 if t0 == 0 else 0
                    o = t0 - p0
                    k.dma(xb[:, :, 0:n], g.xo3[:, :, t0:t0 + n].rearrange("c p t -> p c t"), reads=[g.rxo],
                          writes=[rxb])
                    k.ts(xb[:, :, 0:n], xb[:, :, 0:n], ALPHA, None, ALU.mult, reads=[rxb], writes=[rxb], eng="pool")
                    for grp in range(4):
                        b = wi % 2
                        wi += 1
                        k.dma(wo[b][:], W["w_out"][l, :, grp * 512:(grp + 1) * 512].rearrange("(cc p) n -> p cc n", p=128),
                              writes=[rwo[b]], issuer="pool")
                        for nn in range(4):
                            ni = grp * 4 + nn
                            po, co, rpo = g.psb()
                            for c in range(16):
                                k.mm(po[:, co:co + n], wo[b][:, c, nn * 128:(nn + 1) * 128], mg[:, c, o:o + n],
                                     start=(c == 0), stop=(c == 15), reads=[rwo[b], rmg], writes=[rpo])
                            k.stt(z[:, ni, 0:n], po[:, co:co + n], mod(g, l, 2, ni, j), xb[:, ni, 0:n], ALU.mult,
                                  ALU.add, reads=[rpo, g.rmod, rxb], writes=[rz])
                    ln_block(g, z, rz, n, xb, rxb, lnw, lnb, rpar, od, rod, epsc, lnt)
                    k.dma(g.x1o3[:, :, t0:t0 + n].rearrange("c p t -> p c t"), z[:, :, 0:n], reads=[rz],
                          writes=[g.rx1])
                    for c in range(16):
                        k.act(xb[:, c, 0:n], z[:, c, 0:n], AF.Identity, bias=mod(g, l, 3, c, j),
                              scale=mod(g, l, 4, c, j), reads=[rz, g.rmod], writes=[rxb])
                    k.cp(u2b[:, :, 0:n], xb[:, :, 0:n], [rxb], [ru2b], eng="pool")
                    k.dma(g.u2_d[:, :, t0:t0 + n].rearrange("c p t -> p c t"), u2b[:, :, 0:n], reads=[ru2b],
                          writes=[g.ru2])
                    pl_, cl, rpl = g.psb()
                    for c in range(16):
                        k.mm(pl_[0:36, cl:cl + n], wr[:, c, :], xb[:, c, 0:n], start=(c == 0), stop=(c == 15),
                             reads=[rpar, rxb], writes=[rpl])
                    k.act(lgt[:, 0:n], pl_[0:36, cl:cl + n], AF.Identity, bias=br[:, 0:1], scale=1.0,
                          reads=[rpl, rpar], writes=[rlgt])
                    for ti in range(n // 128):
                        tsl = slice(ti * 128, (ti + 1) * 128)
                        pt, ct, rpt = g.psb()
                        k.tr(pt[:, ct:ct + 36], lgt[:, tsl], g.ident[0:36, 0:36], reads=[rlgt, g.rconst],
                             writes=[rpt])
                        k.cp(L[:], pt[:, ct:ct + 36], [rpt], [rL])
                        D_ = "dve"
                        k.op(D_, lambda e: e.tensor_reduce(sm[:, 0:1], L[:, 0:4], AX.X, ALU.max), [rL], [rsm])
                        k.ts(gh[:], L[:, 0:4], sm[:, 0:1], None, ALU.subtract, reads=[rL, rsm], writes=[rgh])
                        k.act(gh[:], gh[:], AF.Exp, reads=[rgh], writes=[rgh])
                        k.op(D_, lambda e: e.tensor_reduce(sm[:, 1:2], gh[:], AX.X, ALU.add), [rgh], [rsm])
                        k.op(D_, lambda e: e.reciprocal(sm[:, 2:3], sm[:, 1:2]), [rsm], [rsm])
                        k.ts(gh[:], L[:, 0:4], sm[:, 0:1], None, ALU.is_equal, reads=[rL, rsm, rgh], writes=[rgh])
                        k.ts(gh[:], gh[:], -1.0, 1e30, ALU.add, ALU.mult, reads=[rgh], writes=[rgh])
                        k.tt(mk1[:].rearrange("p (a b) -> p a b", b=8), L[:, 4:36].rearrange("p (a b) -> p a b", b=8),
                             gh[:].rearrange("p (a o) -> p a o", o=1).to_broadcast([128, 4, 8]), ALU.add,
                             reads=[rL, rgh], writes=[rmk1])
                        k.op(D_, lambda e: e.tensor_reduce(sm[:, 3:4], mk1[:], AX.X, ALU.max), [rmk1], [rsm])
                        k.ts(oh1[:], mk1[:], sm[:, 3:4], None, ALU.is_equal, reads=[rmk1, rsm], writes=[roh1])
                        k.stt(mk2[:], oh1[:], -1e30, mk1[:], ALU.mult, ALU.add, reads=[roh1, rmk1], writes=[rmk2])
                        k.op(D_, lambda e: e.tensor_reduce(sm[:, 4:5], mk2[:], AX.X, ALU.max), [rmk2], [rsm])
                        k.ts(oh2[:], mk2[:], sm[:, 4:5], None, ALU.is_equal, reads=[rmk2, rsm], writes=[roh2])
                        k.tt(sm[:, 5:6], sm[:, 4:5], sm[:, 3:4], ALU.subtract, reads=[rsm], writes=[rsm])
                        k.act(sm[:, 6:7], sm[:, 5:6], AF.Exp, reads=[rsm], writes=[rsm])
                        k.ts(sm[:, 7:8], sm[:, 6:7], 1.0, None, ALU.add, reads=[rsm], writes=[rsm])
                        k.op(D_, lambda e: e.reciprocal(sm[:, 8:9], sm[:, 7:8]), [rsm], [rsm])
                        k.tt(sm[:, 9:10], sm[:, 8:9], sm[:, 2:3], ALU.mult, reads=[rsm], writes=[rsm])
                        k.tt(sm[:, 10:11], sm[:, 9:10], sm[:, 6:7], ALU.mult, reads=[rsm], writes=[rsm])
                        k.ts(cmb[:], oh1[:], sm[:, 9:10], None, ALU.mult, reads=[roh1, rsm], writes=[rcmb])
                        k.stt(cmb[:], oh2[:], sm[:, 10:11], cmb[:], ALU.mult, ALU.add, reads=[roh2, rsm, rcmb],
                              writes=[rcmb])
                        pt2, ct2, rpt2 = g.psb()
                        k.tr(pt2[0:32, ct2:ct2 + 128], cmb[:], g.ident[:], reads=[rcmb, g.rconst], writes=[rpt2])
                        k.cp(cmbT[:, tsl], pt2[0:32, ct2:ct2 + 128], [rpt2], [rcmbT], eng="act")
                    k.dma(g.cmb_d[:, t0:t0 + n], cmbT[:, 0:n], reads=[rcmbT], writes=[g.rcmbd])
                k.barrier()
        k.barrier()


def phase_moe(g, l, last, n_exp=32):
    k, W, C = g.k, g.W, g.C
    parts = [[0, 1, 2]]
    with ExitStack() as st:
        u2 = k.sb("eu2", [128, 16, HALF], BF16, st)
        acc = k.sb("eacc", [128, 16, HALF], F32, st)
        ru2s, racc = R(), R()
        lnw = k.sb("elnw", [128, 16], F32, st)
        lnb = k.sb("elnb", [128, 16], F32, st)
        od = k.sb("eod", [128, 128], F32, st)
        epsc = k.sb("eepsc", [128, 1], F32, st)
        rpar, rod = R(), R()
        k.dma(lnw[:], W["ln2_w"][l].rearrange("(c p) -> p c", p=128), writes=[rpar], allow_slow_non_contiguous=True)
        k.dma(lnb[:], W["ln2_b"][l].rearrange("(c p) -> p c", p=128), writes=[rpar], allow_slow_non_contiguous=True)
        k.memset(od[:], 1.0 / 2048.0, writes=[rod], eng="dve")
        k.memset(epsc[:], LN_EPS, writes=[rpar], eng="dve")
        for part in parts:
            blks = list(LB)
            p0 = 0
            np_ = HALF
            k.dma(u2[:, :, 0:np_], g.u2_d[:, :, p0:p0 + np_].rearrange("c p t -> p c t"), reads=[g.ru2],
                  writes=[ru2s])
            k.memset(acc[:, :, 0:np_], 0.0, writes=[racc], eng="pool")
            with ExitStack() as st2:
                w13 = [k.sb("ew13_%d" % i, [128, 16, 2, 128], BF16, st2) for i in range(2)]
                rw13 = [R(), R()]
                w2b = [k.sb("ew2_%d" % i, [128, 4, 2048], BF16, st2) for i in range(2)]
                rw2 = [R(), R()]
                hT = k.sb("ehT", [128, 4, HALF], BF16, st2)
                rhT = R()
                cb = [k.sb("ecb%d" % i, [128, 512], F32, st2) for i in range(3)]
                rcb = [R() for _ in range(3)]
                sg = [k.sb("esg%d" % i, [128, 512], F32, st2) for i in range(2)]
                rsg = [R(), R()]
                wi = 0
                ci = 0
                for e in range(n_exp):
                    eb = e % 2
                    k.dma(w2b[eb][:], W["moe_w2"][l, e].rearrange("(f p) n -> p f n", p=128), writes=[rw2[eb]],
                          issuer="pool")
                    cbs = []
                    for (t0, n) in blks:
                        cix = ci % 3
                        ci += 1
                        k.dma(cb[cix][:, 0:n], g.cmb_d[e, t0:t0 + n].partition_broadcast(128), reads=[g.rcmbd],
                              writes=[rcb[cix]])
                        cbs.append(cix)
                    for f in range(4):
                        b = wi % 2
                        wi += 1
                        k.dma(w13[b][:, :, 0, :],
                              W["moe_w1"][l, e, :, f * 128:(f + 1) * 128].rearrange("(c p) n -> p c n", p=128),
                              writes=[rw13[b]], issuer="pool")
                        k.dma(w13[b][:, :, 1, :],
                              W["moe_w3"][l, e, :, f * 128:(f + 1) * 128].rearrange("(c p) n -> p c n", p=128),
                              writes=[rw13[b]], issuer="pool")
                        for bi, (t0, n) in enumerate(blks):
                            o = t0 - p0
                            p1, c1, rp1 = g.psb()
                            for c in range(16):
                                k.mm(p1[:, c1:c1 + n], w13[b][:, c, 0, :], u2[:, c, o:o + n], start=(c == 0),
                                     stop=(c == 15), reads=[rw13[b], ru2s], writes=[rp1])
                            p3, c3, rp3 = g.psb()
                            for c in range(16):
                                k.mm(p3[:, c3:c3 + n], w13[b][:, c, 1, :], u2[:, c, o:o + n], start=(c == 0),
                                     stop=(c == 15), reads=[rw13[b], ru2s], writes=[rp3])
                            sb_ = (f * 8 + bi) % 2
                            k.act(sg[sb_][:, 0:n], p1[:, c1:c1 + n], AF.Silu, reads=[rp1], writes=[rsg[sb_]])
                            k.tt(sg[sb_][:, 0:n], p3[:, c3:c3 + n], sg[sb_][:, 0:n], ALU.mult, reads=[rp3, rsg[sb_]],
                                 writes=[rsg[sb_]])
                            k.tt(hT[:, f, o:o + n], sg[sb_][:, 0:n], cb[cbs[bi]][:, 0:n], ALU.mult,
                                 reads=[rsg[sb_], rcb[cbs[bi]]], writes=[rhT], eng="pool")
                    for bi, (t0, n) in enumerate(blks):
                        o = t0 - p0
                        for nn in range(16):
                            po, co, rpo = g.psb()
                            for f in range(4):
                                k.mm(po[:, co:co + n], w2b[eb][:, f, nn * 128:(nn + 1) * 128], hT[:, f, o:o + n],
                                     start=(f == 0), stop=(f == 3), reads=[rw2[eb], rhT], writes=[rpo])
                            k.tt(acc[:, nn, o:o + n], po[:, co:co + n], acc[:, nn, o:o + n], ALU.add,
                                 reads=[rpo, racc], writes=[racc])
                k.barrier()
            with ExitStack() as st3:
                xb = k.sb("exb", [128, 16, 512], F32, st3)
                zz = k.sb("ezz", [128, 16, 512], F32, st3)
                mean = k.sb("emean", [128, 512], F32, st3)
                rstd = k.sb("erstd", [128, 512], F32, st3)
                rxb, rzz = R(), R()
                lnt = (mean, R(), rstd, R())
                for (t0, n) in blks:
                    j = 2 if t0 == 0 else 0
                    o = t0 - p0
                    k.dma(xb[:, :, 0:n], g.x1o3[:, :, t0:t0 + n].rearrange("c p t -> p c t"), reads=[g.rx1],
                          writes=[rxb])
                    k.ts(xb[:, :, 0:n], xb[:, :, 0:n], ALPHA, None, ALU.mult, reads=[rxb], writes=[rxb], eng="pool")
                    for c in range(16):
                        k.stt(zz[:, c, 0:n], acc[:, c, o:o + n], mod(g, l, 5, c, j), xb[:, c, 0:n], ALU.mult,
                              ALU.add, reads=[racc, g.rmod, rxb], writes=[rzz])
                    ln_block(g, zz, rzz, n, xb, rxb, lnw, lnb, rpar, od, rod, epsc, lnt)
                    k.dma(g.xo3[:, :, t0:t0 + n].rearrange("c p t -> p c t"), zz[:, :, 0:n], reads=[rzz],
                          writes=[g.rxo])
                k.barrier()
        for c_ in range(KC):
            k.coll(lambda e, c_=c_: e.collective_compute(
                "AllGather", ALU.bypass, replica_groups=g.rgroups,
                ins=[g.xown_t.ap()[c_ * 128:(c_ + 1) * 128, :]], outs=[g.xs2_t.ap()[c_ * 256:(c_ + 1) * 256, :]]),
                reads=[g.rxo], writes=[g.rxs])
        k.barrier()

from concourse.bass_utils import run_bass_kernel_spmd

N_CORES = 8


def kernel(**inputs):
    nc = build(nl=DEPTH)
    cs = host_consts()
    f32 = lambda a: np.ascontiguousarray(np.asarray(a, dtype=np.float32))
    wts = {n: f32(inputs[n]) for n in W_SHAPES}
    in_maps = []
    for r in range(N_CORES):
        b, h = r // 2, r % 2
        m = dict(wts)
        xin = np.concatenate([np.asarray(inputs["ctx"][b]), np.asarray(inputs["x"][b])], 0).astype(np.float32)
        m["xin"] = np.ascontiguousarray(xin)
        m["xown_in"] = np.ascontiguousarray(xin[h * HALF:(h + 1) * HALF])
        c_s0 = np.asarray(inputs["c_ctx"]) if h == 0 else np.asarray(inputs["c"][b])
        m["c2"] = f32(np.stack([np.asarray(inputs["c"][b]), np.asarray(inputs["c_ctx"]), c_s0], 0))
        sel = np.zeros((128, 2), np.float32)
        sel[:, h] = 1.0
        m["sel"] = sel
        for n, v in cs.items():
            m["k_" + n] = v
        in_maps.append(m)
    res = run_bass_kernel_spmd(nc, in_maps, core_ids=list(range(N_CORES)))
    out = np.stack([np.asarray(res.results[2 * b]["out"], dtype=np.float32) for b in range(4)], 0)
    return out
```
